# Optimizing a Trainium2 kernel written in Bass

```python
import jax, jax.numpy as jnp
from jax import lax
import numpy as np

D_MODEL = 2048
BATCH = 2
SEQ = 8192
DEPTH = 2

GRID_W = 64
CTX_LEN = 256
HEAD_DIM = 128
ATT_Q_HEADS = 4
ATT_KV_HEADS = 2
LRU_WIDTH = 1024
LRU_BLOCKS = 8
LRU_BLOCK = LRU_WIDTH // LRU_BLOCKS
LRU_C = 8.0
CONV_W = 4
NA_HEADS = 4
NA_ROWS = 8
NA_COLS = 16
Q_BLOCK = 128
ATT_Q_W = ATT_Q_HEADS * HEAD_DIM
ATT_KV_W = ATT_KV_HEADS * HEAD_DIM
NA_W = NA_HEADS * HEAD_DIM
MIX_WIDTH = ATT_Q_W + LRU_WIDTH + NA_W
IN_WIDTH = ATT_Q_W + 2 * ATT_KV_W + 2 * LRU_WIDTH + 3 * NA_W
MOE_GROUPS = 4
MOE_EXPERTS_PER_GROUP = 8
MOE_EXPERTS = MOE_GROUPS * MOE_EXPERTS_PER_GROUP
MOE_TOP_K = 2
MOE_HIDDEN = 1024
MOE_BLOCK = 128
ROPE_THETA = 10000.0
EPS = 1e-6

kernel_name = 'hybrid_parallel_mixer_dit_block'


def _rmsnorm(x, g):
    x32 = x.astype(jnp.float32)
    y = x32 * lax.rsqrt(jnp.mean(x32 * x32, axis=-1, keepdims=True) + EPS)
    return y.astype(x.dtype) * g


def _heads(t, n):
    return t.reshape(*t.shape[:-1], n, HEAD_DIM)


def _split_cols(p):
    sizes = (ATT_Q_W, ATT_KV_W, ATT_KV_W, LRU_WIDTH, LRU_WIDTH, NA_W, NA_W, NA_W)
    idx = [int(i) for i in np.cumsum(sizes)[:-1]]
    return jnp.split(p, idx, axis=-1)


def _axial_rope_tables(n_tokens):
    pos = jnp.arange(n_tokens, dtype=jnp.int32)
    rows = (pos // GRID_W).astype(jnp.float32)
    cols = (pos % GRID_W).astype(jnp.float32)
    n_freq = HEAD_DIM // 4
    inv = 1.0 / (ROPE_THETA ** (jnp.arange(n_freq, dtype=jnp.float32) / n_freq))
    ang = jnp.stack([rows[:, None] * inv, cols[:, None] * inv], axis=1)
    return jnp.cos(ang), jnp.sin(ang)


def _rope(t, cos, sin):
    n_freq = HEAD_DIM // 4
    t32 = t.astype(jnp.float32).reshape(*t.shape[:-1], 2, 2, n_freq)
    t1, t2 = t32[..., 0, :], t32[..., 1, :]
    cs = cos[None, :, None]
    sn = sin[None, :, None]
    out = jnp.stack([t1 * cs - t2 * sn, t2 * cs + t1 * sn], axis=-2)
    return out.reshape(t.shape).astype(t.dtype)


def _gqa_attend(q, k, v):
    s = jnp.einsum('bqkgd,bskd->bkgqs', q, k).astype(jnp.float32) * (HEAD_DIM ** -0.5)
    p = jax.nn.softmax(s, axis=-1).astype(v.dtype)
    return jnp.einsum('bkgqs,bskd->bqkgd', p, v)


def _gqa_latent(q, k, v, k_ctx, v_ctx):
    B, S = q.shape[:2]
    g = ATT_Q_HEADS // ATT_KV_HEADS
    k_all = jnp.concatenate([k, k_ctx], axis=1)
    v_all = jnp.concatenate([v, v_ctx], axis=1)
    qb = jnp.moveaxis(q.reshape(B, S // Q_BLOCK, Q_BLOCK, ATT_KV_HEADS, g, HEAD_DIM), 1, 0)
    o = lax.map(lambda qi: _gqa_attend(qi, k_all, v_all), qb)
    return jnp.moveaxis(o, 0, 1).reshape(B, S, ATT_Q_W)


def _neighbourhood_latent(q, k, v, k_ctx, v_ctx, rpb):
    B, S = q.shape[:2]
    rows = S // GRID_W
    kr = min(NA_ROWS, rows)
    qg = q.reshape(B, rows, GRID_W, NA_HEADS, HEAD_DIM)
    kg = k.reshape(B, rows, GRID_W, NA_HEADS, HEAD_DIM)
    vg = v.reshape(B, rows, GRID_W, NA_HEADS, HEAD_DIM)
    j = np.arange(GRID_W)
    cs = np.clip(j - NA_COLS // 2, 0, GRID_W - NA_COLS)
    col_idx = cs[:, None] + np.arange(NA_COLS)[None, :]
    dc_idx = col_idx - j[:, None] + (NA_COLS - 1)
    bias_c = rpb[:, :, dc_idx]
    n_loc = kr * NA_COLS
    scale = HEAD_DIM ** -0.5

    def one_row(r):
        rs = jnp.clip(r - NA_ROWS // 2, 0, rows - kr)
        kb = lax.dynamic_slice_in_dim(kg, rs, kr, axis=1)
        vb = lax.dynamic_slice_in_dim(vg, rs, kr, axis=1)
        kn = kb[:, :, col_idx]
        vn = vb[:, :, col_idx]
        qr = lax.dynamic_index_in_dim(qg, r, axis=1, keepdims=False)
        dr_idx = rs + jnp.arange(kr) - r + (NA_ROWS - 1)
        bias = jnp.take(bias_c, dr_idx, axis=1).transpose(0, 2, 1, 3)
        s_loc = jnp.einsum('bjhd,bajchd->bhjac', qr, kn).astype(jnp.float32) * scale
        s_loc = s_loc + bias[None].astype(jnp.float32)
        s_ctx = jnp.einsum('bjhd,bnhd->bhjn', qr, k_ctx).astype(jnp.float32) * scale
        s = jnp.concatenate([s_loc.reshape(B, NA_HEADS, GRID_W, n_loc), s_ctx], axis=-1)
        p = jax.nn.softmax(s, axis=-1).astype(v.dtype)
        p_loc = p[..., :n_loc].reshape(B, NA_HEADS, GRID_W, kr, NA_COLS)
        p_ctx = p[..., n_loc:]
        o = jnp.einsum('bhjac,bajchd->bjhd', p_loc, vn) + jnp.einsum('bhjn,bnhd->bjhd', p_ctx, v_ctx)
        return o.reshape(B, GRID_W, NA_W)

    o = lax.map(one_row, jnp.arange(rows, dtype=jnp.int32))
    return jnp.moveaxis(o, 0, 1).reshape(B, S, NA_W)


def _dwconv(x, w, b):
    L = x.shape[1]
    left = (CONV_W - 1) // 2
    right = CONV_W - 1 - left
    xp = jnp.pad(x, ((0, 0), (left, right), (0, 0)))
    y = b
    for i in range(CONV_W):
        y = y + xp[:, i:i + L] * w[i]
    return y


def _blockdiag(x, w, b):
    xb = x.reshape(*x.shape[:-1], LRU_BLOCKS, LRU_BLOCK)
    return jnp.einsum('blnk,nkm->blnm', xb, w).reshape(x.shape) + b


def _lin_combine(left, right):
    return (left[0] * right[0], right[0] * left[1] + right[1])


def _rglru_dir(u, wr, br, wi, bi, lam, h0, reverse):
    if reverse:
        u = jnp.flip(u, axis=1)
    r = jax.nn.sigmoid(_blockdiag(u, wr, br)).astype(jnp.float32)
    i = jax.nn.sigmoid(_blockdiag(u, wi, bi)).astype(jnp.float32)
    log_a = -LRU_C * r * jax.nn.softplus(-lam.astype(jnp.float32))
    a = jnp.exp(log_a)
    b = jnp.sqrt(-jnp.expm1(2.0 * log_a)) * (i * u.astype(jnp.float32))
    a_cum, b_cum = lax.associative_scan(_lin_combine, (a, b), axis=1)
    h = b_cum if h0 is None else a_cum * h0[:, None] + b_cum
    final = h[:, -1]
    if reverse:
        h = jnp.flip(h, axis=1)
    return h, final


def _rglru_mixer(ul, gl, uc, gc, conv_w, conv_b, wr, br, wi, bi, lam, with_ctx_out):
    ul = _dwconv(ul, conv_w, conv_b)
    uc = _dwconv(uc, conv_w, conv_b)
    yl = None
    yc = None
    for d in range(2):
        hc, hc_final = _rglru_dir(uc, wr[d], br[d], wi[d], bi[d], lam[d], None, d == 1)
        hl, _ = _rglru_dir(ul, wr[d], br[d], wi[d], bi[d], lam[d], hc_final, d == 1)
        yl = hl if yl is None else yl + hl
        yc = hc if yc is None else yc + hc
    out_l = yl.astype(gl.dtype) * jax.nn.gelu(gl)
    out_c = yc.astype(gc.dtype) * jax.nn.gelu(gc) if with_ctx_out else None
    return out_l, out_c


def _hmoe(h, wg, bg, we, be, w_gate, w_up, w_down):
    T, D = h.shape
    lg = (h @ wg + bg).astype(jnp.float32)
    pg = jax.nn.softmax(lg, axis=-1)
    g_star = jnp.argmax(lg, axis=-1).astype(jnp.int32)
    g_gate = jnp.max(pg, axis=-1)
    le = (h @ we + be).astype(jnp.float32).reshape(T, MOE_GROUPS, MOE_EXPERTS_PER_GROUP)
    le_sel = jnp.take_along_axis(le, g_star[:, None, None], axis=1)[:, 0]
    vals, idx = lax.top_k(le_sel, MOE_TOP_K)
    w = (g_gate[:, None] * jax.nn.softmax(vals, axis=-1)).reshape(-1)
    e_ids = (g_star[:, None] * MOE_EXPERTS_PER_GROUP + idx).reshape(-1).astype(jnp.int32)
    tok = jnp.repeat(jnp.arange(T, dtype=jnp.int32), MOE_TOP_K)
    tk = T * MOE_TOP_K
    n_blocks = -(-(tk + MOE_EXPERTS * (MOE_BLOCK - 1)) // MOE_BLOCK)
    P = n_blocks * MOE_BLOCK
    order = jnp.argsort(e_ids)
    e_sorted = e_ids[order]
    counts = jnp.bincount(e_ids, length=MOE_EXPERTS)
    starts = jnp.cumsum(counts) - counts
    padded = (counts + MOE_BLOCK - 1) // MOE_BLOCK * MOE_BLOCK
    pad_ends = jnp.cumsum(padded)
    pad_starts = pad_ends - padded
    dest = pad_starts[e_sorted] + (jnp.arange(tk, dtype=jnp.int32) - starts[e_sorted])
    row_tok = jnp.full((P,), T, dtype=jnp.int32).at[dest].set(tok[order])
    row_w = jnp.zeros((P,), jnp.float32).at[dest].set(w[order])
    block_starts = jnp.arange(n_blocks, dtype=jnp.int32) * MOE_BLOCK
    block_e = jnp.minimum(jnp.searchsorted(pad_ends, block_starts, side='right'), MOE_EXPERTS - 1)
    xs = jnp.concatenate([h, jnp.zeros((1, D), h.dtype)], axis=0)[row_tok]
    xs = xs.reshape(n_blocks, MOE_BLOCK, D)

    def expert_block(args):
        xb, e = args
        return (jax.nn.silu(xb @ w_gate[e]) * (xb @ w_up[e])) @ w_down[e]

    yb = lax.map(expert_block, (xs, block_e)).reshape(P, D)
    y = jax.ops.segment_sum(yb * row_w[:, None].astype(yb.dtype), row_tok, num_segments=T + 1)
    return y[:T]


def setup_inputs(seed: int = 0) -> dict:
    key = jax.random.key(seed)
    ks = jax.random.split(key, 32)
    L = DEPTH
    f32 = jnp.float32

    def nrm(k, shape, scale):
        return jax.random.normal(k, shape, f32) * scale

    u = jax.random.uniform(ks[18], (L, 2, LRU_WIDTH), f32, 0.9, 0.999)
    return {
        'x': nrm(ks[0], (BATCH, SEQ, D_MODEL), 1.0),
        'c': nrm(ks[1], (BATCH, D_MODEL), 1.0),
        'ctx': nrm(ks[2], (BATCH, CTX_LEN, D_MODEL), 1.0),
        'c_ctx': nrm(ks[3], (D_MODEL,), 1.0),
        'ada_w': nrm(ks[4], (L, D_MODEL, 6 * D_MODEL), 0.5 * D_MODEL ** -0.5),
        'ada_b': nrm(ks[5], (L, 6 * D_MODEL), 0.01),
        'norm1_g': 1.0 + nrm(ks[6], (L, D_MODEL), 0.01),
        'norm2_g': 1.0 + nrm(ks[7], (L, D_MODEL), 0.01),
        'w_in': nrm(ks[8], (L, D_MODEL, IN_WIDTH), D_MODEL ** -0.5),
        'w_out': nrm(ks[9], (L, MIX_WIDTH, D_MODEL), MIX_WIDTH ** -0.5),
        'att_q_norm': 1.0 + nrm(ks[10], (L, HEAD_DIM), 0.01),
        'att_k_norm': 1.0 + nrm(ks[11], (L, HEAD_DIM), 0.01),
        'conv_w': nrm(ks[12], (L, CONV_W, LRU_WIDTH), CONV_W ** -0.5),
        'conv_b': nrm(ks[13], (L, LRU_WIDTH), 0.01),
        'lru_wr': nrm(ks[14], (L, 2, LRU_BLOCKS, LRU_BLOCK, LRU_BLOCK), LRU_BLOCK ** -0.5),
        'lru_br': nrm(ks[15], (L, 2, LRU_WIDTH), 0.01),
        'lru_wi': nrm(ks[16], (L, 2, LRU_BLOCKS, LRU_BLOCK, LRU_BLOCK), LRU_BLOCK ** -0.5),
        'lru_bi': nrm(ks[17], (L, 2, LRU_WIDTH), 0.01),
        'lru_lambda': jnp.log(u) - jnp.log1p(-u),
        'na_rpb': nrm(ks[19], (L, NA_HEADS, 2 * NA_ROWS - 1, 2 * NA_COLS - 1), 0.1),
        'router_wg': nrm(ks[20], (L, D_MODEL, MOE_GROUPS), D_MODEL ** -0.5),
        'router_bg': nrm(ks[21], (L, MOE_GROUPS), 0.01),
        'router_we': nrm(ks[22], (L, D_MODEL, MOE_EXPERTS), D_MODEL ** -0.5),
        'router_be': nrm(ks[23], (L, MOE_EXPERTS), 0.01),
        'moe_w_gate': nrm(ks[24], (L, MOE_EXPERTS, D_MODEL, MOE_HIDDEN), D_MODEL ** -0.5),
        'moe_w_up': nrm(ks[25], (L, MOE_EXPERTS, D_MODEL, MOE_HIDDEN), D_MODEL ** -0.5),
        'moe_w_down': nrm(ks[26], (L, MOE_EXPERTS, MOE_HIDDEN, D_MODEL), MOE_HIDDEN ** -0.5),
        'final_g': 1.0 + nrm(ks[27], (D_MODEL,), 0.01),
    }


def reference(x, c, ctx, c_ctx, ada_w, ada_b, norm1_g, norm2_g, w_in, w_out, att_q_norm, att_k_norm,
              conv_w, conv_b, lru_wr, lru_br, lru_wi, lru_bi, lru_lambda, na_rpb,
              router_wg, router_bg, router_we, router_be, moe_w_gate, moe_w_up, moe_w_down, final_g):
    B, S, D = x.shape
    C = ctx.shape[1]
    g_att = ATT_Q_HEADS // ATT_KV_HEADS
    cos, sin = _axial_rope_tables(S)
    s_lat = jax.nn.silu(c)
    s_ctx = jax.nn.silu(c_ctx)
    xc = ctx
    for l in range(DEPTH):
        update_ctx = l < DEPTH - 1
        m_lat = jnp.split((s_lat @ ada_w[l] + ada_b[l])[:, None, :], 6, axis=-1)
        m_ctx = jnp.split(s_ctx @ ada_w[l] + ada_b[l], 6, axis=-1)
        h = _rmsnorm(x, norm1_g[l]) * (1.0 + m_lat[1]) + m_lat[0]
        hc = _rmsnorm(xc, norm1_g[l]) * (1.0 + m_ctx[1]) + m_ctx[0]
        qa, ka, va, ub, gb, qn, kn, vn = _split_cols(h @ w_in[l])
        qa_c, ka_c, va_c, ub_c, gb_c, qn_c, kn_c, vn_c = _split_cols(hc @ w_in[l])

        qa = _rope(_rmsnorm(_heads(qa, ATT_Q_HEADS), att_q_norm[l]), cos, sin)
        ka = _rope(_rmsnorm(_heads(ka, ATT_KV_HEADS), att_k_norm[l]), cos, sin)
        ka_c = _rmsnorm(_heads(ka_c, ATT_KV_HEADS), att_k_norm[l])
        va_c = _heads(va_c, ATT_KV_HEADS)
        oa = _gqa_latent(qa, ka, _heads(va, ATT_KV_HEADS), ka_c, va_c)

        ob, ob_c = _rglru_mixer(ub, gb, ub_c, gb_c, conv_w[l], conv_b[l], lru_wr[l], lru_br[l],
                                lru_wi[l], lru_bi[l], lru_lambda[l], update_ctx)

        kn_c = _heads(kn_c, NA_HEADS)
        vn_c = _heads(vn_c, NA_HEADS)
        oc = _neighbourhood_latent(_heads(qn, NA_HEADS), _heads(kn, NA_HEADS), _heads(vn, NA_HEADS),
                                   kn_c, vn_c, na_rpb[l])

        x = x + m_lat[2] * (jnp.concatenate([oa, ob, oc], axis=-1) @ w_out[l])
        h2 = _rmsnorm(x, norm2_g[l]) * (1.0 + m_lat[4]) + m_lat[3]
        tokens = h2.reshape(B * S, D)
        if update_ctx:
            qa_c = _rmsnorm(_heads(qa_c, ATT_Q_HEADS), att_q_norm[l])
            oa_c = _gqa_attend(qa_c.reshape(B, C, ATT_KV_HEADS, g_att, HEAD_DIM), ka_c, va_c)
            oc_c = _gqa_attend(_heads(qn_c, NA_HEADS)[:, :, :, None], kn_c, vn_c)
            o_c = jnp.concatenate([oa_c.reshape(B, C, ATT_Q_W), ob_c, oc_c.reshape(B, C, NA_W)], axis=-1)
            xc = xc + m_ctx[2] * (o_c @ w_out[l])
            h2c = _rmsnorm(xc, norm2_g[l]) * (1.0 + m_ctx[4]) + m_ctx[3]
            y = _hmoe(jnp.concatenate([tokens, h2c.reshape(B * C, D)], axis=0), router_wg[l], router_bg[l],
                      router_we[l], router_be[l], moe_w_gate[l], moe_w_up[l], moe_w_down[l])
            x = x + m_lat[5] * y[:B * S].reshape(B, S, D)
            xc = xc + m_ctx[5] * y[B * S:].reshape(B, C, D)
        else:
            y = _hmoe(tokens, router_wg[l], router_bg[l], router_we[l], router_be[l],
                      moe_w_gate[l], moe_w_up[l], moe_w_down[l])
            x = x + m_lat[5] * y.reshape(B, S, D)
    return _rmsnorm(x, final_g)
```

```python
import numpy as np
import concourse.bass as bass
import concourse.mybir as mybir
from concourse.bass_utils import run_bass_kernel_spmd

F32 = mybir.dt.float32
F32R = mybir.dt.float32r
AF = mybir.ActivationFunctionType
ALU = mybir.AluOpType
AX = mybir.AxisListType

D = 2048; B = 2; S = 8192; C = 256; DEPTH = 2
NCH = 16
INW = 4608; NIC = 36
HID = 1024; NHC = 8
NE = 32
EPS = 1e-6
NCORES = 8


class R:
    __slots__ = ("w", "rs")

    def __init__(s):
        s.w = None
        s.rs = []


class Sch:
    def __init__(s, nc, ndma=20):
        s.nc = nc
        s.E = {"pe": nc.tensor, "act": nc.scalar, "dve": nc.vector, "pool": nc.gpsimd, "sp": nc.sync}
        s.sem = {}
        s.cnt = {}
        for k in ("pe", "act", "dve", "pool"):
            s.sem[k] = nc.alloc_semaphore("q_" + k)
            s.cnt[k] = 0
        s.seen = {k: {} for k in s.E}
        s.dpool = {}
        for q in ("sp", "pool", "act"):
            keys = []
            for i in range(ndma if q != "act" else 4):
                key = "d_%s%d" % (q, i)
                s.sem[key] = nc.alloc_semaphore(key)
                s.cnt[key] = 0
                keys.append(key)
            s.dpool[q] = [keys, 0]
        s.nid = 0

    def _wait(s, eng, toks):
        for t in toks:
            if t is None:
                continue
            key, val = t
            if key == eng and eng == "pe":
                continue
            if key.startswith("d_"):
                val = s.cnt[key]
            if s.seen[eng].get(key, 0) < val:
                s.E[eng].wait_ge(s.sem[key], val)
                s.seen[eng][key] = val

    def _deps(s, rd, wr):
        toks = []
        for r in rd:
            toks.append(r.w)
        for r in wr:
            toks.append(r.w)
            toks.extend(r.rs)
        return toks

    def _mark(s, tk, rd, wr):
        for r in rd:
            r.rs.append(tk)
            if len(r.rs) > 64:
                r.rs = r.rs[-48:]
        for r in wr:
            r.w = tk
            r.rs = []

    def op(s, eng, fn, rd=(), wr=()):
        s._wait(eng, s._deps(rd, wr))
        ins = fn()
        s.cnt[eng] += 1
        ins.then_inc(s.sem[eng], 1)
        s._mark((eng, s.cnt[eng]), rd, wr)

    def dma(s, q, out, in_, rd=(), wr=()):
        s._wait(q, s._deps(rd, wr))
        keys, i = s.dpool[q]
        key = keys[i % len(keys)]
        s.dpool[q][1] = i + 1
        s.cnt[key] += 16
        s.E[q].dma_start(out=out, in_=in_).then_inc(s.sem[key], 16)
        s._mark((key, s.cnt[key]), rd, wr)

    def finish(s, outs):
        toks = []
        for r in outs:
            toks.append(r.w)
        s._wait("sp", toks)
        for key in s.sem:
            if key.startswith("d_") and s.cnt[key] > 0:
                if s.seen["sp"].get(key, 0) < s.cnt[key]:
                    s.E["sp"].wait_ge(s.sem[key], s.cnt[key])
                    s.seen["sp"][key] = s.cnt[key]


def new_nc():
    nc = bass.Bass("TRN2", target_bir_lowering=False)
    nc.dge_precook = False
    return nc


def sb(nc, name, shape, dt=F32):
    return nc.alloc_sbuf_tensor(name, list(shape), dt).ap()


def din(nc, name, shape, dt=F32):
    return nc.dram_tensor(name, list(shape), dt, kind="ExternalInput").ap()


def dout(nc, name, shape, dt=F32):
    return nc.dram_tensor(name, list(shape), dt, kind="ExternalOutput").ap()


class Ring:
    def __init__(s, nc, name, n, shape, dt=F32, psum=False):
        s.b = []
        for i in range(n):
            if psum:
                ap = nc.alloc_psum_tensor("%s%d" % (name, i), list(shape), dt).ap()
            else:
                ap = sb(nc, "%s%d" % (name, i), shape, dt)
            s.b.append((ap, R()))
        s.i = 0

    def nxt(s):
        x = s.b[s.i % len(s.b)]
        s.i += 1
        return x


def mm(s, ps, psr, lhsT, rhs, start, stop, rd):
    s.op("pe", lambda: s.nc.tensor.matmul(ps, lhsT, rhs, start=start, stop=stop), rd=rd, wr=[psr])


def rstd(s, out, outr, ms, msr, epst, cr):
    nc = s.nc
    s.op("act", lambda: nc.scalar.activation(out=out, in_=ms, func=AF.Sqrt, bias=epst[:, 0:1], scale=1.0),
         rd=[msr, cr], wr=[outr])
    s.op("dve", lambda: nc.vector.reciprocal(out=out, in_=out), rd=[outr], wr=[outr])

def build_ada():
    nc = new_nc()
    s = Sch(nc)
    NCOL = 12288 // NCORES
    cT = din(nc, "cT", [128, NCH, 4])
    w = din(nc, "w", [DEPTH, NCH, 128, NCOL])
    bias = din(nc, "bias", [4, DEPTH, NCOL])
    out = dout(nc, "mod", [4, DEPTH, NCOL])
    ct = sb(nc, "ct", [128, NCH, 4]); ctr = R()
    sg = sb(nc, "sg", [128, NCH, 4])
    bt = sb(nc, "bt", [4, DEPTH, NCOL]); btr = R()
    ot = sb(nc, "ot", [4, DEPTH, NCOL]); otr = R()
    wr_ = Ring(nc, "w", 3, [128, 4, NCOL], F32)
    pr = Ring(nc, "ps", 4, [128, 512], F32, psum=True)
    s.dma("sp", ct, cT, wr=[ctr])
    s.dma("sp", bt, bias, wr=[btr])
    sgr = R()
    s.op("act", lambda: nc.scalar.activation(out=sg, in_=ct, func=AF.Sigmoid), rd=[ctr], wr=[sgr])
    s.op("dve", lambda: nc.vector.tensor_tensor(out=sg, in0=sg, in1=ct, op=ALU.mult), rd=[sgr, ctr], wr=[sgr])
    for l in range(DEPTH):
        pss = [pr.nxt() for _ in range(NCOL // 512)]
        for kq in range(NCH // 4):
            wt, wtr = wr_.nxt()
            s.dma("sp", wt, w[l, kq * 4:(kq + 1) * 4].rearrange("c p n -> p c n"), wr=[wtr])
            for kk in range(4):
                k = kq * 4 + kk
                for j, (ps, psr) in enumerate(pss):
                    mm(s, ps[0:4, :], psr, sg[:, k, :], wt[:, kk, j * 512:(j + 1) * 512], k == 0, k == NCH - 1,
                       [sgr, wtr])
        for j, (ps, psr) in enumerate(pss):
            s.op("dve", lambda ps=ps, j=j: nc.vector.tensor_tensor(
                out=ot[:, l, j * 512:(j + 1) * 512], in0=ps[0:4, :], in1=bt[:, l, j * 512:(j + 1) * 512], op=ALU.add),
                rd=[psr, btr], wr=[otr])
    s.dma("sp", out, ot, rd=[otr], wr=[R()])
    s.finish([])
    return nc


def build_p1(tiles):
    nc = new_nc()
    s = Sch(nc)
    T = sum(n for n, _ in tiles)
    TL = sum(n for n, k in tiles if k == 0)
    xT = din(nc, "xT", [NCH, 128, T])
    w = din(nc, "w", [NIC, 128, NCH, 128], F32R)
    g1 = din(nc, "g1", [128, NCH])
    scl = din(nc, "scl", [128, 2, NCH])
    sft = din(nc, "sft", [128, 2, NCH])
    gqk = din(nc, "gqk", [128, 2])
    cs = din(nc, "cs", [128, 2, max(TL, 2)])
    rot = din(nc, "rot", [128, 128])
    PT = dout(nc, "PT", [NIC, 128, T])
    outr = R()
    cr = R()
    g1t = sb(nc, "g1t", [128, NCH]); sclt = sb(nc, "sclt", [128, 2, NCH]); sftt = sb(nc, "sftt", [128, 2, NCH])
    gqt = sb(nc, "gqt", [128, 2]); cst = sb(nc, "cst", [128, 2, max(TL, 2)]); rott = sb(nc, "rott", [128, 128])
    gst = sb(nc, "gst", [128, 2, NCH])
    onesD = sb(nc, "onesD", [128, 128], F32R); onesH = sb(nc, "onesH", [128, 128], F32R)
    epst = sb(nc, "epst", [128, 1])
    for a, b_ in ((g1t, g1), (sclt, scl), (sftt, sft), (gqt, gqk), (cst, cs), (rott, rot)):
        s.dma("sp", a, b_, wr=[cr])
    s.op("dve", lambda: nc.vector.memset(onesD.bitcast(F32), 1.0 / D), wr=[cr])
    s.op("dve", lambda: nc.vector.memset(onesH.bitcast(F32), 1.0 / 128), wr=[cr])
    s.op("dve", lambda: nc.vector.memset(epst, EPS), wr=[cr])
    for k in range(2):
        s.op("dve", lambda k=k: nc.vector.scalar_tensor_tensor(out=gst[:, k, :], in0=sclt[:, k, :], scalar=1.0,
                                                               in1=g1t, op0=ALU.add, op1=ALU.mult), rd=[cr], wr=[cr])
    xr = Ring(nc, "x", 2, [128, NCH, 512])
    hr = Ring(nc, "h", 2, [128, NCH, 512], F32R)
    sqr = Ring(nc, "sq", 2, [128, 512], F32R)
    wr_ = Ring(nc, "w", 4, [128, NCH, 128], F32R)
    psA = Ring(nc, "pA", 3, [128, 512], F32, psum=True)
    psB = Ring(nc, "pB", 2, [128, 512], F32, psum=True)
    psC = Ring(nc, "pC", 2, [128, 512], F32, psum=True)
    rsr = Ring(nc, "rs", 2, [128, 512])
    qnr = Ring(nc, "qn", 2, [128, 512])
    t1r = Ring(nc, "t1", 2, [128, 512])
    str_ = Ring(nc, "st", 4, [128, 512])
    t0 = 0
    tl0 = 0
    for (n, kind) in tiles:
        xt, xtr = xr.nxt()
        s.dma("sp", xt[:, :, :n], xT[:, :, t0:t0 + n].rearrange("c p t -> p c t"), wr=[xtr])
        pb, pbr = psB.nxt()
        for c in range(NCH):
            sq, sqr_ = sqr.nxt()
            s.op("act", lambda c=c, sq=sq: nc.scalar.activation(out=sq[:, :n], in_=xt[:, c, :n], func=AF.Square),
                 rd=[xtr], wr=[sqr_])
            mm(s, pb[:, :n], pbr, onesD, sq[:, :n], c == 0, c == NCH - 1, [sqr_, cr])
        rs, rsr_ = rsr.nxt()
        rstd(s, rs[:, :n], rsr_, pb[:, :n], pbr, epst, cr)
        ht, htr = hr.nxt()
        for c in range(NCH):
            s.op("dve", lambda c=c: nc.vector.scalar_tensor_tensor(
                out=xt[:, c, :n], in0=xt[:, c, :n], scalar=gst[:, kind, c:c + 1], in1=rs[:, :n],
                op0=ALU.mult, op1=ALU.mult), rd=[xtr, rsr_, cr], wr=[xtr])
            s.op("act", lambda c=c: nc.scalar.activation(out=ht[:, c, :n], in_=xt[:, c, :n], func=AF.Identity,
                                                         bias=sftt[:, kind, c:c + 1], scale=1.0),
                 rd=[xtr, cr], wr=[htr])
        for j in range(NIC):
            wt, wtr = wr_.nxt()
            s.dma("sp", wt, w[j], wr=[wtr])
            pa, par = psA.nxt()
            for c in range(NCH):
                mm(s, pa[:, :n], par, wt[:, c, :], ht[:, c, :n], c == 0, c == NCH - 1, [wtr, htr])
            st, sr = str_.nxt()
            if j < 6:
                gi = 0 if j < 4 else 1
                sq, sqr_ = sqr.nxt()
                s.op("act", lambda sq=sq: nc.scalar.activation(out=sq[:, :n], in_=pa[:, :n], func=AF.Square),
                     rd=[par], wr=[sqr_])
                pb, pbr = psB.nxt()
                mm(s, pb[:, :n], pbr, onesH, sq[:, :n], True, True, [sqr_, cr])
                rs2, rs2r = rsr.nxt()
                rstd(s, rs2[:, :n], rs2r, pb[:, :n], pbr, epst, cr)
                if kind == 0:
                    qn, qnr_ = qnr.nxt()
                    s.op("dve", lambda qn=qn, rs2=rs2, pa=pa: nc.vector.scalar_tensor_tensor(
                        out=qn[:, :n], in0=pa[:, :n], scalar=gqt[:, gi:gi + 1], in1=rs2[:, :n],
                        op0=ALU.mult, op1=ALU.mult), rd=[par, rs2r, cr], wr=[qnr_])
                    pc, pcr = psC.nxt()
                    mm(s, pc[:, :n], pcr, rott, qn[:, :n], True, True, [qnr_, cr])
                    t1, t1r_ = t1r.nxt()
                    s.op("pool", lambda t1=t1, qn=qn: nc.gpsimd.tensor_tensor(
                        out=t1[:, :n], in0=qn[:, :n], in1=cst[:, 0, tl0:tl0 + n], op=ALU.mult),
                        rd=[qnr_, cr], wr=[t1r_])
                    s.op("dve", lambda st=st, pc=pc: nc.vector.tensor_tensor(
                        out=st[:, :n], in0=pc[:, :n], in1=cst[:, 1, tl0:tl0 + n], op=ALU.mult),
                        rd=[pcr, cr], wr=[sr])
                    s.op("dve", lambda st=st, t1=t1: nc.vector.tensor_tensor(
                        out=st[:, :n], in0=st[:, :n], in1=t1[:, :n], op=ALU.add), rd=[sr, t1r_], wr=[sr])
                else:
                    s.op("dve", lambda st=st, rs2=rs2, pa=pa: nc.vector.scalar_tensor_tensor(
                        out=st[:, :n], in0=pa[:, :n], scalar=gqt[:, gi:gi + 1], in1=rs2[:, :n],
                        op0=ALU.mult, op1=ALU.mult), rd=[par, rs2r, cr], wr=[sr])
            else:
                if j % 2 == 0:
                    s.op("act", lambda st=st, pa=pa: nc.scalar.copy(out=st[:, :n], in_=pa[:, :n]), rd=[par], wr=[sr])
                else:
                    s.op("dve", lambda st=st, pa=pa: nc.vector.tensor_copy(out=st[:, :n], in_=pa[:, :n]),
                         rd=[par], wr=[sr])
            s.dma("pool", PT[j, :, t0:t0 + n], st[:, :n], rd=[sr], wr=[outr])
        t0 += n
        if kind == 0:
            tl0 += n
    s.finish([outr])
    return nc


def fm(v):
    v = np.asarray(v)
    lead = v.shape[:-1]
    a = v.reshape(*lead, NCH, 128)
    return np.ascontiguousarray(np.moveaxis(a, -1, 0))


def tok_fm(xtok):
    return np.ascontiguousarray(xtok.T.reshape(NCH, 128, xtok.shape[0]))


def fm_tok(xT):
    return np.ascontiguousarray(xT.reshape(D, xT.shape[2]).T)


def rope_consts(pos):
    pos = np.asarray(pos, dtype=np.int32)
    rows = (pos // 64).astype(np.float32)
    cols = (pos % 64).astype(np.float32)
    nf = 32
    inv = (np.float32(1.0) / (np.float32(10000.0) ** (np.arange(nf, dtype=np.float32) / np.float32(nf)))).astype(np.float32)
    ang = np.stack([rows[:, None] * inv, cols[:, None] * inv], axis=1).astype(np.float32)
    cos = np.cos(ang).astype(np.float32)
    sin = np.sin(ang).astype(np.float32)
    cs = np.zeros((128, 2, len(pos)), np.float32)
    for a in range(2):
        for h in range(2):
            cs[a * 64 + h * 32:a * 64 + h * 32 + 32, 0, :] = cos[:, a, :].T
            cs[a * 64 + h * 32:a * 64 + h * 32 + 32, 1, :] = sin[:, a, :].T
    return cs


def rot_const():
    rm = np.zeros((128, 128), np.float32)
    for a in range(2):
        for f in range(32):
            rm[a * 64 + 32 + f, a * 64 + f] = -1.0
            rm[a * 64 + f, a * 64 + 32 + f] = 1.0
    return rm


def w_in_layout(w_in_l):
    return np.ascontiguousarray(w_in_l.reshape(NCH, 128, NIC, 128).transpose(2, 1, 0, 3))


def run(nc, ins, n=NCORES):
    res = run_bass_kernel_spmd(nc, ins, core_ids=list(range(n)))
    return res.results


NKT = (S + C) // 128
SCALE = 128.0 ** -0.5


def build_p2(ctx_out, nqg=16, lchunks=16):
    nc = new_nc()
    s = Sch(nc)
    NQ = nqg * 512
    NTOK = NQ + (C if ctx_out else 0)
    LL = lchunks * 512
    qa = din(nc, "qa", [128, NQ + C], F32R)
    ka = din(nc, "ka", [128, S + C], F32R)
    va = din(nc, "va", [128, NKT, 128], F32R)
    qn = din(nc, "qn", [128, NQ + C], F32R)
    kn = din(nc, "kn", [128, S + C], F32R)
    vn = din(nc, "vn", [128, NKT, 128], F32R)
    bias = din(nc, "bias", [3, 8, 128, 512])
    ub = din(nc, "ub", [2, 128, LL + C], F32R)
    gb = din(nc, "gb", [2, 128, LL + C], F32R)
    cw = din(nc, "cw", [128, 2, 5])
    wri = din(nc, "wri", [2, 2, 2, 128, 128], F32R)
    bri = din(nc, "bri", [128, 2, 2, 2])
    lam = din(nc, "lam", [128, 2, 2])
    oT = dout(nc, "oT", [4, 128, max(NTOK, LL + (C if ctx_out else 0))])
    outr = R()
    cr = R()
    G = [sb(nc, "G%d" % i, [128, S + C], F32R) for i in range(3)]
    Gf = [g.bitcast(F32) for g in G]
    Gr = [R() for _ in range(3)]
    ones = sb(nc, "ones", [128, 128], F32R)
    s.op("dve", lambda: nc.vector.memset(ones.bitcast(F32), 1.0), wr=[cr])
    onec = sb(nc, "onec", [128, 1])
    s.op("dve", lambda: nc.vector.memset(onec, 1.0), wr=[cr])
    psS = Ring(nc, "pS", 3, [128, 512], F32, psum=True)
    psO = Ring(nc, "pO", 2, [128, 512], F32, psum=True)
    psD = Ring(nc, "pD", 2, [128, 512], F32, psum=True)
    ptr_ = Ring(nc, "pt", 3, [128, 512], F32R)
    tmr = Ring(nc, "tm", 3, [128, 512])
    bsr = Ring(nc, "bs", 4, [128, 512])
    rcr = Ring(nc, "rc", 2, [128, 512])
    str_ = Ring(nc, "st", 3, [128, 512])

    def attend(q_ap, nq, ktiles, out_ap):
        po, por = psO.nxt()
        pd, pdr = psD.nxt()
        nk = len(ktiles)

        def emit_s(i):
            ps, psr = psS.nxt()
            mm(s, ps[:, :nq], psr, ktiles[i][0], q_ap, True, True, [Gr[0], Gr[1]])
            return ps, psr
        cur = emit_s(0)
        for i in range(nk):
            nxt_ = emit_s(i + 1) if i + 1 < nk else None
            ps, psr = cur
            pt, ptr__ = ptr_.nxt()
            bd = ktiles[i][2]
            if bd is not None:
                bt, btr = bsr.nxt()
                s.dma("sp", bt[:, :nq], bd[:, :nq], wr=[btr])
                tm, tmr_ = tmr.nxt()
                s.op("dve", lambda tm=tm, ps=ps, bt=bt: nc.vector.scalar_tensor_tensor(
                    out=tm[:, :nq], in0=ps[:, :nq], scalar=SCALE, in1=bt[:, :nq], op0=ALU.mult, op1=ALU.add),
                    rd=[psr, btr], wr=[tmr_])
                s.op("act", lambda pt=pt, tm=tm: nc.scalar.activation(out=pt[:, :nq], in_=tm[:, :nq], func=AF.Exp),
                     rd=[tmr_], wr=[ptr__])
            else:
                s.op("act", lambda pt=pt, ps=ps: nc.scalar.activation(out=pt[:, :nq], in_=ps[:, :nq], func=AF.Exp,
                                                                      scale=SCALE), rd=[psr], wr=[ptr__])
            mm(s, po[:, :nq], por, ktiles[i][1], pt[:, :nq], i == 0, i == nk - 1, [ptr__, Gr[2]])
            mm(s, pd[:, :nq], pdr, ones, pt[:, :nq], i == 0, i == nk - 1, [ptr__, cr])
            cur = nxt_
        rc, rcr_ = rcr.nxt()
        s.op("dve", lambda: nc.vector.reciprocal(out=rc[:, :nq], in_=pd[:, :nq]), rd=[pdr], wr=[rcr_])
        st, sr = str_.nxt()
        s.op("dve", lambda: nc.vector.tensor_tensor(out=st[:, :nq], in0=po[:, :nq], in1=rc[:, :nq], op=ALU.mult),
             rd=[por, rcr_], wr=[sr])
        s.dma("pool", out_ap, st[:, :nq], rd=[sr], wr=[outr])

    Gq = G[0]; Gk = G[1]; Gv = G[2].rearrange("p (t d) -> p t d", d=128)
    for (qd, kd, vd, och, is_na) in ((qa, ka, va, 0, False), (qn, kn, vn, 3, True)):
        s.dma("sp", Gq[:, :NQ + C], qd, wr=[Gr[0]])
        for h in range(2):
            s.dma("sp", Gk[:, h * 4224:(h + 1) * 4224], kd[:, h * 4224:(h + 1) * 4224], wr=[Gr[1]])
            s.dma("sp", Gv[:, h * 33:(h + 1) * 33, :], vd[:, h * 33:(h + 1) * 33, :], wr=[Gr[2]])
        for g in range(nqg):
            if not is_na:
                kts = [(Gk[:, t * 128:(t + 1) * 128], Gv[:, t, :], None) for t in range(NKT)]
            else:
                kr0 = min(max(8 * g - 4, 0), 112)
                tab = 0 if g == 0 else (2 if g == 15 else 1)
                kts = []
                for t8 in range(8):
                    t = kr0 // 2 + t8
                    kts.append((Gk[:, t * 128:(t + 1) * 128], Gv[:, t, :], bias[tab, t8]))
                for t in (64, 65):
                    kts.append((Gk[:, t * 128:(t + 1) * 128], Gv[:, t, :], None))
            attend(Gq[:, g * 512:(g + 1) * 512], 512, kts, oT[och, :, g * 512:(g + 1) * 512])
        if ctx_out:
            kts = [(Gk[:, t * 128:(t + 1) * 128], Gv[:, t, :], None) for t in (64, 65)]
            attend(Gq[:, NQ:NQ + C], C, kts, oT[och, :, NQ:NQ + C])

    cwt = sb(nc, "cwt", [128, 2, 5]); brit = sb(nc, "brit", [128, 2, 2, 2]); lamt = sb(nc, "lamt", [128, 2, 2])
    spt = sb(nc, "spt", [128, 2, 2])
    wt = sb(nc, "wt", [128, 8, 128], F32R)
    cr2 = R()
    s.dma("sp", cwt, cw, wr=[cr2]); s.dma("sp", brit, bri, wr=[cr2]); s.dma("sp", lamt, lam, wr=[cr2])
    s.dma("sp", wt, wri.rearrange("a d b k m -> k (a d b) m"), wr=[cr2])
    s.op("act", lambda: nc.scalar.activation(out=spt, in_=lamt, func=AF.Exp, scale=-1.0), rd=[cr2], wr=[cr2])
    s.op("act", lambda: nc.scalar.activation(out=spt, in_=spt, func=AF.Ln, bias=onec[:, 0:1], scale=1.0),
         rd=[cr2, cr], wr=[cr2])
    s.op("dve", lambda: nc.vector.tensor_scalar(out=spt, in0=spt, scalar1=-8.0, scalar2=None, op0=ALU.mult),
         rd=[cr2], wr=[cr2])
    U, UC, Y = Gf
    UW, UCW, YW = G
    Ur, UCr, Yr = Gr
    ar = Ring(nc, "la", 2, [128, 512]); br_ = Ring(nc, "lb", 2, [128, 512]); ir = Ring(nc, "li", 2, [128, 512])
    hr = Ring(nc, "lh", 2, [128, 512]); gr = Ring(nc, "lg", 2, [128, 512]); g2r = Ring(nc, "lg2", 2, [128, 512])
    ucr = Ring(nc, "luc", 2, [128, 512], F32R)
    hst = sb(nc, "hst", [128, 4]); hstr = R()
    segs = [("ctx", LL, C), ("lat", 0, LL)]
    for blk in range(2):
        s.dma("sp", UW[:, :LL + C], ub[blk], wr=[Ur])
        for (_, o, L) in segs:
            s.op("dve", lambda o=o, L=L: nc.vector.tensor_scalar(
                out=UCW[:, o:o + L], in0=U[:, o:o + L], scalar1=cwt[:, blk, 1:2], scalar2=cwt[:, blk, 4:5],
                op0=ALU.mult, op1=ALU.add), rd=[Ur, cr2], wr=[UCr])
            for (tap, do, so, n) in ((0, 1, 0, L - 1), (2, 0, 1, L - 1), (3, 0, 2, L - 2)):
                s.op("dve", lambda o=o, tap=tap, do=do, so=so, n=n: nc.vector.scalar_tensor_tensor(
                    out=UCW[:, o + do:o + do + n], in0=U[:, o + so:o + so + n], scalar=cwt[:, blk, tap:tap + 1],
                    in1=UC[:, o + do:o + do + n], op0=ALU.mult, op1=ALU.add), rd=[Ur, cr2, UCr], wr=[UCr])
        s.op("dve", lambda: nc.vector.memset(hst, 0.0), wr=[hstr])
        for d in range(2):
            chunks = [(LL, C)] + [(c * 512, 512) for c in (range(lchunks) if d == 0 else range(lchunks - 1, -1, -1))]
            prev = None
            for ci, (o, n) in enumerate(chunks):
                ucc, uccr = ucr.nxt()
                s.op("act", lambda ucc=ucc, o=o, n=n: nc.scalar.copy(out=ucc[:, :n], in_=UC[:, o:o + n]),
                     rd=[UCr], wr=[uccr])
                pr_, prr = psS.nxt()
                pi_, pir = psS.nxt()
                mm(s, pr_[:, :n], prr, wt[:, (0 * 2 + d) * 2 + blk, :], ucc[:, :n], True, True, [uccr, cr2])
                mm(s, pi_[:, :n], pir, wt[:, (1 * 2 + d) * 2 + blk, :], ucc[:, :n], True, True, [uccr, cr2])
                a, ar_ = ar.nxt(); b_, bbr = br_.nxt(); it, itr = ir.nxt(); h, hrr = hr.nxt()
                s.op("act", lambda a=a, pr_=pr_, n=n: nc.scalar.activation(
                    out=a[:, :n], in_=pr_[:, :n], func=AF.Sigmoid, bias=brit[:, 0, d, blk:blk + 1], scale=1.0),
                    rd=[prr, cr2], wr=[ar_])
                s.op("act", lambda it=it, pi_=pi_, n=n: nc.scalar.activation(
                    out=it[:, :n], in_=pi_[:, :n], func=AF.Sigmoid, bias=brit[:, 1, d, blk:blk + 1], scale=1.0),
                    rd=[pir, cr2], wr=[itr])
                s.op("act", lambda a=a, n=n: nc.scalar.activation(
                    out=a[:, :n], in_=a[:, :n], func=AF.Exp, scale=spt[:, d, blk:blk + 1]), rd=[ar_, cr2], wr=[ar_])
                s.op("act", lambda a=a, b_=b_, n=n: nc.scalar.activation(out=b_[:, :n], in_=a[:, :n], func=AF.Square),
                     rd=[ar_], wr=[bbr])
                s.op("act", lambda b_=b_, n=n: nc.scalar.activation(
                    out=b_[:, :n], in_=b_[:, :n], func=AF.Sqrt, bias=onec[:, 0:1], scale=-1.0), rd=[bbr, cr], wr=[bbr])
                s.op("dve", lambda it=it, o=o, n=n: nc.vector.tensor_tensor(
                    out=it[:, :n], in0=it[:, :n], in1=UC[:, o:o + n], op=ALU.mult), rd=[itr, UCr], wr=[itr])
                s.op("dve", lambda it=it, b_=b_, n=n: nc.vector.tensor_tensor(
                    out=b_[:, :n], in0=b_[:, :n], in1=it[:, :n], op=ALU.mult), rd=[itr, bbr], wr=[bbr])
                if d == 0:
                    hv, av, bv = h[:, :n], a[:, :n], b_[:, :n]
                    last = h[:, n - 1:n]
                else:
                    hv, av, bv = h[:, n - 1::-1] if n < 512 else h[:, ::-1], None, None
                    hv = h[:, 0:n][:, ::-1]; av = a[:, 0:n][:, ::-1]; bv = b_[:, 0:n][:, ::-1]
                    last = h[:, 0:1]
                init = 0.0 if ci == 0 else hst[:, d:d + 1]
                s.op("dve", lambda hv=hv, av=av, bv=bv, init=init: nc.vector.tensor_tensor_scan(
                    out=hv, data0=av, data1=bv, initial=init, op0=ALU.mult, op1=ALU.add),
                    rd=[ar_, bbr, hstr], wr=[hrr])
                s.op("dve", lambda last=last: nc.vector.tensor_copy(out=hst[:, d:d + 1], in_=last),
                     rd=[hrr], wr=[hstr])
                if d == 0:
                    s.op("pool", lambda h=h, o=o, n=n: nc.gpsimd.tensor_copy(out=YW[:, o:o + n], in_=h[:, :n]),
                         rd=[hrr], wr=[Yr])
                else:
                    s.op("pool", lambda h=h, o=o, n=n: nc.gpsimd.tensor_tensor(
                        out=YW[:, o:o + n], in0=Y[:, o:o + n], in1=h[:, :n], op=ALU.add), rd=[hrr, Yr], wr=[Yr])
        s.dma("sp", UW[:, :LL + C], gb[blk], wr=[Ur])
        ochunks = [(c * 512, 512, c * 512) for c in range(lchunks)]
        if ctx_out:
            ochunks.append((LL, C, NQ if NQ >= LL else LL))
        for (o, n, oo) in ochunks:
            g2, g2r_ = g2r.nxt(); gg, ggr = gr.nxt()
            gv = U[:, o:o + n]
            s.op("dve", lambda g2=g2, gv=gv, n=n: nc.vector.tensor_tensor(out=g2[:, :n], in0=gv, in1=gv, op=ALU.mult),
                 rd=[Ur], wr=[g2r_])
            s.op("dve", lambda g2=g2, n=n: nc.vector.tensor_scalar(
                out=g2[:, :n], in0=g2[:, :n], scalar1=0.044715, scalar2=1.0, op0=ALU.mult, op1=ALU.add),
                rd=[g2r_], wr=[g2r_])
            s.op("dve", lambda g2=g2, gv=gv, n=n: nc.vector.tensor_tensor(
                out=g2[:, :n], in0=g2[:, :n], in1=gv, op=ALU.mult), rd=[g2r_, Ur], wr=[g2r_])
            s.op("act", lambda g2=g2, gg=gg, n=n: nc.scalar.activation(
                out=gg[:, :n], in_=g2[:, :n], func=AF.Sigmoid, scale=1.5957691216057308), rd=[g2r_], wr=[ggr])
            s.op("dve", lambda gg=gg, gv=gv, n=n: nc.vector.tensor_tensor(
                out=gg[:, :n], in0=gg[:, :n], in1=gv, op=ALU.mult), rd=[ggr, Ur], wr=[ggr])
            st, sr = str_.nxt()
            s.op("dve", lambda st=st, gg=gg, o=o, n=n: nc.vector.tensor_tensor(
                out=st[:, :n], in0=gg[:, :n], in1=Y[:, o:o + n], op=ALU.mult), rd=[ggr, Yr], wr=[sr])
            s.dma("pool", oT[1 + blk, :, oo:oo + n], st[:, :n], rd=[sr], wr=[outr])
    s.finish([outr])
    return nc


def na_bias_tables(rpb_h):
    out = np.full((3, 8, 128, 512), -30000.0, np.float32)
    p = np.arange(128)
    qi = np.arange(512)
    qro = qi // 64
    j = qi % 64
    for ti, G in enumerate((0, 1, 15)):
        r = 8 * G + qro
        rs = np.clip(r - 4, 0, 120)
        cs = np.clip(j - 8, 0, 48)
        kr0 = min(max(8 * G - 4, 0), 112)
        for t8 in range(8):
            krow = (kr0 + 2 * t8 + p // 64)[:, None]
            kcol = (p % 64)[:, None]
            ok = (krow >= rs[None]) & (krow < rs[None] + 8) & (kcol >= cs[None]) & (kcol < cs[None] + 16)
            dr = np.clip(krow - r[None] + 7, 0, 14)
            dc = np.clip(kcol - j[None] + 15, 0, 30)
            vals = rpb_h[dr, dc]
            out[ti, t8] = np.where(ok, vals, np.float32(-30000.0))
    return out


def build_p3(tiles):
    nc = new_nc()
    s = Sch(nc)
    T = sum(n for n, _ in tiles)
    xT = din(nc, "xT", [NCH, 128, T])
    oT = din(nc, "oT", [NCH, 128, T], F32R)
    wo = din(nc, "wo", [NCH, 128, NCH, 128], F32R)
    gate = din(nc, "gate", [128, 2, NCH])
    g2 = din(nc, "g2", [128, NCH])
    scl = din(nc, "scl", [128, 2, NCH])
    sft = din(nc, "sft", [128, 2, NCH])
    wr = din(nc, "wr", [128, NCH, 36])
    rb = din(nc, "rb", [128, 36])
    iot = din(nc, "iot", [128, 32])
    x1T = dout(nc, "x1T", [NCH, 128, T])
    h2T = dout(nc, "h2T", [NCH, 128, T])
    rt = dout(nc, "rt", [T, 4])
    outr = R(); cr = R()
    gatet = sb(nc, "gatet", [128, 2, NCH]); g2t = sb(nc, "g2t", [128, NCH]); sclt = sb(nc, "sclt", [128, 2, NCH])
    sftt = sb(nc, "sftt", [128, 2, NCH]); wrt = sb(nc, "wrt", [128, NCH, 36]); rbt = sb(nc, "rbt", [128, 36])
    iott = sb(nc, "iott", [128, 32]); gst = sb(nc, "gst", [128, 2, NCH])
    onesD = sb(nc, "onesD", [128, 128], F32R); epst = sb(nc, "epst", [128, 1])
    for a, b_ in ((gatet, gate), (g2t, g2), (sclt, scl), (sftt, sft), (wrt, wr), (rbt, rb), (iott, iot)):
        s.dma("sp", a, b_, wr=[cr])
    s.op("dve", lambda: nc.vector.memset(onesD.bitcast(F32), 1.0 / D), wr=[cr])
    s.op("dve", lambda: nc.vector.memset(epst, EPS), wr=[cr])
    for k in range(2):
        s.op("dve", lambda k=k: nc.vector.scalar_tensor_tensor(out=gst[:, k, :], in0=sclt[:, k, :], scalar=1.0,
                                                               in1=g2t, op0=ALU.add, op1=ALU.mult), rd=[cr], wr=[cr])
    xr = Ring(nc, "x", 2, [128, NCH, 512])
    orr = Ring(nc, "o", 1, [128, NCH, 512], F32R)
    hr = Ring(nc, "h", 1, [128, NCH, 512])
    sqr = Ring(nc, "sq", 2, [128, 512], F32R)
    wr_ = Ring(nc, "w", 4, [128, NCH, 128], F32R)
    psA = Ring(nc, "pA", 3, [128, 512], F32, psum=True)
    psB = Ring(nc, "pB", 2, [128, 512], F32, psum=True)
    psR = Ring(nc, "pR", 2, [128, 64], F32, psum=True)
    rsr = Ring(nc, "rs", 2, [128, 512])
    smr = Ring(nc, "sm", 2, [128, 160])
    t0 = 0
    for (n, kind) in tiles:
        xt, xtr = xr.nxt()
        ot, otr = orr.nxt()
        s.dma("sp", xt[:, :, :n], xT[:, :, t0:t0 + n].rearrange("c p t -> p c t"), wr=[xtr])
        s.dma("sp", ot[:, :, :n], oT[:, :, t0:t0 + n].rearrange("c p t -> p c t"), wr=[otr])
        for fc in range(NCH):
            wt, wtr = wr_.nxt()
            s.dma("sp", wt, wo[fc], wr=[wtr])
            pa, par = psA.nxt()
            for c in range(NCH):
                mm(s, pa[:, :n], par, wt[:, c, :], ot[:, c, :n], c == 0, c == NCH - 1, [wtr, otr])
            s.op("dve", lambda fc=fc, pa=pa: nc.vector.scalar_tensor_tensor(
                out=xt[:, fc, :n], in0=pa[:, :n], scalar=gatet[:, kind, fc:fc + 1], in1=xt[:, fc, :n],
                op0=ALU.mult, op1=ALU.add), rd=[par, cr, xtr], wr=[xtr])
        s.dma("pool", x1T[:, :, t0:t0 + n].rearrange("c p t -> p c t"), xt[:, :, :n], rd=[xtr], wr=[outr])
        pb, pbr = psB.nxt()
        for c in range(NCH):
            sq, sqr_ = sqr.nxt()
            s.op("act", lambda c=c, sq=sq: nc.scalar.activation(out=sq[:, :n], in_=xt[:, c, :n], func=AF.Square),
                 rd=[xtr], wr=[sqr_])
            mm(s, pb[:, :n], pbr, onesD, sq[:, :n], c == 0, c == NCH - 1, [sqr_, cr])
        rs, rsr_ = rsr.nxt()
        rstd(s, rs[:, :n], rsr_, pb[:, :n], pbr, epst, cr)
        ht, htr = hr.nxt()
        for c in range(NCH):
            s.op("dve", lambda c=c: nc.vector.scalar_tensor_tensor(
                out=ht[:, c, :n], in0=xt[:, c, :n], scalar=gst[:, kind, c:c + 1], in1=rs[:, :n],
                op0=ALU.mult, op1=ALU.mult), rd=[xtr, rsr_, cr], wr=[htr])
            s.op("act", lambda c=c: nc.scalar.activation(out=ht[:, c, :n], in_=ht[:, c, :n], func=AF.Identity,
                                                         bias=sftt[:, kind, c:c + 1], scale=1.0),
                 rd=[htr, cr], wr=[htr])
        s.dma("pool", h2T[:, :, t0:t0 + n].rearrange("c p t -> p c t"), ht[:, :, :n], rd=[htr], wr=[outr])
        for sub in range((n + 127) // 128):
            nt = min(128, n - sub * 128)
            pr_, prr = psR.nxt()
            for c in range(NCH):
                mm(s, pr_[:nt, :36], prr, ht[:, c, sub * 128:sub * 128 + nt], wrt[:, c, :], c == 0, c == NCH - 1,
                   [htr, cr])
            sm, smr_ = smr.nxt()
            lg = sm[:nt, 0:36]; lem = sm[:nt, 36:68]; mk = sm[:nt, 68:100]; tmp = sm[:nt, 100:132]
            sc = sm[:nt, 132:160]

            def V(fn, rd=(), wr=()):
                s.op("dve", fn, rd=list(rd) + [smr_], wr=[smr_] + list(wr))
            V(lambda: nc.vector.tensor_tensor(out=lg, in0=pr_[:nt, :36], in1=rbt[:nt, :], op=ALU.add), rd=[prr, cr])
            V(lambda: nc.vector.tensor_reduce(out=sc[:, 0:1], in_=lg[:, 0:4], axis=AX.X, op=ALU.max))
            V(lambda: nc.vector.tensor_scalar(out=sc[:, 1:2], in0=sc[:, 0:1], scalar1=-1.0, scalar2=None, op0=ALU.mult))
            s.op("act", lambda: nc.scalar.activation(out=sc[:, 12:16], in_=lg[:, 0:4], func=AF.Exp, bias=sc[:, 1:2],
                                                     scale=1.0), rd=[smr_], wr=[smr_])
            V(lambda: nc.vector.tensor_reduce(out=sc[:, 2:3], in_=sc[:, 12:16], axis=AX.X, op=ALU.add))
            V(lambda: nc.vector.reciprocal(out=sc[:, 3:4], in_=sc[:, 2:3]))
            V(lambda: nc.vector.tensor_scalar(out=sc[:, 16:20], in0=lg[:, 0:4], scalar1=sc[:, 0:1], scalar2=None,
                                              op0=ALU.is_ge))
            V(lambda: nc.vector.tensor_scalar(out=sc[:, 16:20], in0=sc[:, 16:20], scalar1=-1.0, scalar2=1e30,
                                              op0=ALU.add, op1=ALU.mult))
            for g in range(4):
                V(lambda g=g: nc.vector.tensor_scalar(out=lem[:, g * 8:(g + 1) * 8], in0=lg[:, 4 + g * 8:12 + g * 8],
                                                      scalar1=sc[:, 16 + g:17 + g], scalar2=None, op0=ALU.add))
            for k in range(2):
                V(lambda k=k: nc.vector.tensor_reduce(out=sc[:, 4 + k:5 + k], in_=lem, axis=AX.X, op=ALU.max))
                V(lambda k=k: nc.vector.tensor_scalar(out=mk, in0=lem, scalar1=sc[:, 4 + k:5 + k], scalar2=None,
                                                      op0=ALU.is_ge))
                V(lambda: nc.vector.tensor_tensor(out=tmp, in0=mk, in1=iott[:nt, :], op=ALU.mult), rd=[cr])
                V(lambda k=k: nc.vector.tensor_reduce(out=sc[:, 6 + k:7 + k], in_=tmp, axis=AX.X, op=ALU.add))
                if k == 0:
                    V(lambda: nc.vector.scalar_tensor_tensor(out=lem, in0=mk, scalar=-1e30, in1=lem,
                                                             op0=ALU.mult, op1=ALU.add))
            V(lambda: nc.vector.tensor_tensor(out=sc[:, 10:11], in0=sc[:, 5:6], in1=sc[:, 4:5], op=ALU.subtract))
            s.op("act", lambda: nc.scalar.activation(out=sc[:, 10:11], in_=sc[:, 10:11], func=AF.Exp),
                 rd=[smr_], wr=[smr_])
            V(lambda: nc.vector.tensor_scalar(out=sc[:, 10:11], in0=sc[:, 10:11], scalar1=1.0, scalar2=None,
                                              op0=ALU.add))
            V(lambda: nc.vector.reciprocal(out=sc[:, 11:12], in_=sc[:, 10:11]))
            V(lambda: nc.vector.tensor_tensor(out=sc[:, 8:9], in0=sc[:, 11:12], in1=sc[:, 3:4], op=ALU.mult))
            V(lambda: nc.vector.tensor_tensor(out=sc[:, 9:10], in0=sc[:, 3:4], in1=sc[:, 8:9], op=ALU.subtract))
            s.dma("pool", rt[t0 + sub * 128:t0 + sub * 128 + nt, :], sc[:, 6:10], rd=[smr_], wr=[outr])
        t0 += n
    s.finish([outr])
    return nc


def build_p4(cap, nel=4):
    nc = new_nc()
    s = Sch(nc)
    XT = din(nc, "XT", [nel, NCH, 128, cap], F32R)
    wg = din(nc, "wg", [nel, NHC, 128, NCH, 128], F32R)
    wu = din(nc, "wu", [nel, NHC, 128, NCH, 128], F32R)
    wd = din(nc, "wd", [nel, NCH, 128, NHC, 128], F32R)
    YT = dout(nc, "YT", [nel, NCH, 128, cap])
    outr = R()
    xr = Ring(nc, "x", 2, [128, NCH, 512], F32R)
    wgr = Ring(nc, "wg", 3, [128, NCH, 128], F32R)
    wur = Ring(nc, "wu", 3, [128, NCH, 128], F32R)
    wdr = Ring(nc, "wd", 4, [128, NHC, 128], F32R)
    ar = Ring(nc, "a", 2, [128, NHC, 512], F32R)
    sgr = Ring(nc, "sg", 2, [128, 512])
    str_ = Ring(nc, "st", 3, [128, 512])
    psG = Ring(nc, "pG", 2, [128, 512], F32, psum=True)
    psU = Ring(nc, "pU", 2, [128, 512], F32, psum=True)
    psO = Ring(nc, "pO", 3, [128, 512], F32, psum=True)
    for e in range(nel):
        for st_ in range(cap // 512):
            sl = slice(st_ * 512, (st_ + 1) * 512)
            xt, xtr = xr.nxt()
            s.dma("sp", xt, XT[e, :, :, sl].rearrange("c p t -> p c t"), wr=[xtr])
            at, atr = ar.nxt()
            for hc in range(NHC):
                wgt, wgtr = wgr.nxt(); wut, wutr = wur.nxt()
                s.dma("sp", wgt, wg[e, hc], wr=[wgtr])
                s.dma("sp", wut, wu[e, hc], wr=[wutr])
                pg, pgr = psG.nxt(); pu, pur = psU.nxt()
                for c in range(NCH):
                    mm(s, pg, pgr, wgt[:, c, :], xt[:, c, :], c == 0, c == NCH - 1, [wgtr, xtr])
                for c in range(NCH):
                    mm(s, pu, pur, wut[:, c, :], xt[:, c, :], c == 0, c == NCH - 1, [wutr, xtr])
                sg, sgr_ = sgr.nxt()
                s.op("act", lambda sg=sg, pg=pg: nc.scalar.activation(out=sg, in_=pg, func=AF.Silu), rd=[pgr], wr=[sgr_])
                s.op("dve", lambda sg=sg, pu=pu, hc=hc, at=at: nc.vector.tensor_tensor(
                    out=at[:, hc, :], in0=sg, in1=pu, op=ALU.mult), rd=[sgr_, pur], wr=[atr])
            for fc in range(NCH):
                wdt, wdtr = wdr.nxt()
                s.dma("sp", wdt, wd[e, fc], wr=[wdtr])
                po, por = psO.nxt()
                for hc in range(NHC):
                    mm(s, po, por, wdt[:, hc, :], at[:, hc, :], hc == 0, hc == NHC - 1, [wdtr, atr])
                st, sr = str_.nxt()
                if fc % 2 == 0:
                    s.op("act", lambda st=st, po=po: nc.scalar.copy(out=st, in_=po), rd=[por], wr=[sr])
                else:
                    s.op("dve", lambda st=st, po=po: nc.vector.tensor_copy(out=st, in_=po), rd=[por], wr=[sr])
                s.dma("pool", YT[e, fc, :, sl], st, rd=[sr], wr=[outr])
    s.finish([outr])
    return nc


def build_p5(tiles, final):
    nc = new_nc()
    s = Sch(nc)
    T = sum(n for n, _ in tiles)
    x1T = din(nc, "x1T", [NCH, 128, T])
    y1T = din(nc, "y1T", [NCH, 128, T])
    y2T = din(nc, "y2T", [NCH, 128, T])
    wbc = din(nc, "wbc", [128, 2, T])
    gate = din(nc, "gate", [128, 2, NCH])
    fg = din(nc, "fg", [128, NCH])
    xoT = dout(nc, "xoT", [NCH, 128, T])
    outr = R(); cr = R()
    gatet = sb(nc, "gatet", [128, 2, NCH]); fgt = sb(nc, "fgt", [128, NCH])
    onesD = sb(nc, "onesD", [128, 128], F32R); epst = sb(nc, "epst", [128, 1])
    s.dma("sp", gatet, gate, wr=[cr]); s.dma("sp", fgt, fg, wr=[cr])
    s.op("dve", lambda: nc.vector.memset(onesD.bitcast(F32), 1.0 / D), wr=[cr])
    s.op("dve", lambda: nc.vector.memset(epst, EPS), wr=[cr])
    xr = Ring(nc, "x", 2, [128, NCH, 512])
    y1r = Ring(nc, "y1", 1, [128, NCH, 512])
    y2r = Ring(nc, "y2", 1, [128, NCH, 512])
    wr_ = Ring(nc, "w", 2, [128, 2, 512])
    sqr = Ring(nc, "sq", 2, [128, 512], F32R)
    rsr = Ring(nc, "rs", 2, [128, 512])
    psB = Ring(nc, "pB", 2, [128, 512], F32, psum=True)
    t0 = 0
    for (n, kind) in tiles:
        xt, xtr = xr.nxt(); y1, y1r_ = y1r.nxt(); y2, y2r_ = y2r.nxt(); wt, wtr = wr_.nxt()
        s.dma("sp", xt[:, :, :n], x1T[:, :, t0:t0 + n].rearrange("c p t -> p c t"), wr=[xtr])
        s.dma("sp", y1[:, :, :n], y1T[:, :, t0:t0 + n].rearrange("c p t -> p c t"), wr=[y1r_])
        s.dma("sp", y2[:, :, :n], y2T[:, :, t0:t0 + n].rearrange("c p t -> p c t"), wr=[y2r_])
        s.dma("sp", wt[:, :, :n], wbc[:, :, t0:t0 + n], wr=[wtr])
        for c in range(NCH):
            s.op("dve", lambda c=c: nc.vector.tensor_tensor(out=y1[:, c, :n], in0=y1[:, c, :n], in1=wt[:, 0, :n],
                                                            op=ALU.mult), rd=[y1r_, wtr], wr=[y1r_])
            s.op("pool", lambda c=c: nc.gpsimd.tensor_tensor(out=y2[:, c, :n], in0=y2[:, c, :n], in1=wt[:, 1, :n],
                                                             op=ALU.mult), rd=[y2r_, wtr], wr=[y2r_])
            s.op("dve", lambda c=c: nc.vector.tensor_tensor(out=y1[:, c, :n], in0=y1[:, c, :n], in1=y2[:, c, :n],
                                                            op=ALU.add), rd=[y1r_, y2r_], wr=[y1r_])
            s.op("dve", lambda c=c: nc.vector.scalar_tensor_tensor(
                out=xt[:, c, :n], in0=y1[:, c, :n], scalar=gatet[:, kind, c:c + 1], in1=xt[:, c, :n],
                op0=ALU.mult, op1=ALU.add), rd=[y1r_, cr, xtr], wr=[xtr])
        if final:
            pb, pbr = psB.nxt()
            for c in range(NCH):
                sq, sqr_ = sqr.nxt()
                s.op("act", lambda c=c, sq=sq: nc.scalar.activation(out=sq[:, :n], in_=xt[:, c, :n], func=AF.Square),
                     rd=[xtr], wr=[sqr_])
                mm(s, pb[:, :n], pbr, onesD, sq[:, :n], c == 0, c == NCH - 1, [sqr_, cr])
            rs, rsr_ = rsr.nxt()
            rstd(s, rs[:, :n], rsr_, pb[:, :n], pbr, epst, cr)
            for c in range(NCH):
                s.op("dve", lambda c=c: nc.vector.scalar_tensor_tensor(
                    out=xt[:, c, :n], in0=xt[:, c, :n], scalar=fgt[:, c:c + 1], in1=rs[:, :n],
                    op0=ALU.mult, op1=ALU.mult), rd=[xtr, rsr_, cr], wr=[xtr])
        s.dma("pool", xoT[:, :, t0:t0 + n].rearrange("c p t -> p c t"), xt[:, :, :n], rd=[xtr], wr=[outr])
        t0 += n
    s.finish([outr])
    return nc


def _vt(vT):
    nt = vT.shape[1] // 128
    return np.ascontiguousarray(vT.T.reshape(nt, 128, 128).transpose(1, 0, 2))


def _wo_layout(w):
    return np.ascontiguousarray(w.reshape(NCH, 128, NCH, 128).transpose(2, 1, 0, 3))


def _wg_layout(w):
    return w.reshape(NCH, 128, NHC, 128).transpose(2, 1, 0, 3)


def _wd_layout(w):
    return w.reshape(NHC, 128, NCH, 128).transpose(2, 1, 0, 3)


def kernel(x, c, ctx, c_ctx, ada_w, ada_b, norm1_g, norm2_g, w_in, w_out, att_q_norm, att_k_norm,
           conv_w, conv_b, lru_wr, lru_br, lru_wi, lru_bi, lru_lambda, na_rpb,
           router_wg, router_bg, router_we, router_be, moe_w_gate, moe_w_up, moe_w_down, final_g):
    f32 = np.float32
    A = lambda a: np.asarray(a, dtype=f32)
    x = A(x); c = A(c); ctx = A(ctx); c_ctx = A(c_ctx); ada_w = A(ada_w); ada_b = A(ada_b)
    norm1_g = A(norm1_g); norm2_g = A(norm2_g); w_in = A(w_in); w_out = A(w_out)
    att_q_norm = A(att_q_norm); att_k_norm = A(att_k_norm); conv_w = A(conv_w); conv_b = A(conv_b)
    lru_wr = A(lru_wr); lru_br = A(lru_br); lru_wi = A(lru_wi); lru_bi = A(lru_bi); lru_lambda = A(lru_lambda)
    na_rpb = A(na_rpb); router_wg = A(router_wg); router_bg = A(router_bg); router_we = A(router_we)
    router_be = A(router_be); moe_w_gate = A(moe_w_gate); moe_w_up = A(moe_w_up); moe_w_down = A(moe_w_down)
    final_g = A(final_g)
    TLc = S // 4
    TCc = C // 4
    tiles_lc = [(512, 0)] * 4 + [(TCc, 1)]
    tiles_l = [(512, 0)] * 4

    NCOL = 12288 // NCORES
    cc = np.stack([c[0], c[1], c_ctx, c_ctx], 0)
    cT = np.ascontiguousarray(cc.T.reshape(NCH, 128, 4).transpose(1, 0, 2))
    ins = []
    for k in range(NCORES):
        ins.append({"cT": cT,
                    "w": np.ascontiguousarray(ada_w[:, :, k * NCOL:(k + 1) * NCOL].reshape(DEPTH, NCH, 128, NCOL)),
                    "bias": np.ascontiguousarray(np.broadcast_to(ada_b[None, :, k * NCOL:(k + 1) * NCOL],
                                                                 (4, DEPTH, NCOL)))})
    res = run(build_ada(), ins)
    mod = np.concatenate([r["mod"] for r in res], axis=2)

    xs = []
    for k in range(NCORES):
        b, r = k // 4, k % 4
        xs.append(tok_fm(np.concatenate([x[b, r * TLc:(r + 1) * TLc], ctx[b, r * TCc:(r + 1) * TCc]], 0)))
    rot = rot_const()
    iot = np.ascontiguousarray(np.broadcast_to(np.arange(32, dtype=f32)[None], (128, 32)))
    out = np.zeros((B, S, D), f32)

    for l in range(DEPTH):
        last = l == DEPTH - 1
        m = mod[:, l].reshape(4, 6, D)
        wl = w_in_layout(w_in[l])
        g1 = fm(norm1_g[l])
        gqk = np.ascontiguousarray(np.stack([att_q_norm[l], att_k_norm[l]], 1))
        ins = []
        for k in range(NCORES):
            b, r = k // 4, k % 4
            ins.append({"xT": xs[k], "w": wl, "g1": g1,
                        "scl": fm(np.stack([m[b, 1], m[2, 1]])), "sft": fm(np.stack([m[b, 0], m[2, 0]])),
                        "gqk": gqk, "cs": rope_consts(np.arange(r * TLc, (r + 1) * TLc)), "rot": rot})
        res = run(build_p1(tiles_lc), ins)
        full = []
        for b in range(B):
            lat = np.concatenate([res[4 * b + r]["PT"][:, :, :TLc] for r in range(4)], axis=2)
            cx = np.concatenate([res[4 * b + r]["PT"][:, :, TLc:] for r in range(4)], axis=2)
            full.append(np.concatenate([lat, cx], axis=2))
        del res
        ins = []
        for k in range(NCORES):
            b, i = k // 4, k % 4
            F = full[b]
            cw = np.zeros((128, 2, 5), f32)
            bri = np.zeros((128, 2, 2, 2), f32)
            lam = np.zeros((128, 2, 2), f32)
            wri = np.zeros((2, 2, 2, 128, 128), f32)
            for kk in range(2):
                j = 2 * i + kk
                ch = slice(j * 128, (j + 1) * 128)
                cw[:, kk, :4] = conv_w[l][:, ch].T
                cw[:, kk, 4] = conv_b[l][ch]
                for d_ in range(2):
                    bri[:, 0, d_, kk] = lru_br[l][d_, ch]
                    bri[:, 1, d_, kk] = lru_bi[l][d_, ch]
                    lam[:, d_, kk] = lru_lambda[l][d_, ch]
                    wri[0, d_, kk] = lru_wr[l][d_, j]
                    wri[1, d_, kk] = lru_wi[l][d_, j]
            ins.append({"qa": np.ascontiguousarray(F[i]), "ka": np.ascontiguousarray(F[4 + i // 2]),
                        "va": _vt(F[6 + i // 2]),
                        "qn": np.ascontiguousarray(F[24 + i]), "kn": np.ascontiguousarray(F[28 + i]),
                        "vn": _vt(F[32 + i]), "bias": na_bias_tables(na_rpb[l, i]),
                        "ub": np.ascontiguousarray(F[8 + 2 * i:10 + 2 * i]),
                        "gb": np.ascontiguousarray(F[16 + 2 * i:18 + 2 * i]),
                        "cw": cw, "wri": wri, "bri": bri, "lam": lam})
        del full
        res = run(build_p2(not last), ins)
        ntok = S + (0 if last else C)
        OT = []
        for b in range(B):
            o = np.zeros((NCH, 128, ntok), f32)
            for i in range(4):
                oc = res[4 * b + i]["oT"]
                o[i] = oc[0][:, :ntok]
                o[4 + 2 * i] = oc[1][:, :ntok]
                o[5 + 2 * i] = oc[2][:, :ntok]
                o[12 + i] = oc[3][:, :ntok]
            OT.append(o)
        del res
        tiles = tiles_l if last else tiles_lc
        Tc = sum(n for n, _ in tiles)
        wol = _wo_layout(w_out[l])
        wcat = np.concatenate([router_wg[l], router_we[l]], 1)
        bcat = np.concatenate([router_bg[l], router_be[l]])
        wrl = np.ascontiguousarray(wcat.reshape(NCH, 128, 36).transpose(1, 0, 2))
        rbl = np.ascontiguousarray(np.broadcast_to(bcat[None], (128, 36)))
        g2 = fm(norm2_g[l])
        ins = []
        for k in range(NCORES):
            b, r = k // 4, k % 4
            if last:
                oTk = np.ascontiguousarray(OT[b][:, :, r * TLc:(r + 1) * TLc])
                xTk = np.ascontiguousarray(xs[k][:, :, :TLc])
            else:
                oTk = np.concatenate([OT[b][:, :, r * TLc:(r + 1) * TLc],
                                      OT[b][:, :, S + r * TCc:S + (r + 1) * TCc]], axis=2)
                xTk = xs[k]
            ins.append({"xT": xTk, "oT": np.ascontiguousarray(oTk), "wo": wol,
                        "gate": fm(np.stack([m[b, 2], m[2, 2]])), "g2": g2,
                        "scl": fm(np.stack([m[b, 4], m[2, 4]])), "sft": fm(np.stack([m[b, 3], m[2, 3]])),
                        "wr": wrl, "rb": rbl, "iot": iot})
        del OT
        res = run(build_p3(tiles), ins)
        x1 = [r_["x1T"] for r_ in res]
        rts = [r_["rt"] for r_ in res]
        H = np.concatenate([fm_tok(r_["h2T"]) for r_ in res], axis=0)
        del res
        rt_all = np.concatenate(rts, axis=0)
        E = np.rint(rt_all[:, :2]).astype(np.int64)
        lists = []
        for e in range(NE):
            tok, kk = np.nonzero(E == e)
            lists.append((tok, kk))
        cap = 512 * max(1, -(-max(len(t) for t, _ in lists) // 512))
        ins = []
        for k in range(NCORES):
            XT = np.zeros((4, NCH, 128, cap), f32)
            wg = np.empty((4, NHC, 128, NCH, 128), f32)
            wu = np.empty((4, NHC, 128, NCH, 128), f32)
            wd = np.empty((4, NCH, 128, NHC, 128), f32)
            for j in range(4):
                e = 4 * k + j
                tok = lists[e][0]
                XT[j][:, :, :len(tok)] = tok_fm(H[tok])
                wg[j] = _wg_layout(moe_w_gate[l, e])
                wu[j] = _wg_layout(moe_w_up[l, e])
                wd[j] = _wd_layout(moe_w_down[l, e])
            ins.append({"XT": XT, "wg": wg, "wu": wu, "wd": wd})
        del H
        res = run(build_p4(cap, 4), ins)
        del ins
        Y = np.zeros((2, NCORES * Tc, D), f32)
        for k in range(NCORES):
            for j in range(4):
                e = 4 * k + j
                tok, kk = lists[e]
                ye = fm_tok(res[k]["YT"][j])
                Y[kk, tok] = ye[:len(tok)]
        del res
        fg = fm(final_g)
        ins = []
        for k in range(NCORES):
            b = k // 4
            sl = slice(k * Tc, (k + 1) * Tc)
            ins.append({"x1T": x1[k], "y1T": tok_fm(Y[0, sl]), "y2T": tok_fm(Y[1, sl]),
                        "wbc": np.ascontiguousarray(np.broadcast_to(rts[k][:, 2:4].T[None], (128, 2, Tc))),
                        "gate": fm(np.stack([m[b, 5], m[2, 5]])), "fg": fg})
        del Y
        res = run(build_p5(tiles, last), ins)
        if last:
            for k in range(NCORES):
                b, r = k // 4, k % 4
                out[b, r * TLc:(r + 1) * TLc] = fm_tok(res[k]["xoT"])
        else:
            xs = [r_["xoT"] for r_ in res]
        del res
    return out
```

```python
import numpy as np
import concourse.bass as bass
import concourse.mybir as mybir
from concourse.bass_utils import run_bass_kernel_spmd

F32 = mybir.dt.float32
F32R = mybir.dt.float32r
AF = mybir.ActivationFunctionType
ALU = mybir.AluOpType
AX = mybir.AxisListType

D = 2048; B = 2; S = 8192; C = 256; DEPTH = 2
NCH = 16
INW = 4608; NIC = 36
HID = 1024; NHC = 8
NE = 32
EPS = 1e-6
NCORES = 8


class R:
    __slots__ = ("w", "rs")

    def __init__(s):
        s.w = None
        s.rs = []


class Sch:
    def __init__(s, nc, ndma=20):
        s.nc = nc
        s.E = {"pe": nc.tensor, "act": nc.scalar, "dve": nc.vector, "pool": nc.gpsimd, "sp": nc.sync}
        s.sem = {}
        s.cnt = {}
        for k in ("pe", "act", "dve", "pool"):
            s.sem[k] = nc.alloc_semaphore("q_" + k)
            s.cnt[k] = 0
        s.seen = {k: {} for k in s.E}
        s.dpool = {}
        for q in ("sp", "pool", "act"):
            keys = []
            for i in range(ndma if q != "act" else 4):
                key = "d_%s%d" % (q, i)
                s.sem[key] = nc.alloc_semaphore(key)
                s.cnt[key] = 0
                keys.append(key)
            s.dpool[q] = [keys, 0]
        s.nid = 0

    def _wait(s, eng, toks):
        for t in toks:
            if t is None:
                continue
            key, val = t
            if key == eng and eng == "pe":
                continue
            if key.startswith("d_"):
                val = s.cnt[key]
            if s.seen[eng].get(key, 0) < val:
                s.E[eng].wait_ge(s.sem[key], val)
                s.seen[eng][key] = val

    def _deps(s, rd, wr):
        toks = []
        for r in rd:
            toks.append(r.w)
        for r in wr:
            toks.append(r.w)
            toks.extend(r.rs)
        return toks

    def _mark(s, tk, rd, wr):
        for r in rd:
            r.rs.append(tk)
            if len(r.rs) > 64:
                r.rs = r.rs[-48:]
        for r in wr:
            r.w = tk
            r.rs = []

    def op(s, eng, fn, rd=(), wr=()):
        s._wait(eng, s._deps(rd, wr))
        ins = fn()
        s.cnt[eng] += 1
        ins.then_inc(s.sem[eng], 1)
        s._mark((eng, s.cnt[eng]), rd, wr)

    def dma(s, q, out, in_, rd=(), wr=()):
        s._wait(q, s._deps(rd, wr))
        keys, i = s.dpool[q]
        key = keys[i % len(keys)]
        s.dpool[q][1] = i + 1
        s.cnt[key] += 16
        s.E[q].dma_start(out=out, in_=in_).then_inc(s.sem[key], 16)
        s._mark((key, s.cnt[key]), rd, wr)

    def finish(s, outs):
        toks = []
        for r in outs:
            toks.append(r.w)
        s._wait("sp", toks)
        for key in s.sem:
            if key.startswith("d_") and s.cnt[key] > 0:
                if s.seen["sp"].get(key, 0) < s.cnt[key]:
                    s.E["sp"].wait_ge(s.sem[key], s.cnt[key])
                    s.seen["sp"][key] = s.cnt[key]


def new_nc():
    nc = bass.Bass("TRN2", target_bir_lowering=False)
    nc.dge_precook = False
    return nc


def sb(nc, name, shape, dt=F32):
    return nc.alloc_sbuf_tensor(name, list(shape), dt).ap()


def din(nc, name, shape, dt=F32):
    return nc.dram_tensor(name, list(shape), dt, kind="ExternalInput").ap()


def dout(nc, name, shape, dt=F32):
    return nc.dram_tensor(name, list(shape), dt, kind="ExternalOutput").ap()


class Ring:
    def __init__(s, nc, name, n, shape, dt=F32, psum=False):
        s.b = []
        for i in range(n):
            if psum:
                ap = nc.alloc_psum_tensor("%s%d" % (name, i), list(shape), dt).ap()
            else:
                ap = sb(nc, "%s%d" % (name, i), shape, dt)
            s.b.append((ap, R()))
        s.i = 0

    def nxt(s):
        x = s.b[s.i % len(s.b)]
        s.i += 1
        return x


def mm(s, ps, psr, lhsT, rhs, start, stop, rd):
    s.op("pe", lambda: s.nc.tensor.matmul(ps, lhsT, rhs, start=start, stop=stop), rd=rd, wr=[psr])


def rstd(s, out, outr, ms, msr, epst, cr):
    nc = s.nc
    s.op("act", lambda: nc.scalar.activation(out=out, in_=ms, func=AF.Sqrt, bias=epst[:, 0:1], scale=1.0),
         rd=[msr, cr], wr=[outr])
    s.op("dve", lambda: nc.vector.reciprocal(out=out, in_=out), rd=[outr], wr=[outr])

def build_ada():
    nc = new_nc()
    s = Sch(nc)
    NCOL = 12288 // NCORES
    cT = din(nc, "cT", [128, NCH, 4])
    w = din(nc, "w", [DEPTH, NCH, 128, NCOL])
    bias = din(nc, "bias", [4, DEPTH, NCOL])
    out = dout(nc, "mod", [4, DEPTH, NCOL])
    ct = sb(nc, "ct", [128, NCH, 4]); ctr = R()
    sg = sb(nc, "sg", [128, NCH, 4])
    bt = sb(nc, "bt", [4, DEPTH, NCOL]); btr = R()
    ot = sb(nc, "ot", [4, DEPTH, NCOL]); otr = R()
    wr_ = Ring(nc, "w", 3, [128, 4, NCOL], F32)
    pr = Ring(nc, "ps", 4, [128, 512], F32, psum=True)
    s.dma("sp", ct, cT, wr=[ctr])
    s.dma("sp", bt, bias, wr=[btr])
    sgr = R()
    s.op("act", lambda: nc.scalar.activation(out=sg, in_=ct, func=AF.Sigmoid), rd=[ctr], wr=[sgr])
    s.op("dve", lambda: nc.vector.tensor_tensor(out=sg, in0=sg, in1=ct, op=ALU.mult), rd=[sgr, ctr], wr=[sgr])
    for l in range(DEPTH):
        pss = [pr.nxt() for _ in range(NCOL // 512)]
        for kq in range(NCH // 4):
            wt, wtr = wr_.nxt()
            s.dma("sp", wt, w[l, kq * 4:(kq + 1) * 4].rearrange("c p n -> p c n"), wr=[wtr])
            for kk in range(4):
                k = kq * 4 + kk
                for j, (ps, psr) in enumerate(pss):
                    mm(s, ps[0:4, :], psr, sg[:, k, :], wt[:, kk, j * 512:(j + 1) * 512], k == 0, k == NCH - 1,
                       [sgr, wtr])
        for j, (ps, psr) in enumerate(pss):
            s.op("dve", lambda ps=ps, j=j: nc.vector.tensor_tensor(
                out=ot[:, l, j * 512:(j + 1) * 512], in0=ps[0:4, :], in1=bt[:, l, j * 512:(j + 1) * 512], op=ALU.add),
                rd=[psr, btr], wr=[otr])
    s.dma("sp", out, ot, rd=[otr], wr=[R()])
    s.finish([])
    return nc


def build_p1(tiles):
    nc = new_nc()
    s = Sch(nc)
    T = sum(n for n, _ in tiles)
    TL = sum(n for n, k in tiles if k == 0)
    xT = din(nc, "xT", [NCH, 128, T])
    w = din(nc, "w", [NIC, 128, NCH, 128], F32R)
    g1 = din(nc, "g1", [128, NCH])
    scl = din(nc, "scl", [128, 2, NCH])
    sft = din(nc, "sft", [128, 2, NCH])
    gqk = din(nc, "gqk", [128, 2])
    cs = din(nc, "cs", [128, 2, max(TL, 2)])
    rot = din(nc, "rot", [128, 128])
    PT = dout(nc, "PT", [NIC, 128, T])
    outr = R()
    cr = R()
    g1t = sb(nc, "g1t", [128, NCH]); sclt = sb(nc, "sclt", [128, 2, NCH]); sftt = sb(nc, "sftt", [128, 2, NCH])
    gqt = sb(nc, "gqt", [128, 2]); cst = sb(nc, "cst", [128, 2, max(TL, 2)]); rott = sb(nc, "rott", [128, 128])
    gst = sb(nc, "gst", [128, 2, NCH])
    onesD = sb(nc, "onesD", [128, 128], F32R); onesH = sb(nc, "onesH", [128, 128], F32R)
    epst = sb(nc, "epst", [128, 1])
    for a, b_ in ((g1t, g1), (sclt, scl), (sftt, sft), (gqt, gqk), (cst, cs), (rott, rot)):
        s.dma("sp", a, b_, wr=[cr])
    s.op("dve", lambda: nc.vector.memset(onesD.bitcast(F32), 1.0 / D), wr=[cr])
    s.op("dve", lambda: nc.vector.memset(onesH.bitcast(F32), 1.0 / 128), wr=[cr])
    s.op("dve", lambda: nc.vector.memset(epst, EPS), wr=[cr])
    for k in range(2):
        s.op("dve", lambda k=k: nc.vector.scalar_tensor_tensor(out=gst[:, k, :], in0=sclt[:, k, :], scalar=1.0,
                                                               in1=g1t, op0=ALU.add, op1=ALU.mult), rd=[cr], wr=[cr])
    xr = Ring(nc, "x", 2, [128, NCH, 512])
    hr = Ring(nc, "h", 2, [128, NCH, 512], F32R)
    sqr = Ring(nc, "sq", 2, [128, 512], F32R)
    wr_ = Ring(nc, "w", 4, [128, NCH, 128], F32R)
    psA = Ring(nc, "pA", 3, [128, 512], F32, psum=True)
    psB = Ring(nc, "pB", 2, [128, 512], F32, psum=True)
    psC = Ring(nc, "pC", 2, [128, 512], F32, psum=True)
    rsr = Ring(nc, "rs", 2, [128, 512])
    qnr = Ring(nc, "qn", 2, [128, 512])
    t1r = Ring(nc, "t1", 2, [128, 512])
    str_ = Ring(nc, "st", 4, [128, 512])
    t0 = 0
    tl0 = 0
    for (n, kind) in tiles:
        xt, xtr = xr.nxt()
        s.dma("sp", xt[:, :, :n], xT[:, :, t0:t0 + n].rearrange("c p t -> p c t"), wr=[xtr])
        pb, pbr = psB.nxt()
        for c in range(NCH):
            sq, sqr_ = sqr.nxt()
            s.op("act", lambda c=c, sq=sq: nc.scalar.activation(out=sq[:, :n], in_=xt[:, c, :n], func=AF.Square),
                 rd=[xtr], wr=[sqr_])
            mm(s, pb[:, :n], pbr, onesD, sq[:, :n], c == 0, c == NCH - 1, [sqr_, cr])
        rs, rsr_ = rsr.nxt()
        rstd(s, rs[:, :n], rsr_, pb[:, :n], pbr, epst, cr)
        ht, htr = hr.nxt()
        for c in range(NCH):
            s.op("dve", lambda c=c: nc.vector.scalar_tensor_tensor(
                out=xt[:, c, :n], in0=xt[:, c, :n], scalar=gst[:, kind, c:c + 1], in1=rs[:, :n],
                op0=ALU.mult, op1=ALU.mult), rd=[xtr, rsr_, cr], wr=[xtr])
            s.op("act", lambda c=c: nc.scalar.activation(out=ht[:, c, :n], in_=xt[:, c, :n], func=AF.Identity,
                                                         bias=sftt[:, kind, c:c + 1], scale=1.0),
                 rd=[xtr, cr], wr=[htr])
        for j in range(NIC):
            wt, wtr = wr_.nxt()
            s.dma("sp", wt, w[j], wr=[wtr])
            pa, par = psA.nxt()
            for c in range(NCH):
                mm(s, pa[:, :n], par, wt[:, c, :], ht[:, c, :n], c == 0, c == NCH - 1, [wtr, htr])
            st, sr = str_.nxt()
            if j < 6:
                gi = 0 if j < 4 else 1
                sq, sqr_ = sqr.nxt()
                s.op("act", lambda sq=sq: nc.scalar.activation(out=sq[:, :n], in_=pa[:, :n], func=AF.Square),
                     rd=[par], wr=[sqr_])
                pb, pbr = psB.nxt()
                mm(s, pb[:, :n], pbr, onesH, sq[:, :n], True, True, [sqr_, cr])
                rs2, rs2r = rsr.nxt()
                rstd(s, rs2[:, :n], rs2r, pb[:, :n], pbr, epst, cr)
                if kind == 0:
                    qn, qnr_ = qnr.nxt()
                    s.op("dve", lambda qn=qn, rs2=rs2, pa=pa: nc.vector.scalar_tensor_tensor(
                        out=qn[:, :n], in0=pa[:, :n], scalar=gqt[:, gi:gi + 1], in1=rs2[:, :n],
                        op0=ALU.mult, op1=ALU.mult), rd=[par, rs2r, cr], wr=[qnr_])
                    pc, pcr = psC.nxt()
                    mm(s, pc[:, :n], pcr, rott, qn[:, :n], True, True, [qnr_, cr])
                    t1, t1r_ = t1r.nxt()
                    s.op("pool", lambda t1=t1, qn=qn: nc.gpsimd.tensor_tensor(
                        out=t1[:, :n], in0=qn[:, :n], in1=cst[:, 0, tl0:tl0 + n], op=ALU.mult),
                        rd=[qnr_, cr], wr=[t1r_])
                    s.op("dve", lambda st=st, pc=pc: nc.vector.tensor_tensor(
                        out=st[:, :n], in0=pc[:, :n], in1=cst[:, 1, tl0:tl0 + n], op=ALU.mult),
                        rd=[pcr, cr], wr=[sr])
                    s.op("dve", lambda st=st, t1=t1: nc.vector.tensor_tensor(
                        out=st[:, :n], in0=st[:, :n], in1=t1[:, :n], op=ALU.add), rd=[sr, t1r_], wr=[sr])
                else:
                    s.op("dve", lambda st=st, rs2=rs2, pa=pa: nc.vector.scalar_tensor_tensor(
                        out=st[:, :n], in0=pa[:, :n], scalar=gqt[:, gi:gi + 1], in1=rs2[:, :n],
                        op0=ALU.mult, op1=ALU.mult), rd=[par, rs2r, cr], wr=[sr])
            else:
                if j % 2 == 0:
                    s.op("act", lambda st=st, pa=pa: nc.scalar.copy(out=st[:, :n], in_=pa[:, :n]), rd=[par], wr=[sr])
                else:
                    s.op("dve", lambda st=st, pa=pa: nc.vector.tensor_copy(out=st[:, :n], in_=pa[:, :n]),
                         rd=[par], wr=[sr])
            s.dma("pool", PT[j, :, t0:t0 + n], st[:, :n], rd=[sr], wr=[outr])
        t0 += n
        if kind == 0:
            tl0 += n
    s.finish([outr])
    return nc


def fm(v):
    v = np.asarray(v)
    lead = v.shape[:-1]
    a = v.reshape(*lead, NCH, 128)
    return np.ascontiguousarray(np.moveaxis(a, -1, 0))


def tok_fm(xtok):
    return np.ascontiguousarray(xtok.T.reshape(NCH, 128, xtok.shape[0]))


def fm_tok(xT):
    return np.ascontiguousarray(xT.reshape(D, xT.shape[2]).T)


def rope_consts(pos):
    pos = np.asarray(pos, dtype=np.int32)
    rows = (pos // 64).astype(np.float32)
    cols = (pos % 64).astype(np.float32)
    nf = 32
    inv = (np.float32(1.0) / (np.float32(10000.0) ** (np.arange(nf, dtype=np.float32) / np.float32(nf)))).astype(np.float32)
    ang = np.stack([rows[:, None] * inv, cols[:, None] * inv], axis=1).astype(np.float32)
    cos = np.cos(ang).astype(np.float32)
    sin = np.sin(ang).astype(np.float32)
    cs = np.zeros((128, 2, len(pos)), np.float32)
    for a in range(2):
        for h in range(2):
            cs[a * 64 + h * 32:a * 64 + h * 32 + 32, 0, :] = cos[:, a, :].T
            cs[a * 64 + h * 32:a * 64 + h * 32 + 32, 1, :] = sin[:, a, :].T
    return cs


def rot_const():
    rm = np.zeros((128, 128), np.float32)
    for a in range(2):
        for f in range(32):
            rm[a * 64 + 32 + f, a * 64 + f] = -1.0
            rm[a * 64 + f, a * 64 + 32 + f] = 1.0
    return rm


def w_in_layout(w_in_l):
    return np.ascontiguousarray(w_in_l.reshape(NCH, 128, NIC, 128).transpose(2, 1, 0, 3))


def run(nc, ins, n=NCORES):
    res = run_bass_kernel_spmd(nc, ins, core_ids=list(range(n)))
    return res.results


NKT = (S + C) // 128
SCALE = 128.0 ** -0.5


def build_p2(ctx_out, nqg=16, lchunks=16):
    nc = new_nc()
    s = Sch(nc)
    NQ = nqg * 512
    NTOK = NQ + (C if ctx_out else 0)
    LL = lchunks * 512
    qa = din(nc, "qa", [128, NQ + C], F32R)
    ka = din(nc, "ka", [128, S + C], F32R)
    va = din(nc, "va", [128, NKT, 128], F32R)
    qn = din(nc, "qn", [128, NQ + C], F32R)
    kn = din(nc, "kn", [128, S + C], F32R)
    vn = din(nc, "vn", [128, NKT, 128], F32R)
    bias = din(nc, "bias", [3, 8, 128, 512])
    ub = din(nc, "ub", [2, 128, LL + C], F32R)
    gb = din(nc, "gb", [2, 128, LL + C], F32R)
    cw = din(nc, "cw", [128, 2, 5])
    wri = din(nc, "wri", [2, 2, 2, 128, 128], F32R)
    bri = din(nc, "bri", [128, 2, 2, 2])
    lam = din(nc, "lam", [128, 2, 2])
    oT = dout(nc, "oT", [4, 128, max(NTOK, LL + (C if ctx_out else 0))])
    outr = R()
    cr = R()
    G = [sb(nc, "G%d" % i, [128, S + C], F32R) for i in range(3)]
    Gf = [g.bitcast(F32) for g in G]
    Gr = [R() for _ in range(3)]
    ones = sb(nc, "ones", [128, 128], F32R)
    s.op("dve", lambda: nc.vector.memset(ones.bitcast(F32), 1.0), wr=[cr])
    onec = sb(nc, "onec", [128, 1])
    s.op("dve", lambda: nc.vector.memset(onec, 1.0), wr=[cr])
    psS = Ring(nc, "pS", 3, [128, 512], F32, psum=True)
    psO = Ring(nc, "pO", 2, [128, 512], F32, psum=True)
    psD = Ring(nc, "pD", 2, [128, 512], F32, psum=True)
    ptr_ = Ring(nc, "pt", 3, [128, 512], F32R)
    tmr = Ring(nc, "tm", 3, [128, 512])
    bsr = Ring(nc, "bs", 4, [128, 512])
    rcr = Ring(nc, "rc", 2, [128, 512])
    str_ = Ring(nc, "st", 3, [128, 512])

    def attend(q_ap, nq, ktiles, out_ap):
        po, por = psO.nxt()
        pd, pdr = psD.nxt()
        nk = len(ktiles)

        def emit_s(i):
            ps, psr = psS.nxt()
            mm(s, ps[:, :nq], psr, ktiles[i][0], q_ap, True, True, [Gr[0], Gr[1]])
            return ps, psr
        cur = emit_s(0)
        for i in range(nk):
            nxt_ = emit_s(i + 1) if i + 1 < nk else None
            ps, psr = cur
            pt, ptr__ = ptr_.nxt()
            bd = ktiles[i][2]
            if bd is not None:
                bt, btr = bsr.nxt()
                s.dma("sp", bt[:, :nq], bd[:, :nq], wr=[btr])
                tm, tmr_ = tmr.nxt()
                s.op("dve", lambda tm=tm, ps=ps, bt=bt: nc.vector.scalar_tensor_tensor(
                    out=tm[:, :nq], in0=ps[:, :nq], scalar=SCALE, in1=bt[:, :nq], op0=ALU.mult, op1=ALU.add),
                    rd=[psr, btr], wr=[tmr_])
                s.op("act", lambda pt=pt, tm=tm: nc.scalar.activation(out=pt[:, :nq], in_=tm[:, :nq], func=AF.Exp),
                     rd=[tmr_], wr=[ptr__])
            else:
                s.op("act", lambda pt=pt, ps=ps: nc.scalar.activation(out=pt[:, :nq], in_=ps[:, :nq], func=AF.Exp,
                                                                      scale=SCALE), rd=[psr], wr=[ptr__])
            mm(s, po[:, :nq], por, ktiles[i][1], pt[:, :nq], i == 0, i == nk - 1, [ptr__, Gr[2]])
            mm(s, pd[:, :nq], pdr, ones, pt[:, :nq], i == 0, i == nk - 1, [ptr__, cr])
            cur = nxt_
        rc, rcr_ = rcr.nxt()
        s.op("dve", lambda: nc.vector.reciprocal(out=rc[:, :nq], in_=pd[:, :nq]), rd=[pdr], wr=[rcr_])
        st, sr = str_.nxt()
        s.op("dve", lambda: nc.vector.tensor_tensor(out=st[:, :nq], in0=po[:, :nq], in1=rc[:, :nq], op=ALU.mult),
             rd=[por, rcr_], wr=[sr])
        s.dma("pool", out_ap, st[:, :nq], rd=[sr], wr=[outr])

    Gq = G[0]; Gk = G[1]; Gv = G[2].rearrange("p (t d) -> p t d", d=128)
    for (qd, kd, vd, och, is_na) in ((qa, ka, va, 0, False), (qn, kn, vn, 3, True)):
        s.dma("sp", Gq[:, :NQ + C], qd, wr=[Gr[0]])
        for h in range(2):
            s.dma("sp", Gk[:, h * 4224:(h + 1) * 4224], kd[:, h * 4224:(h + 1) * 4224], wr=[Gr[1]])
            s.dma("sp", Gv[:, h * 33:(h + 1) * 33, :], vd[:, h * 33:(h + 1) * 33, :], wr=[Gr[2]])
        for g in range(nqg):
            if not is_na:
                kts = [(Gk[:, t * 128:(t + 1) * 128], Gv[:, t, :], None) for t in range(NKT)]
            else:
                kr0 = min(max(8 * g - 4, 0), 112)
                tab = 0 if g == 0 else (2 if g == 15 else 1)
                kts = []
                for t8 in range(8):
                    t = kr0 // 2 + t8
                    kts.append((Gk[:, t * 128:(t + 1) * 128], Gv[:, t, :], bias[tab, t8]))
                for t in (64, 65):
                    kts.append((Gk[:, t * 128:(t + 1) * 128], Gv[:, t, :], None))
            attend(Gq[:, g * 512:(g + 1) * 512], 512, kts, oT[och, :, g * 512:(g + 1) * 512])
        if ctx_out:
            kts = [(Gk[:, t * 128:(t + 1) * 128], Gv[:, t, :], None) for t in (64, 65)]
            attend(Gq[:, NQ:NQ + C], C, kts, oT[och, :, NQ:NQ + C])

    cwt = sb(nc, "cwt", [128, 2, 5]); brit = sb(nc, "brit", [128, 2, 2, 2]); lamt = sb(nc, "lamt", [128, 2, 2])
    spt = sb(nc, "spt", [128, 2, 2])
    wt = sb(nc, "wt", [128, 8, 128], F32R)
    cr2 = R()
    s.dma("sp", cwt, cw, wr=[cr2]); s.dma("sp", brit, bri, wr=[cr2]); s.dma("sp", lamt, lam, wr=[cr2])
    s.dma("sp", wt, wri.rearrange("a d b k m -> k (a d b) m"), wr=[cr2])
    s.op("act", lambda: nc.scalar.activation(out=spt, in_=lamt, func=AF.Exp, scale=-1.0), rd=[cr2], wr=[cr2])
    s.op("act", lambda: nc.scalar.activation(out=spt, in_=spt, func=AF.Ln, bias=onec[:, 0:1], scale=1.0),
         rd=[cr2, cr], wr=[cr2])
    s.op("dve", lambda: nc.vector.tensor_scalar(out=spt, in0=spt, scalar1=-8.0, scalar2=None, op0=ALU.mult),
         rd=[cr2], wr=[cr2])
    U, UC, Y = Gf
    UW, UCW, YW = G
    Ur, UCr, Yr = Gr
    ar = Ring(nc, "la", 2, [128, 512]); br_ = Ring(nc, "lb", 2, [128, 512]); ir = Ring(nc, "li", 2, [128, 512])
    hr = Ring(nc, "lh", 2, [128, 512]); gr = Ring(nc, "lg", 2, [128, 512]); g2r = Ring(nc, "lg2", 2, [128, 512])
    ucr = Ring(nc, "luc", 2, [128, 512], F32R)
    hst = sb(nc, "hst", [128, 4]); hstr = R()
    segs = [("ctx", LL, C), ("lat", 0, LL)]
    for blk in range(2):
        s.dma("sp", UW[:, :LL + C], ub[blk], wr=[Ur])
        for (_, o, L) in segs:
            s.op("dve", lambda o=o, L=L: nc.vector.tensor_scalar(
                out=UCW[:, o:o + L], in0=U[:, o:o + L], scalar1=cwt[:, blk, 1:2], scalar2=cwt[:, blk, 4:5],
                op0=ALU.mult, op1=ALU.add), rd=[Ur, cr2], wr=[UCr])
            for (tap, do, so, n) in ((0, 1, 0, L - 1), (2, 0, 1, L - 1), (3, 0, 2, L - 2)):
                s.op("dve", lambda o=o, tap=tap, do=do, so=so, n=n: nc.vector.scalar_tensor_tensor(
                    out=UCW[:, o + do:o + do + n], in0=U[:, o + so:o + so + n], scalar=cwt[:, blk, tap:tap + 1],
                    in1=UC[:, o + do:o + do + n], op0=ALU.mult, op1=ALU.add), rd=[Ur, cr2, UCr], wr=[UCr])
        s.op("dve", lambda: nc.vector.memset(hst, 0.0), wr=[hstr])
        for d in range(2):
            chunks = [(LL, C)] + [(c * 512, 512) for c in (range(lchunks) if d == 0 else range(lchunks - 1, -1, -1))]
            prev = None
            for ci, (o, n) in enumerate(chunks):
                ucc, uccr = ucr.nxt()
                s.op("act", lambda ucc=ucc, o=o, n=n: nc.scalar.copy(out=ucc[:, :n], in_=UC[:, o:o + n]),
                     rd=[UCr], wr=[uccr])
                pr_, prr = psS.nxt()
                pi_, pir = psS.nxt()
                mm(s, pr_[:, :n], prr, wt[:, (0 * 2 + d) * 2 + blk, :], ucc[:, :n], True, True, [uccr, cr2])
                mm(s, pi_[:, :n], pir, wt[:, (1 * 2 + d) * 2 + blk, :], ucc[:, :n], True, True, [uccr, cr2])
                a, ar_ = ar.nxt(); b_, bbr = br_.nxt(); it, itr = ir.nxt(); h, hrr = hr.nxt()
                s.op("act", lambda a=a, pr_=pr_, n=n: nc.scalar.activation(
                    out=a[:, :n], in_=pr_[:, :n], func=AF.Sigmoid, bias=brit[:, 0, d, blk:blk + 1], scale=1.0),
                    rd=[prr, cr2], wr=[ar_])
                s.op("act", lambda it=it, pi_=pi_, n=n: nc.scalar.activation(
                    out=it[:, :n], in_=pi_[:, :n], func=AF.Sigmoid, bias=brit[:, 1, d, blk:blk + 1], scale=1.0),
                    rd=[pir, cr2], wr=[itr])
                s.op("act", lambda a=a, n=n: nc.scalar.activation(
                    out=a[:, :n], in_=a[:, :n], func=AF.Exp, scale=spt[:, d, blk:blk + 1]), rd=[ar_, cr2], wr=[ar_])
                s.op("act", lambda a=a, b_=b_, n=n: nc.scalar.activation(out=b_[:, :n], in_=a[:, :n], func=AF.Square),
                     rd=[ar_], wr=[bbr])
                s.op("act", lambda b_=b_, n=n: nc.scalar.activation(
                    out=b_[:, :n], in_=b_[:, :n], func=AF.Sqrt, bias=onec[:, 0:1], scale=-1.0), rd=[bbr, cr], wr=[bbr])
                s.op("dve", lambda it=it, o=o, n=n: nc.vector.tensor_tensor(
                    out=it[:, :n], in0=it[:, :n], in1=UC[:, o:o + n], op=ALU.mult), rd=[itr, UCr], wr=[itr])
                s.op("dve", lambda it=it, b_=b_, n=n: nc.vector.tensor_tensor(
                    out=b_[:, :n], in0=b_[:, :n], in1=it[:, :n], op=ALU.mult), rd=[itr, bbr], wr=[bbr])
                if d == 0:
                    hv, av, bv = h[:, :n], a[:, :n], b_[:, :n]
                    last = h[:, n - 1:n]
                else:
                    hv, av, bv = h[:, n - 1::-1] if n < 512 else h[:, ::-1], None, None
                    hv = h[:, 0:n][:, ::-1]; av = a[:, 0:n][:, ::-1]; bv = b_[:, 0:n][:, ::-1]
                    last = h[:, 0:1]
                init = 0.0 if ci == 0 else hst[:, d:d + 1]
                s.op("dve", lambda hv=hv, av=av, bv=bv, init=init: nc.vector.tensor_tensor_scan(
                    out=hv, data0=av, data1=bv, initial=init, op0=ALU.mult, op1=ALU.add),
                    rd=[ar_, bbr, hstr], wr=[hrr])
                s.op("dve", lambda last=last: nc.vector.tensor_copy(out=hst[:, d:d + 1], in_=last),
                     rd=[hrr], wr=[hstr])
                if d == 0:
                    s.op("pool", lambda h=h, o=o, n=n: nc.gpsimd.tensor_copy(out=YW[:, o:o + n], in_=h[:, :n]),
                         rd=[hrr], wr=[Yr])
                else:
                    s.op("pool", lambda h=h, o=o, n=n: nc.gpsimd.tensor_tensor(
                        out=YW[:, o:o + n], in0=Y[:, o:o + n], in1=h[:, :n], op=ALU.add), rd=[hrr, Yr], wr=[Yr])
        s.dma("sp", UW[:, :LL + C], gb[blk], wr=[Ur])
        ochunks = [(c * 512, 512, c * 512) for c in range(lchunks)]
        if ctx_out:
            ochunks.append((LL, C, NQ if NQ >= LL else LL))
        for (o, n, oo) in ochunks:
            g2, g2r_ = g2r.nxt(); gg, ggr = gr.nxt()
            gv = U[:, o:o + n]
            s.op("dve", lambda g2=g2, gv=gv, n=n: nc.vector.tensor_tensor(out=g2[:, :n], in0=gv, in1=gv, op=ALU.mult),
                 rd=[Ur], wr=[g2r_])
            s.op("dve", lambda g2=g2, n=n: nc.vector.tensor_scalar(
                out=g2[:, :n], in0=g2[:, :n], scalar1=0.044715, scalar2=1.0, op0=ALU.mult, op1=ALU.add),
                rd=[g2r_], wr=[g2r_])
            s.op("dve", lambda g2=g2, gv=gv, n=n: nc.vector.tensor_tensor(
                out=g2[:, :n], in0=g2[:, :n], in1=gv, op=ALU.mult), rd=[g2r_, Ur], wr=[g2r_])
            s.op("act", lambda g2=g2, gg=gg, n=n: nc.scalar.activation(
                out=gg[:, :n], in_=g2[:, :n], func=AF.Sigmoid, scale=1.5957691216057308), rd=[g2r_], wr=[ggr])
            s.op("dve", lambda gg=gg, gv=gv, n=n: nc.vector.tensor_tensor(
                out=gg[:, :n], in0=gg[:, :n], in1=gv, op=ALU.mult), rd=[ggr, Ur], wr=[ggr])
            st, sr = str_.nxt()
            s.op("dve", lambda st=st, gg=gg, o=o, n=n: nc.vector.tensor_tensor(
                out=st[:, :n], in0=gg[:, :n], in1=Y[:, o:o + n], op=ALU.mult), rd=[ggr, Yr], wr=[sr])
            s.dma("pool", oT[1 + blk, :, oo:oo + n], st[:, :n], rd=[sr], wr=[outr])
    s.finish([outr])
    return nc


def na_bias_tables(rpb_h):
    out = np.full((3, 8, 128, 512), -30000.0, np.float32)
    p = np.arange(128)
    qi = np.arange(512)
    qro = qi // 64
    j = qi % 64
    for ti, G in enumerate((0, 1, 15)):
        r = 8 * G + qro
        rs = np.clip(r - 4, 0, 120)
        cs = np.clip(j - 8, 0, 48)
        kr0 = min(max(8 * G - 4, 0), 112)
        for t8 in range(8):
            krow = (kr0 + 2 * t8 + p // 64)[:, None]
            kcol = (p % 64)[:, None]
            ok = (krow >= rs[None]) & (krow < rs[None] + 8) & (kcol >= cs[None]) & (kcol < cs[None] + 16)
            dr = np.clip(krow - r[None] + 7, 0, 14)
            dc = np.clip(kcol - j[None] + 15, 0, 30)
            vals = rpb_h[dr, dc]
            out[ti, t8] = np.where(ok, vals, np.float32(-30000.0))
    return out


def build_p3(tiles):
    nc = new_nc()
    s = Sch(nc)
    T = sum(n for n, _ in tiles)
    xT = din(nc, "xT", [NCH, 128, T])
    oT = din(nc, "oT", [NCH, 128, T], F32R)
    wo = din(nc, "wo", [NCH, 128, NCH, 128], F32R)
    gate = din(nc, "gate", [128, 2, NCH])
    g2 = din(nc, "g2", [128, NCH])
    scl = din(nc, "scl", [128, 2, NCH])
    sft = din(nc, "sft", [128, 2, NCH])
    wr = din(nc, "wr", [128, NCH, 36])
    rb = din(nc, "rb", [128, 36])
    iot = din(nc, "iot", [128, 32])
    x1T = dout(nc, "x1T", [NCH, 128, T])
    h2T = dout(nc, "h2T", [NCH, 128, T])
    rt = dout(nc, "rt", [T, 4])
    outr = R(); cr = R()
    gatet = sb(nc, "gatet", [128, 2, NCH]); g2t = sb(nc, "g2t", [128, NCH]); sclt = sb(nc, "sclt", [128, 2, NCH])
    sftt = sb(nc, "sftt", [128, 2, NCH]); wrt = sb(nc, "wrt", [128, NCH, 36]); rbt = sb(nc, "rbt", [128, 36])
    iott = sb(nc, "iott", [128, 32]); gst = sb(nc, "gst", [128, 2, NCH])
    onesD = sb(nc, "onesD", [128, 128], F32R); epst = sb(nc, "epst", [128, 1])
    for a, b_ in ((gatet, gate), (g2t, g2), (sclt, scl), (sftt, sft), (wrt, wr), (rbt, rb), (iott, iot)):
        s.dma("sp", a, b_, wr=[cr])
    s.op("dve", lambda: nc.vector.memset(onesD.bitcast(F32), 1.0 / D), wr=[cr])
    s.op("dve", lambda: nc.vector.memset(epst, EPS), wr=[cr])
    for k in range(2):
        s.op("dve", lambda k=k: nc.vector.scalar_tensor_tensor(out=gst[:, k, :], in0=sclt[:, k, :], scalar=1.0,
                                                               in1=g2t, op0=ALU.add, op1=ALU.mult), rd=[cr], wr=[cr])
    xr = Ring(nc, "x", 2, [128, NCH, 512])
    orr = Ring(nc, "o", 1, [128, NCH, 512], F32R)
    hr = Ring(nc, "h", 1, [128, NCH, 512])
    sqr = Ring(nc, "sq", 2, [128, 512], F32R)
    wr_ = Ring(nc, "w", 4, [128, NCH, 128], F32R)
    psA = Ring(nc, "pA", 3, [128, 512], F32, psum=True)
    psB = Ring(nc, "pB", 2, [128, 512], F32, psum=True)
    psR = Ring(nc, "pR", 2, [128, 64], F32, psum=True)
    rsr = Ring(nc, "rs", 2, [128, 512])
    smr = Ring(nc, "sm", 2, [128, 160])
    t0 = 0
    for (n, kind) in tiles:
        xt, xtr = xr.nxt()
        ot, otr = orr.nxt()
        s.dma("sp", xt[:, :, :n], xT[:, :, t0:t0 + n].rearrange("c p t -> p c t"), wr=[xtr])
        s.dma("sp", ot[:, :, :n], oT[:, :, t0:t0 + n].rearrange("c p t -> p c t"), wr=[otr])
        for fc in range(NCH):
            wt, wtr = wr_.nxt()
            s.dma("sp", wt, wo[fc], wr=[wtr])
            pa, par = psA.nxt()
            for c in range(NCH):
                mm(s, pa[:, :n], par, wt[:, c, :], ot[:, c, :n], c == 0, c == NCH - 1, [wtr, otr])
            s.op("dve", lambda fc=fc, pa=pa: nc.vector.scalar_tensor_tensor(
                out=xt[:, fc, :n], in0=pa[:, :n], scalar=gatet[:, kind, fc:fc + 1], in1=xt[:, fc, :n],
                op0=ALU.mult, op1=ALU.add), rd=[par, cr, xtr], wr=[xtr])
        s.dma("pool", x1T[:, :, t0:t0 + n].rearrange("c p t -> p c t"), xt[:, :, :n], rd=[xtr], wr=[outr])
        pb, pbr = psB.nxt()
        for c in range(NCH):
            sq, sqr_ = sqr.nxt()
            s.op("act", lambda c=c, sq=sq: nc.scalar.activation(out=sq[:, :n], in_=xt[:, c, :n], func=AF.Square),
                 rd=[xtr], wr=[sqr_])
            mm(s, pb[:, :n], pbr, onesD, sq[:, :n], c == 0, c == NCH - 1, [sqr_, cr])
        rs, rsr_ = rsr.nxt()
        rstd(s, rs[:, :n], rsr_, pb[:, :n], pbr, epst, cr)
        ht, htr = hr.nxt()
        for c in range(NCH):
            s.op("dve", lambda c=c: nc.vector.scalar_tensor_tensor(
                out=ht[:, c, :n], in0=xt[:, c, :n], scalar=gst[:, kind, c:c + 1], in1=rs[:, :n],
                op0=ALU.mult, op1=ALU.mult), rd=[xtr, rsr_, cr], wr=[htr])
            s.op("act", lambda c=c: nc.scalar.activation(out=ht[:, c, :n], in_=ht[:, c, :n], func=AF.Identity,
                                                         bias=sftt[:, kind, c:c + 1], scale=1.0),
                 rd=[htr, cr], wr=[htr])
        s.dma("pool", h2T[:, :, t0:t0 + n].rearrange("c p t -> p c t"), ht[:, :, :n], rd=[htr], wr=[outr])
        for sub in range((n + 127) // 128):
            nt = min(128, n - sub * 128)
            pr_, prr = psR.nxt()
            for c in range(NCH):
                mm(s, pr_[:nt, :36], prr, ht[:, c, sub * 128:sub * 128 + nt], wrt[:, c, :], c == 0, c == NCH - 1,
                   [htr, cr])
            sm, smr_ = smr.nxt()
            lg = sm[:nt, 0:36]; lem = sm[:nt, 36:68]; mk = sm[:nt, 68:100]; tmp = sm[:nt, 100:132]
            sc = sm[:nt, 132:160]

            def V(fn, rd=(), wr=()):
                s.op("dve", fn, rd=list(rd) + [smr_], wr=[smr_] + list(wr))
            V(lambda: nc.vector.tensor_tensor(out=lg, in0=pr_[:nt, :36], in1=rbt[:nt, :], op=ALU.add), rd=[prr, cr])
            V(lambda: nc.vector.tensor_reduce(out=sc[:, 0:1], in_=lg[:, 0:4], axis=AX.X, op=ALU.max))
            V(lambda: nc.vector.tensor_scalar(out=sc[:, 1:2], in0=sc[:, 0:1], scalar1=-1.0, scalar2=None, op0=ALU.mult))
            s.op("act", lambda: nc.scalar.activation(out=sc[:, 12:16], in_=lg[:, 0:4], func=AF.Exp, bias=sc[:, 1:2],
                                                     scale=1.0), rd=[smr_], wr=[smr_])
            V(lambda: nc.vector.tensor_reduce(out=sc[:, 2:3], in_=sc[:, 12:16], axis=AX.X, op=ALU.add))
            V(lambda: nc.vector.reciprocal(out=sc[:, 3:4], in_=sc[:, 2:3]))
            V(lambda: nc.vector.tensor_scalar(out=sc[:, 16:20], in0=lg[:, 0:4], scalar1=sc[:, 0:1], scalar2=None,
                                              op0=ALU.is_ge))
            V(lambda: nc.vector.tensor_scalar(out=sc[:, 16:20], in0=sc[:, 16:20], scalar1=-1.0, scalar2=1e30,
                                              op0=ALU.add, op1=ALU.mult))
            for g in range(4):
                V(lambda g=g: nc.vector.tensor_scalar(out=lem[:, g * 8:(g + 1) * 8], in0=lg[:, 4 + g * 8:12 + g * 8],
                                                      scalar1=sc[:, 16 + g:17 + g], scalar2=None, op0=ALU.add))
            for k in range(2):
                V(lambda k=k: nc.vector.tensor_reduce(out=sc[:, 4 + k:5 + k], in_=lem, axis=AX.X, op=ALU.max))
                V(lambda k=k: nc.vector.tensor_scalar(out=mk, in0=lem, scalar1=sc[:, 4 + k:5 + k], scalar2=None,
                                                      op0=ALU.is_ge))
                V(lambda: nc.vector.tensor_tensor(out=tmp, in0=mk, in1=iott[:nt, :], op=ALU.mult), rd=[cr])
                V(lambda k=k: nc.vector.tensor_reduce(out=sc[:, 6 + k:7 + k], in_=tmp, axis=AX.X, op=ALU.add))
                if k == 0:
                    V(lambda: nc.vector.scalar_tensor_tensor(out=lem, in0=mk, scalar=-1e30, in1=lem,
                                                             op0=ALU.mult, op1=ALU.add))
            V(lambda: nc.vector.tensor_tensor(out=sc[:, 10:11], in0=sc[:, 5:6], in1=sc[:, 4:5], op=ALU.subtract))
            s.op("act", lambda: nc.scalar.activation(out=sc[:, 10:11], in_=sc[:, 10:11], func=AF.Exp),
                 rd=[smr_], wr=[smr_])
            V(lambda: nc.vector.tensor_scalar(out=sc[:, 10:11], in0=sc[:, 10:11], scalar1=1.0, scalar2=None,
                                              op0=ALU.add))
            V(lambda: nc.vector.reciprocal(out=sc[:, 11:12], in_=sc[:, 10:11]))
            V(lambda: nc.vector.tensor_tensor(out=sc[:, 8:9], in0=sc[:, 11:12], in1=sc[:, 3:4], op=ALU.mult))
            V(lambda: nc.vector.tensor_tensor(out=sc[:, 9:10], in0=sc[:, 3:4], in1=sc[:, 8:9], op=ALU.subtract))
            s.dma("pool", rt[t0 + sub * 128:t0 + sub * 128 + nt, :], sc[:, 6:10], rd=[smr_], wr=[outr])
        t0 += n
    s.finish([outr])
    return nc


def build_p4(cap, nel=4):
    nc = new_nc()
    s = Sch(nc)
    XT = din(nc, "XT", [nel, NCH, 128, cap], F32R)
    wg = din(nc, "wg", [nel, NHC, 128, NCH, 128], F32R)
    wu = din(nc, "wu", [nel, NHC, 128, NCH, 128], F32R)
    wd = din(nc, "wd", [nel, NCH, 128, NHC, 128], F32R)
    YT = dout(nc, "YT", [nel, NCH, 128, cap])
    outr = R()
    GS = 1024
    xr = Ring(nc, "x", 1, [128, NCH, GS], F32R)
    wgr = Ring(nc, "wg", 3, [128, NCH, 128], F32R)
    wur = Ring(nc, "wu", 3, [128, NCH, 128], F32R)
    wdr = Ring(nc, "wd", 4, [128, NHC, 128], F32R)
    ar = Ring(nc, "a", 1, [128, NHC, GS], F32R)
    sgr = Ring(nc, "sg", 2, [128, 512])
    str_ = Ring(nc, "st", 3, [128, 512])
    psG = Ring(nc, "pG", 2, [128, 512], F32, psum=True)
    psU = Ring(nc, "pU", 2, [128, 512], F32, psum=True)
    psO = Ring(nc, "pO", 3, [128, 512], F32, psum=True)
    groups = [(g0, min(GS, cap - g0)) for g0 in range(0, cap, GS)]
    for e in range(nel):
        for (g0, gn) in groups:
            nt = gn // 512
            xt, xtr = xr.nxt()
            s.dma("sp", xt[:, :, :gn], XT[e, :, :, g0:g0 + gn].rearrange("c p t -> p c t"), wr=[xtr])
            at, atr = ar.nxt()
            for hc in range(NHC):
                wgt, wgtr = wgr.nxt(); wut, wutr = wur.nxt()
                s.dma("sp", wgt, wg[e, hc], wr=[wgtr])
                s.dma("sp", wut, wu[e, hc], wr=[wutr])
                for t in range(nt):
                    ts_ = slice(t * 512, (t + 1) * 512)
                    pg, pgr = psG.nxt(); pu, pur = psU.nxt()
                    for c in range(NCH):
                        mm(s, pg, pgr, wgt[:, c, :], xt[:, c, ts_], c == 0, c == NCH - 1, [wgtr, xtr])
                    for c in range(NCH):
                        mm(s, pu, pur, wut[:, c, :], xt[:, c, ts_], c == 0, c == NCH - 1, [wutr, xtr])
                    sg, sgr_ = sgr.nxt()
                    s.op("act", lambda sg=sg, pg=pg: nc.scalar.activation(out=sg, in_=pg, func=AF.Silu),
                         rd=[pgr], wr=[sgr_])
                    s.op("dve", lambda sg=sg, pu=pu, hc=hc, at=at, ts_=ts_: nc.vector.tensor_tensor(
                        out=at[:, hc, ts_], in0=sg, in1=pu, op=ALU.mult), rd=[sgr_, pur], wr=[atr])
            for fc in range(NCH):
                wdt, wdtr = wdr.nxt()
                s.dma("sp", wdt, wd[e, fc], wr=[wdtr])
                for t in range(nt):
                    ts_ = slice(t * 512, (t + 1) * 512)
                    po, por = psO.nxt()
                    for hc in range(NHC):
                        mm(s, po, por, wdt[:, hc, :], at[:, hc, ts_], hc == 0, hc == NHC - 1, [wdtr, atr])
                    st, sr = str_.nxt()
                    if (fc + t) % 2 == 0:
                        s.op("act", lambda st=st, po=po: nc.scalar.copy(out=st, in_=po), rd=[por], wr=[sr])
                    else:
                        s.op("dve", lambda st=st, po=po: nc.vector.tensor_copy(out=st, in_=po), rd=[por], wr=[sr])
                    s.dma("pool", YT[e, fc, :, g0 + t * 512:g0 + (t + 1) * 512], st, rd=[sr], wr=[outr])
    s.finish([outr])
    return nc


def build_p5(tiles, final):
    nc = new_nc()
    s = Sch(nc)
    T = sum(n for n, _ in tiles)
    x1T = din(nc, "x1T", [NCH, 128, T])
    y1T = din(nc, "y1T", [NCH, 128, T])
    y2T = din(nc, "y2T", [NCH, 128, T])
    wbc = din(nc, "wbc", [128, 2, T])
    gate = din(nc, "gate", [128, 2, NCH])
    fg = din(nc, "fg", [128, NCH])
    xoT = dout(nc, "xoT", [NCH, 128, T])
    outr = R(); cr = R()
    gatet = sb(nc, "gatet", [128, 2, NCH]); fgt = sb(nc, "fgt", [128, NCH])
    onesD = sb(nc, "onesD", [128, 128], F32R); epst = sb(nc, "epst", [128, 1])
    s.dma("sp", gatet, gate, wr=[cr]); s.dma("sp", fgt, fg, wr=[cr])
    s.op("dve", lambda: nc.vector.memset(onesD.bitcast(F32), 1.0 / D), wr=[cr])
    s.op("dve", lambda: nc.vector.memset(epst, EPS), wr=[cr])
    xr = Ring(nc, "x", 2, [128, NCH, 512])
    y1r = Ring(nc, "y1", 1, [128, NCH, 512])
    y2r = Ring(nc, "y2", 1, [128, NCH, 512])
    wr_ = Ring(nc, "w", 2, [128, 2, 512])
    sqr = Ring(nc, "sq", 2, [128, 512], F32R)
    rsr = Ring(nc, "rs", 2, [128, 512])
    psB = Ring(nc, "pB", 2, [128, 512], F32, psum=True)
    t0 = 0
    for (n, kind) in tiles:
        xt, xtr = xr.nxt(); y1, y1r_ = y1r.nxt(); y2, y2r_ = y2r.nxt(); wt, wtr = wr_.nxt()
        s.dma("sp", xt[:, :, :n], x1T[:, :, t0:t0 + n].rearrange("c p t -> p c t"), wr=[xtr])
        s.dma("sp", y1[:, :, :n], y1T[:, :, t0:t0 + n].rearrange("c p t -> p c t"), wr=[y1r_])
        s.dma("sp", y2[:, :, :n], y2T[:, :, t0:t0 + n].rearrange("c p t -> p c t"), wr=[y2r_])
        s.dma("sp", wt[:, :, :n], wbc[:, :, t0:t0 + n], wr=[wtr])
        for c in range(NCH):
            s.op("dve", lambda c=c: nc.vector.tensor_tensor(out=y1[:, c, :n], in0=y1[:, c, :n], in1=wt[:, 0, :n],
                                                            op=ALU.mult), rd=[y1r_, wtr], wr=[y1r_])
            s.op("pool", lambda c=c: nc.gpsimd.tensor_tensor(out=y2[:, c, :n], in0=y2[:, c, :n], in1=wt[:, 1, :n],
                                                             op=ALU.mult), rd=[y2r_, wtr], wr=[y2r_])
            s.op("dve", lambda c=c: nc.vector.tensor_tensor(out=y1[:, c, :n], in0=y1[:, c, :n], in1=y2[:, c, :n],
                                                            op=ALU.add), rd=[y1r_, y2r_], wr=[y1r_])
            s.op("dve", lambda c=c: nc.vector.scalar_tensor_tensor(
                out=xt[:, c, :n], in0=y1[:, c, :n], scalar=gatet[:, kind, c:c + 1], in1=xt[:, c, :n],
                op0=ALU.mult, op1=ALU.add), rd=[y1r_, cr, xtr], wr=[xtr])
        if final:
            pb, pbr = psB.nxt()
            for c in range(NCH):
                sq, sqr_ = sqr.nxt()
                s.op("act", lambda c=c, sq=sq: nc.scalar.activation(out=sq[:, :n], in_=xt[:, c, :n], func=AF.Square),
                     rd=[xtr], wr=[sqr_])
                mm(s, pb[:, :n], pbr, onesD, sq[:, :n], c == 0, c == NCH - 1, [sqr_, cr])
            rs, rsr_ = rsr.nxt()
            rstd(s, rs[:, :n], rsr_, pb[:, :n], pbr, epst, cr)
            for c in range(NCH):
                s.op("dve", lambda c=c: nc.vector.scalar_tensor_tensor(
                    out=xt[:, c, :n], in0=xt[:, c, :n], scalar=fgt[:, c:c + 1], in1=rs[:, :n],
                    op0=ALU.mult, op1=ALU.mult), rd=[xtr, rsr_, cr], wr=[xtr])
        s.dma("pool", xoT[:, :, t0:t0 + n].rearrange("c p t -> p c t"), xt[:, :, :n], rd=[xtr], wr=[outr])
        t0 += n
    s.finish([outr])
    return nc


def _vt(vT):
    nt = vT.shape[1] // 128
    return np.ascontiguousarray(vT.T.reshape(nt, 128, 128).transpose(1, 0, 2))


def _wo_layout(w):
    return np.ascontiguousarray(w.reshape(NCH, 128, NCH, 128).transpose(2, 1, 0, 3))


def _wg_layout(w):
    return w.reshape(NCH, 128, NHC, 128).transpose(2, 1, 0, 3)


def _wd_layout(w):
    return w.reshape(NHC, 128, NCH, 128).transpose(2, 1, 0, 3)


def kernel(x, c, ctx, c_ctx, ada_w, ada_b, norm1_g, norm2_g, w_in, w_out, att_q_norm, att_k_norm,
           conv_w, conv_b, lru_wr, lru_br, lru_wi, lru_bi, lru_lambda, na_rpb,
           router_wg, router_bg, router_we, router_be, moe_w_gate, moe_w_up, moe_w_down, final_g):
    f32 = np.float32
    A = lambda a: np.asarray(a, dtype=f32)
    x = A(x); c = A(c); ctx = A(ctx); c_ctx = A(c_ctx); ada_w = A(ada_w); ada_b = A(ada_b)
    norm1_g = A(norm1_g); norm2_g = A(norm2_g); w_in = A(w_in); w_out = A(w_out)
    att_q_norm = A(att_q_norm); att_k_norm = A(att_k_norm); conv_w = A(conv_w); conv_b = A(conv_b)
    lru_wr = A(lru_wr); lru_br = A(lru_br); lru_wi = A(lru_wi); lru_bi = A(lru_bi); lru_lambda = A(lru_lambda)
    na_rpb = A(na_rpb); router_wg = A(router_wg); router_bg = A(router_bg); router_we = A(router_we)
    router_be = A(router_be); moe_w_gate = A(moe_w_gate); moe_w_up = A(moe_w_up); moe_w_down = A(moe_w_down)
    final_g = A(final_g)
    TLc = S // 4
    TCc = C // 4
    tiles_lc = [(512, 0)] * 4 + [(TCc, 1)]
    tiles_l = [(512, 0)] * 4

    NCOL = 12288 // NCORES
    cc = np.stack([c[0], c[1], c_ctx, c_ctx], 0)
    cT = np.ascontiguousarray(cc.T.reshape(NCH, 128, 4).transpose(1, 0, 2))
    ins = []
    for k in range(NCORES):
        ins.append({"cT": cT,
                    "w": np.ascontiguousarray(ada_w[:, :, k * NCOL:(k + 1) * NCOL].reshape(DEPTH, NCH, 128, NCOL)),
                    "bias": np.ascontiguousarray(np.broadcast_to(ada_b[None, :, k * NCOL:(k + 1) * NCOL],
                                                                 (4, DEPTH, NCOL)))})
    res = run(build_ada(), ins)
    mod = np.concatenate([r["mod"] for r in res], axis=2)

    xs = []
    for k in range(NCORES):
        b, r = k // 4, k % 4
        xs.append(tok_fm(np.concatenate([x[b, r * TLc:(r + 1) * TLc], ctx[b, r * TCc:(r + 1) * TCc]], 0)))
    rot = rot_const()
    iot = np.ascontiguousarray(np.broadcast_to(np.arange(32, dtype=f32)[None], (128, 32)))
    out = np.zeros((B, S, D), f32)

    for l in range(DEPTH):
        last = l == DEPTH - 1
        m = mod[:, l].reshape(4, 6, D)
        wl = w_in_layout(w_in[l])
        g1 = fm(norm1_g[l])
        gqk = np.ascontiguousarray(np.stack([att_q_norm[l], att_k_norm[l]], 1))
        ins = []
        for k in range(NCORES):
            b, r = k // 4, k % 4
            ins.append({"xT": xs[k], "w": wl, "g1": g1,
                        "scl": fm(np.stack([m[b, 1], m[2, 1]])), "sft": fm(np.stack([m[b, 0], m[2, 0]])),
                        "gqk": gqk, "cs": rope_consts(np.arange(r * TLc, (r + 1) * TLc)), "rot": rot})
        res = run(build_p1(tiles_lc), ins)
        full = []
        for b in range(B):
            lat = np.concatenate([res[4 * b + r]["PT"][:, :, :TLc] for r in range(4)], axis=2)
            cx = np.concatenate([res[4 * b + r]["PT"][:, :, TLc:] for r in range(4)], axis=2)
            full.append(np.concatenate([lat, cx], axis=2))
        del res
        ins = []
        for k in range(NCORES):
            b, i = k // 4, k % 4
            F = full[b]
            cw = np.zeros((128, 2, 5), f32)
            bri = np.zeros((128, 2, 2, 2), f32)
            lam = np.zeros((128, 2, 2), f32)
            wri = np.zeros((2, 2, 2, 128, 128), f32)
            for kk in range(2):
                j = 2 * i + kk
                ch = slice(j * 128, (j + 1) * 128)
                cw[:, kk, :4] = conv_w[l][:, ch].T
                cw[:, kk, 4] = conv_b[l][ch]
                for d_ in range(2):
                    bri[:, 0, d_, kk] = lru_br[l][d_, ch]
                    bri[:, 1, d_, kk] = lru_bi[l][d_, ch]
                    lam[:, d_, kk] = lru_lambda[l][d_, ch]
                    wri[0, d_, kk] = lru_wr[l][d_, j]
                    wri[1, d_, kk] = lru_wi[l][d_, j]
            ins.append({"qa": np.ascontiguousarray(F[i]), "ka": np.ascontiguousarray(F[4 + i // 2]),
                        "va": _vt(F[6 + i // 2]),
                        "qn": np.ascontiguousarray(F[24 + i]), "kn": np.ascontiguousarray(F[28 + i]),
                        "vn": _vt(F[32 + i]), "bias": na_bias_tables(na_rpb[l, i]),
                        "ub": np.ascontiguousarray(F[8 + 2 * i:10 + 2 * i]),
                        "gb": np.ascontiguousarray(F[16 + 2 * i:18 + 2 * i]),
                        "cw": cw, "wri": wri, "bri": bri, "lam": lam})
        del full
        res = run(build_p2(not last), ins)
        ntok = S + (0 if last else C)
        OT = []
        for b in range(B):
            o = np.zeros((NCH, 128, ntok), f32)
            for i in range(4):
                oc = res[4 * b + i]["oT"]
                o[i] = oc[0][:, :ntok]
                o[4 + 2 * i] = oc[1][:, :ntok]
                o[5 + 2 * i] = oc[2][:, :ntok]
                o[12 + i] = oc[3][:, :ntok]
            OT.append(o)
        del res
        tiles = tiles_l if last else tiles_lc
        Tc = sum(n for n, _ in tiles)
        wol = _wo_layout(w_out[l])
        wcat = np.concatenate([router_wg[l], router_we[l]], 1)
        bcat = np.concatenate([router_bg[l], router_be[l]])
        wrl = np.ascontiguousarray(wcat.reshape(NCH, 128, 36).transpose(1, 0, 2))
        rbl = np.ascontiguousarray(np.broadcast_to(bcat[None], (128, 36)))
        g2 = fm(norm2_g[l])
        ins = []
        for k in range(NCORES):
            b, r = k // 4, k % 4
            if last:
                oTk = np.ascontiguousarray(OT[b][:, :, r * TLc:(r + 1) * TLc])
                xTk = np.ascontiguousarray(xs[k][:, :, :TLc])
            else:
                oTk = np.concatenate([OT[b][:, :, r * TLc:(r + 1) * TLc],
                                      OT[b][:, :, S + r * TCc:S + (r + 1) * TCc]], axis=2)
                xTk = xs[k]
            ins.append({"xT": xTk, "oT": np.ascontiguousarray(oTk), "wo": wol,
                        "gate": fm(np.stack([m[b, 2], m[2, 2]])), "g2": g2,
                        "scl": fm(np.stack([m[b, 4], m[2, 4]])), "sft": fm(np.stack([m[b, 3], m[2, 3]])),
                        "wr": wrl, "rb": rbl, "iot": iot})
        del OT
        res = run(build_p3(tiles), ins)
        x1 = [r_["x1T"] for r_ in res]
        rts = [r_["rt"] for r_ in res]
        H = np.concatenate([fm_tok(r_["h2T"]) for r_ in res], axis=0)
        del res
        rt_all = np.concatenate(rts, axis=0)
        E = np.rint(rt_all[:, :2]).astype(np.int64)
        lists = []
        for e in range(NE):
            tok, kk = np.nonzero(E == e)
            lists.append((tok, kk))
        cap = 512 * max(1, -(-max(len(t) for t, _ in lists) // 512))
        ins = []
        for k in range(NCORES):
            XT = np.zeros((4, NCH, 128, cap), f32)
            wg = np.empty((4, NHC, 128, NCH, 128), f32)
            wu = np.empty((4, NHC, 128, NCH, 128), f32)
            wd = np.empty((4, NCH, 128, NHC, 128), f32)
            for j in range(4):
                e = 4 * k + j
                tok = lists[e][0]
                XT[j][:, :, :len(tok)] = tok_fm(H[tok])
                wg[j] = _wg_layout(moe_w_gate[l, e])
                wu[j] = _wg_layout(moe_w_up[l, e])
                wd[j] = _wd_layout(moe_w_down[l, e])
            ins.append({"XT": XT, "wg": wg, "wu": wu, "wd": wd})
        del H
        res = run(build_p4(cap, 4), ins)
        del ins
        Y = np.zeros((2, NCORES * Tc, D), f32)
        for k in range(NCORES):
            for j in range(4):
                e = 4 * k + j
                tok, kk = lists[e]
                ye = fm_tok(res[k]["YT"][j])
                Y[kk, tok] = ye[:len(tok)]
        del res
        fg = fm(final_g)
        ins = []
        for k in range(NCORES):
            b = k // 4
            sl = slice(k * Tc, (k + 1) * Tc)
            ins.append({"x1T": x1[k], "y1T": tok_fm(Y[0, sl]), "y2T": tok_fm(Y[1, sl]),
                        "wbc": np.ascontiguousarray(np.broadcast_to(rts[k][:, 2:4].T[None], (128, 2, Tc))),
                        "gate": fm(np.stack([m[b, 5], m[2, 5]])), "fg": fg})
        del Y
        res = run(build_p5(tiles, last), ins)
        if last:
            for k in range(NCORES):
                b, r = k // 4, k % 4
                out[b, r * TLc:(r + 1) * TLc] = fm_tok(res[k]["xoT"])
        else:
            xs = [r_["xoT"] for r_ in res]
        del res
    return out
```

```python
import numpy as np
import concourse.bass as bass
import concourse.mybir as mybir
from concourse.bass_utils import run_bass_kernel_spmd

F32 = mybir.dt.float32
F32R = mybir.dt.float32r
AF = mybir.ActivationFunctionType
ALU = mybir.AluOpType
AX = mybir.AxisListType

D = 2048; B = 2; S = 8192; C = 256; DEPTH = 2
NCH = 16
INW = 4608; NIC = 36
HID = 1024; NHC = 8
NE = 32
EPS = 1e-6
NCORES = 8


class R:
    __slots__ = ("w", "rs")

    def __init__(s):
        s.w = None
        s.rs = []


class Sch:
    def __init__(s, nc, ndma=20):
        s.nc = nc
        s.E = {"pe": nc.tensor, "act": nc.scalar, "dve": nc.vector, "pool": nc.gpsimd, "sp": nc.sync}
        s.sem = {}
        s.cnt = {}
        for k in ("pe", "act", "dve", "pool"):
            s.sem[k] = nc.alloc_semaphore("q_" + k)
            s.cnt[k] = 0
        s.seen = {k: {} for k in s.E}
        s.dpool = {}
        for q in ("sp", "pool", "act"):
            keys = []
            for i in range(ndma if q != "act" else 4):
                key = "d_%s%d" % (q, i)
                s.sem[key] = nc.alloc_semaphore(key)
                s.cnt[key] = 0
                keys.append(key)
            s.dpool[q] = [keys, 0]
        s.nid = 0

    def _wait(s, eng, toks):
        for t in toks:
            if t is None:
                continue
            key, val = t
            if key == eng and eng == "pe":
                continue
            if key.startswith("d_"):
                val = s.cnt[key]
            if s.seen[eng].get(key, 0) < val:
                s.E[eng].wait_ge(s.sem[key], val)
                s.seen[eng][key] = val

    def _deps(s, rd, wr):
        toks = []
        for r in rd:
            toks.append(r.w)
        for r in wr:
            toks.append(r.w)
            toks.extend(r.rs)
        return toks

    def _mark(s, tk, rd, wr):
        for r in rd:
            r.rs.append(tk)
            if len(r.rs) > 64:
                r.rs = r.rs[-48:]
        for r in wr:
            r.w = tk
            r.rs = []

    def op(s, eng, fn, rd=(), wr=()):
        s._wait(eng, s._deps(rd, wr))
        ins = fn()
        s.cnt[eng] += 1
        ins.then_inc(s.sem[eng], 1)
        s._mark((eng, s.cnt[eng]), rd, wr)

    def dma(s, q, out, in_, rd=(), wr=()):
        s._wait(q, s._deps(rd, wr))
        keys, i = s.dpool[q]
        key = keys[i % len(keys)]
        s.dpool[q][1] = i + 1
        s.cnt[key] += 16
        s.E[q].dma_start(out=out, in_=in_).then_inc(s.sem[key], 16)
        s._mark((key, s.cnt[key]), rd, wr)

    def finish(s, outs):
        toks = []
        for r in outs:
            toks.append(r.w)
        s._wait("sp", toks)
        for key in s.sem:
            if key.startswith("d_") and s.cnt[key] > 0:
                if s.seen["sp"].get(key, 0) < s.cnt[key]:
                    s.E["sp"].wait_ge(s.sem[key], s.cnt[key])
                    s.seen["sp"][key] = s.cnt[key]


def new_nc():
    nc = bass.Bass("TRN2", target_bir_lowering=False)
    nc.dge_precook = False
    return nc


def sb(nc, name, shape, dt=F32):
    return nc.alloc_sbuf_tensor(name, list(shape), dt).ap()


def din(nc, name, shape, dt=F32):
    return nc.dram_tensor(name, list(shape), dt, kind="ExternalInput").ap()


def dout(nc, name, shape, dt=F32):
    return nc.dram_tensor(name, list(shape), dt, kind="ExternalOutput").ap()


class Ring:
    def __init__(s, nc, name, n, shape, dt=F32, psum=False):
        s.b = []
        for i in range(n):
            if psum:
                ap = nc.alloc_psum_tensor("%s%d" % (name, i), list(shape), dt).ap()
            else:
                ap = sb(nc, "%s%d" % (name, i), shape, dt)
            s.b.append((ap, R()))
        s.i = 0

    def nxt(s):
        x = s.b[s.i % len(s.b)]
        s.i += 1
        return x


def mm(s, ps, psr, lhsT, rhs, start, stop, rd):
    s.op("pe", lambda: s.nc.tensor.matmul(ps, lhsT, rhs, start=start, stop=stop), rd=rd, wr=[psr])


def rstd(s, out, outr, ms, msr, epst, cr):
    nc = s.nc
    s.op("act", lambda: nc.scalar.activation(out=out, in_=ms, func=AF.Sqrt, bias=epst[:, 0:1], scale=1.0),
         rd=[msr, cr], wr=[outr])
    s.op("dve", lambda: nc.vector.reciprocal(out=out, in_=out), rd=[outr], wr=[outr])

def build_ada():
    nc = new_nc()
    s = Sch(nc)
    NCOL = 12288 // NCORES
    cT = din(nc, "cT", [128, NCH, 4])
    w = din(nc, "w", [DEPTH, NCH, 128, NCOL])
    bias = din(nc, "bias", [4, DEPTH, NCOL])
    out = dout(nc, "mod", [4, DEPTH, NCOL])
    ct = sb(nc, "ct", [128, NCH, 4]); ctr = R()
    sg = sb(nc, "sg", [128, NCH, 4])
    bt = sb(nc, "bt", [4, DEPTH, NCOL]); btr = R()
    ot = sb(nc, "ot", [4, DEPTH, NCOL]); otr = R()
    wr_ = Ring(nc, "w", 3, [128, 4, NCOL], F32)
    pr = Ring(nc, "ps", 4, [128, 512], F32, psum=True)
    s.dma("sp", ct, cT, wr=[ctr])
    s.dma("sp", bt, bias, wr=[btr])
    sgr = R()
    s.op("act", lambda: nc.scalar.activation(out=sg, in_=ct, func=AF.Sigmoid), rd=[ctr], wr=[sgr])
    s.op("dve", lambda: nc.vector.tensor_tensor(out=sg, in0=sg, in1=ct, op=ALU.mult), rd=[sgr, ctr], wr=[sgr])
    for l in range(DEPTH):
        pss = [pr.nxt() for _ in range(NCOL // 512)]
        for kq in range(NCH // 4):
            wt, wtr = wr_.nxt()
            s.dma("sp", wt, w[l, kq * 4:(kq + 1) * 4].rearrange("c p n -> p c n"), wr=[wtr])
            for kk in range(4):
                k = kq * 4 + kk
                for j, (ps, psr) in enumerate(pss):
                    mm(s, ps[0:4, :], psr, sg[:, k, :], wt[:, kk, j * 512:(j + 1) * 512], k == 0, k == NCH - 1,
                       [sgr, wtr])
        for j, (ps, psr) in enumerate(pss):
            s.op("dve", lambda ps=ps, j=j: nc.vector.tensor_tensor(
                out=ot[:, l, j * 512:(j + 1) * 512], in0=ps[0:4, :], in1=bt[:, l, j * 512:(j + 1) * 512], op=ALU.add),
                rd=[psr, btr], wr=[otr])
    s.dma("sp", out, ot, rd=[otr], wr=[R()])
    s.finish([])
    return nc


def build_p1(tiles):
    nc = new_nc()
    s = Sch(nc)
    T = sum(n for n, _ in tiles)
    TL = sum(n for n, k in tiles if k == 0)
    xT = din(nc, "xT", [NCH, 128, T])
    w = din(nc, "w", [NIC, 128, NCH, 128], F32R)
    g1 = din(nc, "g1", [128, NCH])
    scl = din(nc, "scl", [128, 2, NCH])
    sft = din(nc, "sft", [128, 2, NCH])
    gqk = din(nc, "gqk", [128, 2])
    cs = din(nc, "cs", [128, 2, max(TL, 2)])
    rot = din(nc, "rot", [128, 128])
    PT = dout(nc, "PT", [NIC, 128, T])
    outr = R()
    cr = R()
    g1t = sb(nc, "g1t", [128, NCH]); sclt = sb(nc, "sclt", [128, 2, NCH]); sftt = sb(nc, "sftt", [128, 2, NCH])
    gqt = sb(nc, "gqt", [128, 2]); cst = sb(nc, "cst", [128, 2, max(TL, 2)]); rott = sb(nc, "rott", [128, 128])
    gst = sb(nc, "gst", [128, 2, NCH])
    onesD = sb(nc, "onesD", [128, 128], F32R); onesH = sb(nc, "onesH", [128, 128], F32R)
    epst = sb(nc, "epst", [128, 1])
    for a, b_ in ((g1t, g1), (sclt, scl), (sftt, sft), (gqt, gqk), (cst, cs), (rott, rot)):
        s.dma("sp", a, b_, wr=[cr])
    s.op("dve", lambda: nc.vector.memset(onesD.bitcast(F32), 1.0 / D), wr=[cr])
    s.op("dve", lambda: nc.vector.memset(onesH.bitcast(F32), 1.0 / 128), wr=[cr])
    s.op("dve", lambda: nc.vector.memset(epst, EPS), wr=[cr])
    for k in range(2):
        s.op("dve", lambda k=k: nc.vector.scalar_tensor_tensor(out=gst[:, k, :], in0=sclt[:, k, :], scalar=1.0,
                                                               in1=g1t, op0=ALU.add, op1=ALU.mult), rd=[cr], wr=[cr])
    groups = []
    cur, tot = [], 0
    for tl in tiles:
        if cur and tot + tl[0] > 1088:
            groups.append(cur); cur, tot = [], 0
        cur.append(tl); tot += tl[0]
    if cur:
        groups.append(cur)
    GMAX = max(sum(n for n, _ in g) for g in groups)
    xr = Ring(nc, "x", 1, [128, NCH, 512])
    hg = sb(nc, "hg", [128, NCH, GMAX], F32R); hgr = R()
    sqr = Ring(nc, "sq", 2, [128, 512], F32R)
    wr_ = Ring(nc, "w", 4, [128, NCH, 128], F32R)
    psA = Ring(nc, "pA", 3, [128, 512], F32, psum=True)
    psB = Ring(nc, "pB", 2, [128, 512], F32, psum=True)
    psC = Ring(nc, "pC", 2, [128, 512], F32, psum=True)
    rsr = Ring(nc, "rs", 2, [128, 512])
    qnr = Ring(nc, "qn", 2, [128, 512])
    t1r = Ring(nc, "t1", 2, [128, 512])
    str_ = Ring(nc, "st", 4, [128, 512])
    t0 = 0
    tl0 = 0
    for grp in groups:
        info = []
        off = 0
        for (n, kind) in grp:
            xt, xtr = xr.nxt()
            s.dma("sp", xt[:, :, :n], xT[:, :, t0 + off:t0 + off + n].rearrange("c p t -> p c t"), wr=[xtr])
            pb, pbr = psB.nxt()
            for c in range(NCH):
                sq, sqr_ = sqr.nxt()
                s.op("act", lambda c=c, sq=sq: nc.scalar.activation(out=sq[:, :n], in_=xt[:, c, :n], func=AF.Square),
                     rd=[xtr], wr=[sqr_])
                mm(s, pb[:, :n], pbr, onesD, sq[:, :n], c == 0, c == NCH - 1, [sqr_, cr])
            rs, rsr_ = rsr.nxt()
            rstd(s, rs[:, :n], rsr_, pb[:, :n], pbr, epst, cr)
            for c in range(NCH):
                s.op("dve", lambda c=c: nc.vector.scalar_tensor_tensor(
                    out=xt[:, c, :n], in0=xt[:, c, :n], scalar=gst[:, kind, c:c + 1], in1=rs[:, :n],
                    op0=ALU.mult, op1=ALU.mult), rd=[xtr, rsr_, cr], wr=[xtr])
                s.op("act", lambda c=c: nc.scalar.activation(out=hg[:, c, off:off + n], in_=xt[:, c, :n],
                                                             func=AF.Identity, bias=sftt[:, kind, c:c + 1], scale=1.0),
                     rd=[xtr, cr], wr=[hgr])
            info.append((off, n, kind, tl0))
            off += n
            if kind == 0:
                tl0 += n
        for j in range(NIC):
            wt, wtr = wr_.nxt()
            s.dma("sp", wt, w[j], wr=[wtr])
            for (o_, n, kind, tl) in info:
                pa, par = psA.nxt()
                for c in range(NCH):
                    mm(s, pa[:, :n], par, wt[:, c, :], hg[:, c, o_:o_ + n], c == 0, c == NCH - 1, [wtr, hgr])
                st, sr = str_.nxt()
                if j < 6:
                    gi = 0 if j < 4 else 1
                    sq, sqr_ = sqr.nxt()
                    s.op("act", lambda sq=sq: nc.scalar.activation(out=sq[:, :n], in_=pa[:, :n], func=AF.Square),
                         rd=[par], wr=[sqr_])
                    pb, pbr = psB.nxt()
                    mm(s, pb[:, :n], pbr, onesH, sq[:, :n], True, True, [sqr_, cr])
                    rs2, rs2r = rsr.nxt()
                    rstd(s, rs2[:, :n], rs2r, pb[:, :n], pbr, epst, cr)
                    if kind == 0:
                        qn, qnr_ = qnr.nxt()
                        s.op("dve", lambda qn=qn, rs2=rs2, pa=pa: nc.vector.scalar_tensor_tensor(
                            out=qn[:, :n], in0=pa[:, :n], scalar=gqt[:, gi:gi + 1], in1=rs2[:, :n],
                            op0=ALU.mult, op1=ALU.mult), rd=[par, rs2r, cr], wr=[qnr_])
                        pc, pcr = psC.nxt()
                        mm(s, pc[:, :n], pcr, rott, qn[:, :n], True, True, [qnr_, cr])
                        t1, t1r_ = t1r.nxt()
                        s.op("pool", lambda t1=t1, qn=qn: nc.gpsimd.tensor_tensor(
                            out=t1[:, :n], in0=qn[:, :n], in1=cst[:, 0, tl:tl + n], op=ALU.mult),
                            rd=[qnr_, cr], wr=[t1r_])
                        s.op("dve", lambda st=st, pc=pc: nc.vector.tensor_tensor(
                            out=st[:, :n], in0=pc[:, :n], in1=cst[:, 1, tl:tl + n], op=ALU.mult),
                            rd=[pcr, cr], wr=[sr])
                        s.op("dve", lambda st=st, t1=t1: nc.vector.tensor_tensor(
                            out=st[:, :n], in0=st[:, :n], in1=t1[:, :n], op=ALU.add), rd=[sr, t1r_], wr=[sr])
                    else:
                        s.op("dve", lambda st=st, rs2=rs2, pa=pa: nc.vector.scalar_tensor_tensor(
                            out=st[:, :n], in0=pa[:, :n], scalar=gqt[:, gi:gi + 1], in1=rs2[:, :n],
                            op0=ALU.mult, op1=ALU.mult), rd=[par, rs2r, cr], wr=[sr])
                else:
                    if j % 2 == 0:
                        s.op("act", lambda st=st, pa=pa: nc.scalar.copy(out=st[:, :n], in_=pa[:, :n]),
                             rd=[par], wr=[sr])
                    else:
                        s.op("dve", lambda st=st, pa=pa: nc.vector.tensor_copy(out=st[:, :n], in_=pa[:, :n]),
                             rd=[par], wr=[sr])
                s.dma("pool", PT[j, :, t0 + o_:t0 + o_ + n], st[:, :n], rd=[sr], wr=[outr])
        t0 += off
    s.finish([outr])
    return nc


def fm(v):
    v = np.asarray(v)
    lead = v.shape[:-1]
    a = v.reshape(*lead, NCH, 128)
    return np.ascontiguousarray(np.moveaxis(a, -1, 0))


def tok_fm(xtok):
    return np.ascontiguousarray(xtok.T.reshape(NCH, 128, xtok.shape[0]))


def fm_tok(xT):
    return np.ascontiguousarray(xT.reshape(D, xT.shape[2]).T)


def rope_consts(pos):
    pos = np.asarray(pos, dtype=np.int32)
    rows = (pos // 64).astype(np.float32)
    cols = (pos % 64).astype(np.float32)
    nf = 32
    inv = (np.float32(1.0) / (np.float32(10000.0) ** (np.arange(nf, dtype=np.float32) / np.float32(nf)))).astype(np.float32)
    ang = np.stack([rows[:, None] * inv, cols[:, None] * inv], axis=1).astype(np.float32)
    cos = np.cos(ang).astype(np.float32)
    sin = np.sin(ang).astype(np.float32)
    cs = np.zeros((128, 2, len(pos)), np.float32)
    for a in range(2):
        for h in range(2):
            cs[a * 64 + h * 32:a * 64 + h * 32 + 32, 0, :] = cos[:, a, :].T
            cs[a * 64 + h * 32:a * 64 + h * 32 + 32, 1, :] = sin[:, a, :].T
    return cs


def rot_const():
    rm = np.zeros((128, 128), np.float32)
    for a in range(2):
        for f in range(32):
            rm[a * 64 + 32 + f, a * 64 + f] = -1.0
            rm[a * 64 + f, a * 64 + 32 + f] = 1.0
    return rm


def w_in_layout(w_in_l):
    return np.ascontiguousarray(w_in_l.reshape(NCH, 128, NIC, 128).transpose(2, 1, 0, 3))


def run(nc, ins, n=NCORES):
    res = run_bass_kernel_spmd(nc, ins, core_ids=list(range(n)))
    return res.results


NKT = (S + C) // 128
SCALE = 128.0 ** -0.5


def build_p2(ctx_out, nqg=16, lchunks=16):
    nc = new_nc()
    s = Sch(nc)
    NQ = nqg * 512
    NTOK = NQ + (C if ctx_out else 0)
    LL = lchunks * 512
    qa = din(nc, "qa", [128, NQ + C], F32R)
    ka = din(nc, "ka", [128, S + C], F32R)
    va = din(nc, "va", [128, NKT, 128], F32R)
    qn = din(nc, "qn", [128, NQ + C], F32R)
    kn = din(nc, "kn", [128, S + C], F32R)
    vn = din(nc, "vn", [128, NKT, 128], F32R)
    bias = din(nc, "bias", [3, 8, 128, 512])
    ub = din(nc, "ub", [2, 128, LL + C], F32R)
    gb = din(nc, "gb", [2, 128, LL + C], F32R)
    cw = din(nc, "cw", [128, 2, 5])
    wri = din(nc, "wri", [2, 2, 2, 128, 128], F32R)
    bri = din(nc, "bri", [128, 2, 2, 2])
    lam = din(nc, "lam", [128, 2, 2])
    oT = dout(nc, "oT", [4, 128, max(NTOK, LL + (C if ctx_out else 0))])
    outr = R()
    cr = R()
    G = [sb(nc, "G%d" % i, [128, S + C], F32R) for i in range(3)]
    Gf = [g.bitcast(F32) for g in G]
    Gr = [R() for _ in range(3)]
    ones = sb(nc, "ones", [128, 128], F32R)
    s.op("dve", lambda: nc.vector.memset(ones.bitcast(F32), 1.0), wr=[cr])
    onec = sb(nc, "onec", [128, 1])
    s.op("dve", lambda: nc.vector.memset(onec, 1.0), wr=[cr])
    psS = Ring(nc, "pS", 3, [128, 512], F32, psum=True)
    psO = Ring(nc, "pO", 2, [128, 512], F32, psum=True)
    psD = Ring(nc, "pD", 2, [128, 512], F32, psum=True)
    ptr_ = Ring(nc, "pt", 3, [128, 512], F32R)
    tmr = Ring(nc, "tm", 3, [128, 512])
    bsr = Ring(nc, "bs", 4, [128, 512])
    rcr = Ring(nc, "rc", 2, [128, 512])
    str_ = Ring(nc, "st", 3, [128, 512])

    def attend(q_ap, nq, ktiles, out_ap):
        po, por = psO.nxt()
        pd, pdr = psD.nxt()
        nk = len(ktiles)

        def emit_s(i):
            ps, psr = psS.nxt()
            mm(s, ps[:, :nq], psr, ktiles[i][0], q_ap, True, True, [Gr[0], Gr[1]])
            return ps, psr
        cur = emit_s(0)
        for i in range(nk):
            nxt_ = emit_s(i + 1) if i + 1 < nk else None
            ps, psr = cur
            pt, ptr__ = ptr_.nxt()
            bd = ktiles[i][2]
            if bd is not None:
                bt, btr = bsr.nxt()
                s.dma("sp", bt[:, :nq], bd[:, :nq], wr=[btr])
                tm, tmr_ = tmr.nxt()
                s.op("dve", lambda tm=tm, ps=ps, bt=bt: nc.vector.scalar_tensor_tensor(
                    out=tm[:, :nq], in0=ps[:, :nq], scalar=SCALE, in1=bt[:, :nq], op0=ALU.mult, op1=ALU.add),
                    rd=[psr, btr], wr=[tmr_])
                s.op("act", lambda pt=pt, tm=tm: nc.scalar.activation(out=pt[:, :nq], in_=tm[:, :nq], func=AF.Exp),
                     rd=[tmr_], wr=[ptr__])
            else:
                s.op("act", lambda pt=pt, ps=ps: nc.scalar.activation(out=pt[:, :nq], in_=ps[:, :nq], func=AF.Exp,
                                                                      scale=SCALE), rd=[psr], wr=[ptr__])
            mm(s, po[:, :nq], por, ktiles[i][1], pt[:, :nq], i == 0, i == nk - 1, [ptr__, Gr[2]])
            mm(s, pd[:, :nq], pdr, ones, pt[:, :nq], i == 0, i == nk - 1, [ptr__, cr])
            cur = nxt_
        rc, rcr_ = rcr.nxt()
        s.op("dve", lambda: nc.vector.reciprocal(out=rc[:, :nq], in_=pd[:, :nq]), rd=[pdr], wr=[rcr_])
        st, sr = str_.nxt()
        s.op("dve", lambda: nc.vector.tensor_tensor(out=st[:, :nq], in0=po[:, :nq], in1=rc[:, :nq], op=ALU.mult),
             rd=[por, rcr_], wr=[sr])
        s.dma("pool", out_ap, st[:, :nq], rd=[sr], wr=[outr])

    Gq = G[0]; Gk = G[1]; Gv = G[2].rearrange("p (t d) -> p t d", d=128)
    for (qd, kd, vd, och, is_na) in ((qa, ka, va, 0, False), (qn, kn, vn, 3, True)):
        s.dma("sp", Gq[:, :NQ + C], qd, wr=[Gr[0]])
        for h in range(2):
            s.dma("sp", Gk[:, h * 4224:(h + 1) * 4224], kd[:, h * 4224:(h + 1) * 4224], wr=[Gr[1]])
            s.dma("sp", Gv[:, h * 33:(h + 1) * 33, :], vd[:, h * 33:(h + 1) * 33, :], wr=[Gr[2]])
        for g in range(nqg):
            if not is_na:
                kts = [(Gk[:, t * 128:(t + 1) * 128], Gv[:, t, :], None) for t in range(NKT)]
            else:
                kr0 = min(max(8 * g - 4, 0), 112)
                tab = 0 if g == 0 else (2 if g == 15 else 1)
                kts = []
                for t8 in range(8):
                    t = kr0 // 2 + t8
                    kts.append((Gk[:, t * 128:(t + 1) * 128], Gv[:, t, :], bias[tab, t8]))
                for t in (64, 65):
                    kts.append((Gk[:, t * 128:(t + 1) * 128], Gv[:, t, :], None))
            attend(Gq[:, g * 512:(g + 1) * 512], 512, kts, oT[och, :, g * 512:(g + 1) * 512])
        if ctx_out:
            kts = [(Gk[:, t * 128:(t + 1) * 128], Gv[:, t, :], None) for t in (64, 65)]
            attend(Gq[:, NQ:NQ + C], C, kts, oT[och, :, NQ:NQ + C])

    cwt = sb(nc, "cwt", [128, 2, 5]); brit = sb(nc, "brit", [128, 2, 2, 2]); lamt = sb(nc, "lamt", [128, 2, 2])
    spt = sb(nc, "spt", [128, 2, 2])
    wt = sb(nc, "wt", [128, 8, 128], F32R)
    cr2 = R()
    s.dma("sp", cwt, cw, wr=[cr2]); s.dma("sp", brit, bri, wr=[cr2]); s.dma("sp", lamt, lam, wr=[cr2])
    s.dma("sp", wt, wri.rearrange("a d b k m -> k (a d b) m"), wr=[cr2])
    s.op("act", lambda: nc.scalar.activation(out=spt, in_=lamt, func=AF.Exp, scale=-1.0), rd=[cr2], wr=[cr2])
    s.op("act", lambda: nc.scalar.activation(out=spt, in_=spt, func=AF.Ln, bias=onec[:, 0:1], scale=1.0),
         rd=[cr2, cr], wr=[cr2])
    s.op("dve", lambda: nc.vector.tensor_scalar(out=spt, in0=spt, scalar1=-8.0, scalar2=None, op0=ALU.mult),
         rd=[cr2], wr=[cr2])
    U, UC, Y = Gf
    UW, UCW, YW = G
    Ur, UCr, Yr = Gr
    ar = Ring(nc, "la", 2, [128, 512]); br_ = Ring(nc, "lb", 2, [128, 512]); ir = Ring(nc, "li", 2, [128, 512])
    hr = Ring(nc, "lh", 2, [128, 512]); gr = Ring(nc, "lg", 2, [128, 512]); g2r = Ring(nc, "lg2", 2, [128, 512])
    ucr = Ring(nc, "luc", 2, [128, 512], F32R)
    hst = sb(nc, "hst", [128, 4]); hstr = R()
    segs = [("ctx", LL, C), ("lat", 0, LL)]
    for blk in range(2):
        s.dma("sp", UW[:, :LL + C], ub[blk], wr=[Ur])
        for (_, o, L) in segs:
            s.op("dve", lambda o=o, L=L: nc.vector.tensor_scalar(
                out=UCW[:, o:o + L], in0=U[:, o:o + L], scalar1=cwt[:, blk, 1:2], scalar2=cwt[:, blk, 4:5],
                op0=ALU.mult, op1=ALU.add), rd=[Ur, cr2], wr=[UCr])
            for (tap, do, so, n) in ((0, 1, 0, L - 1), (2, 0, 1, L - 1), (3, 0, 2, L - 2)):
                s.op("dve", lambda o=o, tap=tap, do=do, so=so, n=n: nc.vector.scalar_tensor_tensor(
                    out=UCW[:, o + do:o + do + n], in0=U[:, o + so:o + so + n], scalar=cwt[:, blk, tap:tap + 1],
                    in1=UC[:, o + do:o + do + n], op0=ALU.mult, op1=ALU.add), rd=[Ur, cr2, UCr], wr=[UCr])
        s.op("dve", lambda: nc.vector.memset(hst, 0.0), wr=[hstr])
        for d in range(2):
            chunks = [(LL, C)] + [(c * 512, 512) for c in (range(lchunks) if d == 0 else range(lchunks - 1, -1, -1))]
            prev = None
            for ci, (o, n) in enumerate(chunks):
                ucc, uccr = ucr.nxt()
                s.op("act", lambda ucc=ucc, o=o, n=n: nc.scalar.copy(out=ucc[:, :n], in_=UC[:, o:o + n]),
                     rd=[UCr], wr=[uccr])
                pr_, prr = psS.nxt()
                pi_, pir = psS.nxt()
                mm(s, pr_[:, :n], prr, wt[:, (0 * 2 + d) * 2 + blk, :], ucc[:, :n], True, True, [uccr, cr2])
                mm(s, pi_[:, :n], pir, wt[:, (1 * 2 + d) * 2 + blk, :], ucc[:, :n], True, True, [uccr, cr2])
                a, ar_ = ar.nxt(); b_, bbr = br_.nxt(); it, itr = ir.nxt(); h, hrr = hr.nxt()
                s.op("act", lambda a=a, pr_=pr_, n=n: nc.scalar.activation(
                    out=a[:, :n], in_=pr_[:, :n], func=AF.Sigmoid, bias=brit[:, 0, d, blk:blk + 1], scale=1.0),
                    rd=[prr, cr2], wr=[ar_])
                s.op("act", lambda it=it, pi_=pi_, n=n: nc.scalar.activation(
                    out=it[:, :n], in_=pi_[:, :n], func=AF.Sigmoid, bias=brit[:, 1, d, blk:blk + 1], scale=1.0),
                    rd=[pir, cr2], wr=[itr])
                s.op("act", lambda a=a, n=n: nc.scalar.activation(
                    out=a[:, :n], in_=a[:, :n], func=AF.Exp, scale=spt[:, d, blk:blk + 1]), rd=[ar_, cr2], wr=[ar_])
                s.op("act", lambda a=a, b_=b_, n=n: nc.scalar.activation(out=b_[:, :n], in_=a[:, :n], func=AF.Square),
                     rd=[ar_], wr=[bbr])
                s.op("act", lambda b_=b_, n=n: nc.scalar.activation(
                    out=b_[:, :n], in_=b_[:, :n], func=AF.Sqrt, bias=onec[:, 0:1], scale=-1.0), rd=[bbr, cr], wr=[bbr])
                s.op("dve", lambda it=it, o=o, n=n: nc.vector.tensor_tensor(
                    out=it[:, :n], in0=it[:, :n], in1=UC[:, o:o + n], op=ALU.mult), rd=[itr, UCr], wr=[itr])
                s.op("dve", lambda it=it, b_=b_, n=n: nc.vector.tensor_tensor(
                    out=b_[:, :n], in0=b_[:, :n], in1=it[:, :n], op=ALU.mult), rd=[itr, bbr], wr=[bbr])
                if d == 0:
                    hv, av, bv = h[:, :n], a[:, :n], b_[:, :n]
                    last = h[:, n - 1:n]
                else:
                    hv, av, bv = h[:, n - 1::-1] if n < 512 else h[:, ::-1], None, None
                    hv = h[:, 0:n][:, ::-1]; av = a[:, 0:n][:, ::-1]; bv = b_[:, 0:n][:, ::-1]
                    last = h[:, 0:1]
                init = 0.0 if ci == 0 else hst[:, d:d + 1]
                s.op("dve", lambda hv=hv, av=av, bv=bv, init=init: nc.vector.tensor_tensor_scan(
                    out=hv, data0=av, data1=bv, initial=init, op0=ALU.mult, op1=ALU.add),
                    rd=[ar_, bbr, hstr], wr=[hrr])
                s.op("dve", lambda last=last: nc.vector.tensor_copy(out=hst[:, d:d + 1], in_=last),
                     rd=[hrr], wr=[hstr])
                if d == 0:
                    s.op("pool", lambda h=h, o=o, n=n: nc.gpsimd.tensor_copy(out=YW[:, o:o + n], in_=h[:, :n]),
                         rd=[hrr], wr=[Yr])
                else:
                    s.op("pool", lambda h=h, o=o, n=n: nc.gpsimd.tensor_tensor(
                        out=YW[:, o:o + n], in0=Y[:, o:o + n], in1=h[:, :n], op=ALU.add), rd=[hrr, Yr], wr=[Yr])
        s.dma("sp", UW[:, :LL + C], gb[blk], wr=[Ur])
        ochunks = [(c * 512, 512, c * 512) for c in range(lchunks)]
        if ctx_out:
            ochunks.append((LL, C, NQ if NQ >= LL else LL))
        for (o, n, oo) in ochunks:
            g2, g2r_ = g2r.nxt(); gg, ggr = gr.nxt()
            gv = U[:, o:o + n]
            s.op("dve", lambda g2=g2, gv=gv, n=n: nc.vector.tensor_tensor(out=g2[:, :n], in0=gv, in1=gv, op=ALU.mult),
                 rd=[Ur], wr=[g2r_])
            s.op("dve", lambda g2=g2, n=n: nc.vector.tensor_scalar(
                out=g2[:, :n], in0=g2[:, :n], scalar1=0.044715, scalar2=1.0, op0=ALU.mult, op1=ALU.add),
                rd=[g2r_], wr=[g2r_])
            s.op("dve", lambda g2=g2, gv=gv, n=n: nc.vector.tensor_tensor(
                out=g2[:, :n], in0=g2[:, :n], in1=gv, op=ALU.mult), rd=[g2r_, Ur], wr=[g2r_])
            s.op("act", lambda g2=g2, gg=gg, n=n: nc.scalar.activation(
                out=gg[:, :n], in_=g2[:, :n], func=AF.Sigmoid, scale=1.5957691216057308), rd=[g2r_], wr=[ggr])
            s.op("dve", lambda gg=gg, gv=gv, n=n: nc.vector.tensor_tensor(
                out=gg[:, :n], in0=gg[:, :n], in1=gv, op=ALU.mult), rd=[ggr, Ur], wr=[ggr])
            st, sr = str_.nxt()
            s.op("dve", lambda st=st, gg=gg, o=o, n=n: nc.vector.tensor_tensor(
                out=st[:, :n], in0=gg[:, :n], in1=Y[:, o:o + n], op=ALU.mult), rd=[ggr, Yr], wr=[sr])
            s.dma("pool", oT[1 + blk, :, oo:oo + n], st[:, :n], rd=[sr], wr=[outr])
    s.finish([outr])
    return nc


def na_bias_tables(rpb_h):
    out = np.full((3, 8, 128, 512), -30000.0, np.float32)
    p = np.arange(128)
    qi = np.arange(512)
    qro = qi // 64
    j = qi % 64
    for ti, G in enumerate((0, 1, 15)):
        r = 8 * G + qro
        rs = np.clip(r - 4, 0, 120)
        cs = np.clip(j - 8, 0, 48)
        kr0 = min(max(8 * G - 4, 0), 112)
        for t8 in range(8):
            krow = (kr0 + 2 * t8 + p // 64)[:, None]
            kcol = (p % 64)[:, None]
            ok = (krow >= rs[None]) & (krow < rs[None] + 8) & (kcol >= cs[None]) & (kcol < cs[None] + 16)
            dr = np.clip(krow - r[None] + 7, 0, 14)
            dc = np.clip(kcol - j[None] + 15, 0, 30)
            vals = rpb_h[dr, dc]
            out[ti, t8] = np.where(ok, vals, np.float32(-30000.0))
    return out


def build_p3(tiles):
    nc = new_nc()
    s = Sch(nc)
    T = sum(n for n, _ in tiles)
    xT = din(nc, "xT", [NCH, 128, T])
    oT = din(nc, "oT", [NCH, 128, T], F32R)
    wo = din(nc, "wo", [NCH, 128, NCH, 128], F32R)
    gate = din(nc, "gate", [128, 2, NCH])
    g2 = din(nc, "g2", [128, NCH])
    scl = din(nc, "scl", [128, 2, NCH])
    sft = din(nc, "sft", [128, 2, NCH])
    wr = din(nc, "wr", [128, NCH, 36])
    rb = din(nc, "rb", [128, 36])
    iot = din(nc, "iot", [128, 32])
    x1T = dout(nc, "x1T", [NCH, 128, T])
    h2T = dout(nc, "h2T", [NCH, 128, T])
    rt = dout(nc, "rt", [T, 4])
    outr = R(); cr = R()
    gatet = sb(nc, "gatet", [128, 2, NCH]); g2t = sb(nc, "g2t", [128, NCH]); sclt = sb(nc, "sclt", [128, 2, NCH])
    sftt = sb(nc, "sftt", [128, 2, NCH]); wrt = sb(nc, "wrt", [128, NCH, 36]); rbt = sb(nc, "rbt", [128, 36])
    iott = sb(nc, "iott", [128, 32]); gst = sb(nc, "gst", [128, 2, NCH])
    onesD = sb(nc, "onesD", [128, 128], F32R); epst = sb(nc, "epst", [128, 1])
    for a, b_ in ((gatet, gate), (g2t, g2), (sclt, scl), (sftt, sft), (wrt, wr), (rbt, rb), (iott, iot)):
        s.dma("sp", a, b_, wr=[cr])
    s.op("dve", lambda: nc.vector.memset(onesD.bitcast(F32), 1.0 / D), wr=[cr])
    s.op("dve", lambda: nc.vector.memset(epst, EPS), wr=[cr])
    for k in range(2):
        s.op("dve", lambda k=k: nc.vector.scalar_tensor_tensor(out=gst[:, k, :], in0=sclt[:, k, :], scalar=1.0,
                                                               in1=g2t, op0=ALU.add, op1=ALU.mult), rd=[cr], wr=[cr])
    xr = Ring(nc, "x", 2, [128, NCH, 512])
    orr = Ring(nc, "o", 1, [128, NCH, 512], F32R)
    hr = Ring(nc, "h", 1, [128, NCH, 512])
    sqr = Ring(nc, "sq", 2, [128, 512], F32R)
    wr_ = Ring(nc, "w", 4, [128, NCH, 128], F32R)
    psA = Ring(nc, "pA", 3, [128, 512], F32, psum=True)
    psB = Ring(nc, "pB", 2, [128, 512], F32, psum=True)
    psR = Ring(nc, "pR", 2, [128, 64], F32, psum=True)
    rsr = Ring(nc, "rs", 2, [128, 512])
    smr = Ring(nc, "sm", 2, [128, 160])
    t0 = 0
    for (n, kind) in tiles:
        xt, xtr = xr.nxt()
        ot, otr = orr.nxt()
        s.dma("sp", xt[:, :, :n], xT[:, :, t0:t0 + n].rearrange("c p t -> p c t"), wr=[xtr])
        s.dma("sp", ot[:, :, :n], oT[:, :, t0:t0 + n].rearrange("c p t -> p c t"), wr=[otr])
        for fc in range(NCH):
            wt, wtr = wr_.nxt()
            s.dma("sp", wt, wo[fc], wr=[wtr])
            pa, par = psA.nxt()
            for c in range(NCH):
                mm(s, pa[:, :n], par, wt[:, c, :], ot[:, c, :n], c == 0, c == NCH - 1, [wtr, otr])
            s.op("dve", lambda fc=fc, pa=pa: nc.vector.scalar_tensor_tensor(
                out=xt[:, fc, :n], in0=pa[:, :n], scalar=gatet[:, kind, fc:fc + 1], in1=xt[:, fc, :n],
                op0=ALU.mult, op1=ALU.add), rd=[par, cr, xtr], wr=[xtr])
        s.dma("pool", x1T[:, :, t0:t0 + n].rearrange("c p t -> p c t"), xt[:, :, :n], rd=[xtr], wr=[outr])
        pb, pbr = psB.nxt()
        for c in range(NCH):
            sq, sqr_ = sqr.nxt()
            s.op("act", lambda c=c, sq=sq: nc.scalar.activation(out=sq[:, :n], in_=xt[:, c, :n], func=AF.Square),
                 rd=[xtr], wr=[sqr_])
            mm(s, pb[:, :n], pbr, onesD, sq[:, :n], c == 0, c == NCH - 1, [sqr_, cr])
        rs, rsr_ = rsr.nxt()
        rstd(s, rs[:, :n], rsr_, pb[:, :n], pbr, epst, cr)
        ht, htr = hr.nxt()
        for c in range(NCH):
            s.op("dve", lambda c=c: nc.vector.scalar_tensor_tensor(
                out=ht[:, c, :n], in0=xt[:, c, :n], scalar=gst[:, kind, c:c + 1], in1=rs[:, :n],
                op0=ALU.mult, op1=ALU.mult), rd=[xtr, rsr_, cr], wr=[htr])
            s.op("act", lambda c=c: nc.scalar.activation(out=ht[:, c, :n], in_=ht[:, c, :n], func=AF.Identity,
                                                         bias=sftt[:, kind, c:c + 1], scale=1.0),
                 rd=[htr, cr], wr=[htr])
        s.dma("pool", h2T[:, :, t0:t0 + n].rearrange("c p t -> p c t"), ht[:, :, :n], rd=[htr], wr=[outr])
        for sub in range((n + 127) // 128):
            nt = min(128, n - sub * 128)
            pr_, prr = psR.nxt()
            for c in range(NCH):
                mm(s, pr_[:nt, :36], prr, ht[:, c, sub * 128:sub * 128 + nt], wrt[:, c, :], c == 0, c == NCH - 1,
                   [htr, cr])
            sm, smr_ = smr.nxt()
            lg = sm[:nt, 0:36]; lem = sm[:nt, 36:68]; mk = sm[:nt, 68:100]; tmp = sm[:nt, 100:132]
            sc = sm[:nt, 132:160]

            def V(fn, rd=(), wr=()):
                s.op("dve", fn, rd=list(rd) + [smr_], wr=[smr_] + list(wr))
            V(lambda: nc.vector.tensor_tensor(out=lg, in0=pr_[:nt, :36], in1=rbt[:nt, :], op=ALU.add), rd=[prr, cr])
            V(lambda: nc.vector.tensor_reduce(out=sc[:, 0:1], in_=lg[:, 0:4], axis=AX.X, op=ALU.max))
            V(lambda: nc.vector.tensor_scalar(out=sc[:, 1:2], in0=sc[:, 0:1], scalar1=-1.0, scalar2=None, op0=ALU.mult))
            s.op("act", lambda: nc.scalar.activation(out=sc[:, 12:16], in_=lg[:, 0:4], func=AF.Exp, bias=sc[:, 1:2],
                                                     scale=1.0), rd=[smr_], wr=[smr_])
            V(lambda: nc.vector.tensor_reduce(out=sc[:, 2:3], in_=sc[:, 12:16], axis=AX.X, op=ALU.add))
            V(lambda: nc.vector.reciprocal(out=sc[:, 3:4], in_=sc[:, 2:3]))
            V(lambda: nc.vector.tensor_scalar(out=sc[:, 16:20], in0=lg[:, 0:4], scalar1=sc[:, 0:1], scalar2=None,
                                              op0=ALU.is_ge))
            V(lambda: nc.vector.tensor_scalar(out=sc[:, 16:20], in0=sc[:, 16:20], scalar1=-1.0, scalar2=1e30,
                                              op0=ALU.add, op1=ALU.mult))
            for g in range(4):
                V(lambda g=g: nc.vector.tensor_scalar(out=lem[:, g * 8:(g + 1) * 8], in0=lg[:, 4 + g * 8:12 + g * 8],
                                                      scalar1=sc[:, 16 + g:17 + g], scalar2=None, op0=ALU.add))
            for k in range(2):
                V(lambda k=k: nc.vector.tensor_reduce(out=sc[:, 4 + k:5 + k], in_=lem, axis=AX.X, op=ALU.max))
                V(lambda k=k: nc.vector.tensor_scalar(out=mk, in0=lem, scalar1=sc[:, 4 + k:5 + k], scalar2=None,
                                                      op0=ALU.is_ge))
                V(lambda: nc.vector.tensor_tensor(out=tmp, in0=mk, in1=iott[:nt, :], op=ALU.mult), rd=[cr])
                V(lambda k=k: nc.vector.tensor_reduce(out=sc[:, 6 + k:7 + k], in_=tmp, axis=AX.X, op=ALU.add))
                if k == 0:
                    V(lambda: nc.vector.scalar_tensor_tensor(out=lem, in0=mk, scalar=-1e30, in1=lem,
                                                             op0=ALU.mult, op1=ALU.add))
            V(lambda: nc.vector.tensor_tensor(out=sc[:, 10:11], in0=sc[:, 5:6], in1=sc[:, 4:5], op=ALU.subtract))
            s.op("act", lambda: nc.scalar.activation(out=sc[:, 10:11], in_=sc[:, 10:11], func=AF.Exp),
                 rd=[smr_], wr=[smr_])
            V(lambda: nc.vector.tensor_scalar(out=sc[:, 10:11], in0=sc[:, 10:11], scalar1=1.0, scalar2=None,
                                              op0=ALU.add))
            V(lambda: nc.vector.reciprocal(out=sc[:, 11:12], in_=sc[:, 10:11]))
            V(lambda: nc.vector.tensor_tensor(out=sc[:, 8:9], in0=sc[:, 11:12], in1=sc[:, 3:4], op=ALU.mult))
            V(lambda: nc.vector.tensor_tensor(out=sc[:, 9:10], in0=sc[:, 3:4], in1=sc[:, 8:9], op=ALU.subtract))
            s.dma("pool", rt[t0 + sub * 128:t0 + sub * 128 + nt, :], sc[:, 6:10], rd=[smr_], wr=[outr])
        t0 += n
    s.finish([outr])
    return nc


def build_p4(cap, nel=4):
    nc = new_nc()
    s = Sch(nc)
    XT = din(nc, "XT", [nel, NCH, 128, cap], F32R)
    wg = din(nc, "wg", [nel, NHC, 128, NCH, 128], F32R)
    wu = din(nc, "wu", [nel, NHC, 128, NCH, 128], F32R)
    wd = din(nc, "wd", [nel, NCH, 128, NHC, 128], F32R)
    YT = dout(nc, "YT", [nel, NCH, 128, cap])
    outr = R()
    GS = 1024
    xr = Ring(nc, "x", 1, [128, NCH, GS], F32R)
    wgr = Ring(nc, "wg", 3, [128, NCH, 128], F32R)
    wur = Ring(nc, "wu", 3, [128, NCH, 128], F32R)
    wdr = Ring(nc, "wd", 4, [128, NHC, 128], F32R)
    ar = Ring(nc, "a", 1, [128, NHC, GS], F32R)
    sgr = Ring(nc, "sg", 2, [128, 512])
    str_ = Ring(nc, "st", 3, [128, 512])
    psG = Ring(nc, "pG", 2, [128, 512], F32, psum=True)
    psU = Ring(nc, "pU", 2, [128, 512], F32, psum=True)
    psO = Ring(nc, "pO", 3, [128, 512], F32, psum=True)
    groups = [(g0, min(GS, cap - g0)) for g0 in range(0, cap, GS)]
    for e in range(nel):
        for (g0, gn) in groups:
            nt = gn // 512
            xt, xtr = xr.nxt()
            s.dma("sp", xt[:, :, :gn], XT[e, :, :, g0:g0 + gn].rearrange("c p t -> p c t"), wr=[xtr])
            at, atr = ar.nxt()
            for hc in range(NHC):
                wgt, wgtr = wgr.nxt(); wut, wutr = wur.nxt()
                s.dma("sp", wgt, wg[e, hc], wr=[wgtr])
                s.dma("sp", wut, wu[e, hc], wr=[wutr])
                for t in range(nt):
                    ts_ = slice(t * 512, (t + 1) * 512)
                    pg, pgr = psG.nxt(); pu, pur = psU.nxt()
                    for c in range(NCH):
                        mm(s, pg, pgr, wgt[:, c, :], xt[:, c, ts_], c == 0, c == NCH - 1, [wgtr, xtr])
                    for c in range(NCH):
                        mm(s, pu, pur, wut[:, c, :], xt[:, c, ts_], c == 0, c == NCH - 1, [wutr, xtr])
                    sg, sgr_ = sgr.nxt()
                    s.op("act", lambda sg=sg, pg=pg: nc.scalar.activation(out=sg, in_=pg, func=AF.Silu),
                         rd=[pgr], wr=[sgr_])
                    s.op("dve", lambda sg=sg, pu=pu, hc=hc, at=at, ts_=ts_: nc.vector.tensor_tensor(
                        out=at[:, hc, ts_], in0=sg, in1=pu, op=ALU.mult), rd=[sgr_, pur], wr=[atr])
            for fc in range(NCH):
                wdt, wdtr = wdr.nxt()
                s.dma("sp", wdt, wd[e, fc], wr=[wdtr])
                for t in range(nt):
                    ts_ = slice(t * 512, (t + 1) * 512)
                    po, por = psO.nxt()
                    for hc in range(NHC):
                        mm(s, po, por, wdt[:, hc, :], at[:, hc, ts_], hc == 0, hc == NHC - 1, [wdtr, atr])
                    st, sr = str_.nxt()
                    if (fc + t) % 2 == 0:
                        s.op("act", lambda st=st, po=po: nc.scalar.copy(out=st, in_=po), rd=[por], wr=[sr])
                    else:
                        s.op("dve", lambda st=st, po=po: nc.vector.tensor_copy(out=st, in_=po), rd=[por], wr=[sr])
                    s.dma("pool", YT[e, fc, :, g0 + t * 512:g0 + (t + 1) * 512], st, rd=[sr], wr=[outr])
    s.finish([outr])
    return nc


def build_p5(tiles, final):
    nc = new_nc()
    s = Sch(nc)
    T = sum(n for n, _ in tiles)
    x1T = din(nc, "x1T", [NCH, 128, T])
    y1T = din(nc, "y1T", [NCH, 128, T])
    y2T = din(nc, "y2T", [NCH, 128, T])
    wbc = din(nc, "wbc", [128, 2, T])
    gate = din(nc, "gate", [128, 2, NCH])
    fg = din(nc, "fg", [128, NCH])
    xoT = dout(nc, "xoT", [NCH, 128, T])
    outr = R(); cr = R()
    gatet = sb(nc, "gatet", [128, 2, NCH]); fgt = sb(nc, "fgt", [128, NCH])
    onesD = sb(nc, "onesD", [128, 128], F32R); epst = sb(nc, "epst", [128, 1])
    s.dma("sp", gatet, gate, wr=[cr]); s.dma("sp", fgt, fg, wr=[cr])
    s.op("dve", lambda: nc.vector.memset(onesD.bitcast(F32), 1.0 / D), wr=[cr])
    s.op("dve", lambda: nc.vector.memset(epst, EPS), wr=[cr])
    xr = Ring(nc, "x", 2, [128, NCH, 512])
    y1r = Ring(nc, "y1", 1, [128, NCH, 512])
    y2r = Ring(nc, "y2", 1, [128, NCH, 512])
    wr_ = Ring(nc, "w", 2, [128, 2, 512])
    sqr = Ring(nc, "sq", 2, [128, 512], F32R)
    rsr = Ring(nc, "rs", 2, [128, 512])
    psB = Ring(nc, "pB", 2, [128, 512], F32, psum=True)
    t0 = 0
    for (n, kind) in tiles:
        xt, xtr = xr.nxt(); y1, y1r_ = y1r.nxt(); y2, y2r_ = y2r.nxt(); wt, wtr = wr_.nxt()
        s.dma("sp", xt[:, :, :n], x1T[:, :, t0:t0 + n].rearrange("c p t -> p c t"), wr=[xtr])
        s.dma("sp", y1[:, :, :n], y1T[:, :, t0:t0 + n].rearrange("c p t -> p c t"), wr=[y1r_])
        s.dma("sp", y2[:, :, :n], y2T[:, :, t0:t0 + n].rearrange("c p t -> p c t"), wr=[y2r_])
        s.dma("sp", wt[:, :, :n], wbc[:, :, t0:t0 + n], wr=[wtr])
        for c in range(NCH):
            s.op("dve", lambda c=c: nc.vector.tensor_tensor(out=y1[:, c, :n], in0=y1[:, c, :n], in1=wt[:, 0, :n],
                                                            op=ALU.mult), rd=[y1r_, wtr], wr=[y1r_])
            s.op("pool", lambda c=c: nc.gpsimd.tensor_tensor(out=y2[:, c, :n], in0=y2[:, c, :n], in1=wt[:, 1, :n],
                                                             op=ALU.mult), rd=[y2r_, wtr], wr=[y2r_])
            s.op("dve", lambda c=c: nc.vector.tensor_tensor(out=y1[:, c, :n], in0=y1[:, c, :n], in1=y2[:, c, :n],
                                                            op=ALU.add), rd=[y1r_, y2r_], wr=[y1r_])
            s.op("dve", lambda c=c: nc.vector.scalar_tensor_tensor(
                out=xt[:, c, :n], in0=y1[:, c, :n], scalar=gatet[:, kind, c:c + 1], in1=xt[:, c, :n],
                op0=ALU.mult, op1=ALU.add), rd=[y1r_, cr, xtr], wr=[xtr])
        if final:
            pb, pbr = psB.nxt()
            for c in range(NCH):
                sq, sqr_ = sqr.nxt()
                s.op("act", lambda c=c, sq=sq: nc.scalar.activation(out=sq[:, :n], in_=xt[:, c, :n], func=AF.Square),
                     rd=[xtr], wr=[sqr_])
                mm(s, pb[:, :n], pbr, onesD, sq[:, :n], c == 0, c == NCH - 1, [sqr_, cr])
            rs, rsr_ = rsr.nxt()
            rstd(s, rs[:, :n], rsr_, pb[:, :n], pbr, epst, cr)
            for c in range(NCH):
                s.op("dve", lambda c=c: nc.vector.scalar_tensor_tensor(
                    out=xt[:, c, :n], in0=xt[:, c, :n], scalar=fgt[:, c:c + 1], in1=rs[:, :n],
                    op0=ALU.mult, op1=ALU.mult), rd=[xtr, rsr_, cr], wr=[xtr])
        s.dma("pool", xoT[:, :, t0:t0 + n].rearrange("c p t -> p c t"), xt[:, :, :n], rd=[xtr], wr=[outr])
        t0 += n
    s.finish([outr])
    return nc


def _vt(vT):
    nt = vT.shape[1] // 128
    return np.ascontiguousarray(vT.T.reshape(nt, 128, 128).transpose(1, 0, 2))


def _wo_layout(w):
    return np.ascontiguousarray(w.reshape(NCH, 128, NCH, 128).transpose(2, 1, 0, 3))


def _wg_layout(w):
    return w.reshape(NCH, 128, NHC, 128).transpose(2, 1, 0, 3)


def _wd_layout(w):
    return w.reshape(NHC, 128, NCH, 128).transpose(2, 1, 0, 3)


def kernel(x, c, ctx, c_ctx, ada_w, ada_b, norm1_g, norm2_g, w_in, w_out, att_q_norm, att_k_norm,
           conv_w, conv_b, lru_wr, lru_br, lru_wi, lru_bi, lru_lambda, na_rpb,
           router_wg, router_bg, router_we, router_be, moe_w_gate, moe_w_up, moe_w_down, final_g):
    f32 = np.float32
    A = lambda a: np.asarray(a, dtype=f32)
    x = A(x); c = A(c); ctx = A(ctx); c_ctx = A(c_ctx); ada_w = A(ada_w); ada_b = A(ada_b)
    norm1_g = A(norm1_g); norm2_g = A(norm2_g); w_in = A(w_in); w_out = A(w_out)
    att_q_norm = A(att_q_norm); att_k_norm = A(att_k_norm); conv_w = A(conv_w); conv_b = A(conv_b)
    lru_wr = A(lru_wr); lru_br = A(lru_br); lru_wi = A(lru_wi); lru_bi = A(lru_bi); lru_lambda = A(lru_lambda)
    na_rpb = A(na_rpb); router_wg = A(router_wg); router_bg = A(router_bg); router_we = A(router_we)
    router_be = A(router_be); moe_w_gate = A(moe_w_gate); moe_w_up = A(moe_w_up); moe_w_down = A(moe_w_down)
    final_g = A(final_g)
    TLc = S // 4
    TCc = C // 4
    tiles_lc = [(512, 0)] * 4 + [(TCc, 1)]
    tiles_l = [(512, 0)] * 4

    NCOL = 12288 // NCORES
    cc = np.stack([c[0], c[1], c_ctx, c_ctx], 0)
    cT = np.ascontiguousarray(cc.T.reshape(NCH, 128, 4).transpose(1, 0, 2))
    ins = []
    for k in range(NCORES):
        ins.append({"cT": cT,
                    "w": np.ascontiguousarray(ada_w[:, :, k * NCOL:(k + 1) * NCOL].reshape(DEPTH, NCH, 128, NCOL)),
                    "bias": np.ascontiguousarray(np.broadcast_to(ada_b[None, :, k * NCOL:(k + 1) * NCOL],
                                                                 (4, DEPTH, NCOL)))})
    res = run(build_ada(), ins)
    mod = np.concatenate([r["mod"] for r in res], axis=2)

    xs = []
    for k in range(NCORES):
        b, r = k // 4, k % 4
        xs.append(tok_fm(np.concatenate([x[b, r * TLc:(r + 1) * TLc], ctx[b, r * TCc:(r + 1) * TCc]], 0)))
    rot = rot_const()
    iot = np.ascontiguousarray(np.broadcast_to(np.arange(32, dtype=f32)[None], (128, 32)))
    out = np.zeros((B, S, D), f32)

    for l in range(DEPTH):
        last = l == DEPTH - 1
        m = mod[:, l].reshape(4, 6, D)
        wl = w_in_layout(w_in[l])
        g1 = fm(norm1_g[l])
        gqk = np.ascontiguousarray(np.stack([att_q_norm[l], att_k_norm[l]], 1))
        ins = []
        for k in range(NCORES):
            b, r = k // 4, k % 4
            ins.append({"xT": xs[k], "w": wl, "g1": g1,
                        "scl": fm(np.stack([m[b, 1], m[2, 1]])), "sft": fm(np.stack([m[b, 0], m[2, 0]])),
                        "gqk": gqk, "cs": rope_consts(np.arange(r * TLc, (r + 1) * TLc)), "rot": rot})
        res = run(build_p1(tiles_lc), ins)
        full = []
        for b in range(B):
            lat = np.concatenate([res[4 * b + r]["PT"][:, :, :TLc] for r in range(4)], axis=2)
            cx = np.concatenate([res[4 * b + r]["PT"][:, :, TLc:] for r in range(4)], axis=2)
            full.append(np.concatenate([lat, cx], axis=2))
        del res
        ins = []
        for k in range(NCORES):
            b, i = k // 4, k % 4
            F = full[b]
            cw = np.zeros((128, 2, 5), f32)
            bri = np.zeros((128, 2, 2, 2), f32)
            lam = np.zeros((128, 2, 2), f32)
            wri = np.zeros((2, 2, 2, 128, 128), f32)
            for kk in range(2):
                j = 2 * i + kk
                ch = slice(j * 128, (j + 1) * 128)
                cw[:, kk, :4] = conv_w[l][:, ch].T
                cw[:, kk, 4] = conv_b[l][ch]
                for d_ in range(2):
                    bri[:, 0, d_, kk] = lru_br[l][d_, ch]
                    bri[:, 1, d_, kk] = lru_bi[l][d_, ch]
                    lam[:, d_, kk] = lru_lambda[l][d_, ch]
                    wri[0, d_, kk] = lru_wr[l][d_, j]
                    wri[1, d_, kk] = lru_wi[l][d_, j]
            ins.append({"qa": np.ascontiguousarray(F[i]), "ka": np.ascontiguousarray(F[4 + i // 2]),
                        "va": _vt(F[6 + i // 2]),
                        "qn": np.ascontiguousarray(F[24 + i]), "kn": np.ascontiguousarray(F[28 + i]),
                        "vn": _vt(F[32 + i]), "bias": na_bias_tables(na_rpb[l, i]),
                        "ub": np.ascontiguousarray(F[8 + 2 * i:10 + 2 * i]),
                        "gb": np.ascontiguousarray(F[16 + 2 * i:18 + 2 * i]),
                        "cw": cw, "wri": wri, "bri": bri, "lam": lam})
        del full
        res = run(build_p2(not last), ins)
        ntok = S + (0 if last else C)
        OT = []
        for b in range(B):
            o = np.zeros((NCH, 128, ntok), f32)
            for i in range(4):
                oc = res[4 * b + i]["oT"]
                o[i] = oc[0][:, :ntok]
                o[4 + 2 * i] = oc[1][:, :ntok]
                o[5 + 2 * i] = oc[2][:, :ntok]
                o[12 + i] = oc[3][:, :ntok]
            OT.append(o)
        del res
        tiles = tiles_l if last else tiles_lc
        Tc = sum(n for n, _ in tiles)
        wol = _wo_layout(w_out[l])
        wcat = np.concatenate([router_wg[l], router_we[l]], 1)
        bcat = np.concatenate([router_bg[l], router_be[l]])
        wrl = np.ascontiguousarray(wcat.reshape(NCH, 128, 36).transpose(1, 0, 2))
        rbl = np.ascontiguousarray(np.broadcast_to(bcat[None], (128, 36)))
        g2 = fm(norm2_g[l])
        ins = []
        for k in range(NCORES):
            b, r = k // 4, k % 4
            if last:
                oTk = np.ascontiguousarray(OT[b][:, :, r * TLc:(r + 1) * TLc])
                xTk = np.ascontiguousarray(xs[k][:, :, :TLc])
            else:
                oTk = np.concatenate([OT[b][:, :, r * TLc:(r + 1) * TLc],
                                      OT[b][:, :, S + r * TCc:S + (r + 1) * TCc]], axis=2)
                xTk = xs[k]
            ins.append({"xT": xTk, "oT": np.ascontiguousarray(oTk), "wo": wol,
                        "gate": fm(np.stack([m[b, 2], m[2, 2]])), "g2": g2,
                        "scl": fm(np.stack([m[b, 4], m[2, 4]])), "sft": fm(np.stack([m[b, 3], m[2, 3]])),
                        "wr": wrl, "rb": rbl, "iot": iot})
        del OT
        res = run(build_p3(tiles), ins)
        x1 = [r_["x1T"] for r_ in res]
        rts = [r_["rt"] for r_ in res]
        H = np.concatenate([fm_tok(r_["h2T"]) for r_ in res], axis=0)
        del res
        rt_all = np.concatenate(rts, axis=0)
        E = np.rint(rt_all[:, :2]).astype(np.int64)
        lists = []
        for e in range(NE):
            tok, kk = np.nonzero(E == e)
            lists.append((tok, kk))
        cap = 512 * max(1, -(-max(len(t) for t, _ in lists) // 512))
        ins = []
        for k in range(NCORES):
            XT = np.zeros((4, NCH, 128, cap), f32)
            wg = np.empty((4, NHC, 128, NCH, 128), f32)
            wu = np.empty((4, NHC, 128, NCH, 128), f32)
            wd = np.empty((4, NCH, 128, NHC, 128), f32)
            for j in range(4):
                e = 4 * k + j
                tok = lists[e][0]
                XT[j][:, :, :len(tok)] = tok_fm(H[tok])
                wg[j] = _wg_layout(moe_w_gate[l, e])
                wu[j] = _wg_layout(moe_w_up[l, e])
                wd[j] = _wd_layout(moe_w_down[l, e])
            ins.append({"XT": XT, "wg": wg, "wu": wu, "wd": wd})
        del H
        res = run(build_p4(cap, 4), ins)
        del ins
        Y = np.zeros((2, NCORES * Tc, D), f32)
        for k in range(NCORES):
            for j in range(4):
                e = 4 * k + j
                tok, kk = lists[e]
                ye = fm_tok(res[k]["YT"][j])
                Y[kk, tok] = ye[:len(tok)]
        del res
        fg = fm(final_g)
        ins = []
        for k in range(NCORES):
            b = k // 4
            sl = slice(k * Tc, (k + 1) * Tc)
            ins.append({"x1T": x1[k], "y1T": tok_fm(Y[0, sl]), "y2T": tok_fm(Y[1, sl]),
                        "wbc": np.ascontiguousarray(np.broadcast_to(rts[k][:, 2:4].T[None], (128, 2, Tc))),
                        "gate": fm(np.stack([m[b, 5], m[2, 5]])), "fg": fg})
        del Y
        res = run(build_p5(tiles, last), ins)
        if last:
            for k in range(NCORES):
                b, r = k // 4, k % 4
                out[b, r * TLc:(r + 1) * TLc] = fm_tok(res[k]["xoT"])
        else:
            xs = [r_["xoT"] for r_ in res]
        del res
    return out
```

```python
import numpy as np
import concourse.bass as bass
import concourse.mybir as mybir
from concourse.bass_utils import run_bass_kernel_spmd

F32 = mybir.dt.float32
F32R = mybir.dt.float32r
AF = mybir.ActivationFunctionType
ALU = mybir.AluOpType
AX = mybir.AxisListType

D = 2048; B = 2; S = 8192; C = 256; DEPTH = 2
NCH = 16
INW = 4608; NIC = 36
HID = 1024; NHC = 8
NE = 32
EPS = 1e-6
NCORES = 8


class R:
    __slots__ = ("w", "rs")

    def __init__(s):
        s.w = None
        s.rs = []


class Sch:
    def __init__(s, nc, ndma=20):
        s.nc = nc
        s.E = {"pe": nc.tensor, "act": nc.scalar, "dve": nc.vector, "pool": nc.gpsimd, "sp": nc.sync}
        s.sem = {}
        s.cnt = {}
        for k in ("pe", "act", "dve", "pool"):
            s.sem[k] = nc.alloc_semaphore("q_" + k)
            s.cnt[k] = 0
        s.seen = {k: {} for k in s.E}
        s.dpool = {}
        for q in ("sp", "pool", "act"):
            keys = []
            for i in range(ndma if q != "act" else 4):
                key = "d_%s%d" % (q, i)
                s.sem[key] = nc.alloc_semaphore(key)
                s.cnt[key] = 0
                keys.append(key)
            s.dpool[q] = [keys, 0]
        s.nid = 0

    def _wait(s, eng, toks):
        for t in toks:
            if t is None:
                continue
            key, val = t
            if key == eng and eng == "pe":
                continue
            if key.startswith("d_"):
                val = s.cnt[key]
            if s.seen[eng].get(key, 0) < val:
                s.E[eng].wait_ge(s.sem[key], val)
                s.seen[eng][key] = val

    def _deps(s, rd, wr):
        toks = []
        for r in rd:
            toks.append(r.w)
        for r in wr:
            toks.append(r.w)
            toks.extend(r.rs)
        return toks

    def _mark(s, tk, rd, wr):
        for r in rd:
            r.rs.append(tk)
            if len(r.rs) > 64:
                r.rs = r.rs[-48:]
        for r in wr:
            r.w = tk
            r.rs = []

    def op(s, eng, fn, rd=(), wr=()):
        s._wait(eng, s._deps(rd, wr))
        ins = fn()
        s.cnt[eng] += 1
        ins.then_inc(s.sem[eng], 1)
        s._mark((eng, s.cnt[eng]), rd, wr)

    def dma(s, q, out, in_, rd=(), wr=()):
        s._wait(q, s._deps(rd, wr))
        keys, i = s.dpool[q]
        key = keys[i % len(keys)]
        s.dpool[q][1] = i + 1
        s.cnt[key] += 16
        s.E[q].dma_start(out=out, in_=in_).then_inc(s.sem[key], 16)
        s._mark((key, s.cnt[key]), rd, wr)

    def finish(s, outs):
        toks = []
        for r in outs:
            toks.append(r.w)
        s._wait("sp", toks)
        for key in s.sem:
            if key.startswith("d_") and s.cnt[key] > 0:
                if s.seen["sp"].get(key, 0) < s.cnt[key]:
                    s.E["sp"].wait_ge(s.sem[key], s.cnt[key])
                    s.seen["sp"][key] = s.cnt[key]


def new_nc():
    nc = bass.Bass("TRN2", target_bir_lowering=False)
    nc.dge_precook = False
    return nc


def sb(nc, name, shape, dt=F32):
    return nc.alloc_sbuf_tensor(name, list(shape), dt).ap()


def din(nc, name, shape, dt=F32):
    return nc.dram_tensor(name, list(shape), dt, kind="ExternalInput").ap()


def dout(nc, name, shape, dt=F32):
    return nc.dram_tensor(name, list(shape), dt, kind="ExternalOutput").ap()


class Ring:
    def __init__(s, nc, name, n, shape, dt=F32, psum=False):
        s.b = []
        for i in range(n):
            if psum:
                ap = nc.alloc_psum_tensor("%s%d" % (name, i), list(shape), dt).ap()
            else:
                ap = sb(nc, "%s%d" % (name, i), shape, dt)
            s.b.append((ap, R()))
        s.i = 0

    def nxt(s):
        x = s.b[s.i % len(s.b)]
        s.i += 1
        return x


def mm(s, ps, psr, lhsT, rhs, start, stop, rd):
    s.op("pe", lambda: s.nc.tensor.matmul(ps, lhsT, rhs, start=start, stop=stop), rd=rd, wr=[psr])


def rstd(s, out, outr, ms, msr, epst, cr):
    nc = s.nc
    s.op("act", lambda: nc.scalar.activation(out=out, in_=ms, func=AF.Sqrt, bias=epst[:, 0:1], scale=1.0),
         rd=[msr, cr], wr=[outr])
    s.op("dve", lambda: nc.vector.reciprocal(out=out, in_=out), rd=[outr], wr=[outr])

def build_ada():
    nc = new_nc()
    s = Sch(nc)
    NCOL = 12288 // NCORES
    cT = din(nc, "cT", [128, NCH, 4])
    w = din(nc, "w", [DEPTH, NCH, 128, NCOL])
    bias = din(nc, "bias", [4, DEPTH, NCOL])
    out = dout(nc, "mod", [4, DEPTH, NCOL])
    ct = sb(nc, "ct", [128, NCH, 4]); ctr = R()
    sg = sb(nc, "sg", [128, NCH, 4])
    bt = sb(nc, "bt", [4, DEPTH, NCOL]); btr = R()
    ot = sb(nc, "ot", [4, DEPTH, NCOL]); otr = R()
    wr_ = Ring(nc, "w", 3, [128, 4, NCOL], F32)
    pr = Ring(nc, "ps", 4, [128, 512], F32, psum=True)
    s.dma("sp", ct, cT, wr=[ctr])
    s.dma("sp", bt, bias, wr=[btr])
    sgr = R()
    s.op("act", lambda: nc.scalar.activation(out=sg, in_=ct, func=AF.Sigmoid), rd=[ctr], wr=[sgr])
    s.op("dve", lambda: nc.vector.tensor_tensor(out=sg, in0=sg, in1=ct, op=ALU.mult), rd=[sgr, ctr], wr=[sgr])
    for l in range(DEPTH):
        pss = [pr.nxt() for _ in range(NCOL // 512)]
        for kq in range(NCH // 4):
            wt, wtr = wr_.nxt()
            s.dma("sp", wt, w[l, kq * 4:(kq + 1) * 4].rearrange("c p n -> p c n"), wr=[wtr])
            for kk in range(4):
                k = kq * 4 + kk
                for j, (ps, psr) in enumerate(pss):
                    mm(s, ps[0:4, :], psr, sg[:, k, :], wt[:, kk, j * 512:(j + 1) * 512], k == 0, k == NCH - 1,
                       [sgr, wtr])
        for j, (ps, psr) in enumerate(pss):
            s.op("dve", lambda ps=ps, j=j: nc.vector.tensor_tensor(
                out=ot[:, l, j * 512:(j + 1) * 512], in0=ps[0:4, :], in1=bt[:, l, j * 512:(j + 1) * 512], op=ALU.add),
                rd=[psr, btr], wr=[otr])
    s.dma("sp", out, ot, rd=[otr], wr=[R()])
    s.finish([])
    return nc


def build_p1(tiles):
    nc = new_nc()
    s = Sch(nc)
    T = sum(n for n, _ in tiles)
    TL = sum(n for n, k in tiles if k == 0)
    xT = din(nc, "xT", [NCH, 128, T])
    w = din(nc, "w", [NIC, 128, NCH, 128], F32R)
    g1 = din(nc, "g1", [128, NCH])
    scl = din(nc, "scl", [128, 2, NCH])
    sft = din(nc, "sft", [128, 2, NCH])
    gqk = din(nc, "gqk", [128, 2])
    cs = din(nc, "cs", [128, 2, max(TL, 2)])
    rot = din(nc, "rot", [128, 128])
    PT = dout(nc, "PT", [NIC, 128, T])
    outr = R()
    cr = R()
    g1t = sb(nc, "g1t", [128, NCH]); sclt = sb(nc, "sclt", [128, 2, NCH]); sftt = sb(nc, "sftt", [128, 2, NCH])
    gqt = sb(nc, "gqt", [128, 2]); cst = sb(nc, "cst", [128, 2, max(TL, 2)]); rott = sb(nc, "rott", [128, 128])
    gst = sb(nc, "gst", [128, 2, NCH])
    onesD = sb(nc, "onesD", [128, 128], F32R); onesH = sb(nc, "onesH", [128, 128], F32R)
    epst = sb(nc, "epst", [128, 1])
    for a, b_ in ((g1t, g1), (sclt, scl), (sftt, sft), (gqt, gqk), (cst, cs), (rott, rot)):
        s.dma("sp", a, b_, wr=[cr])
    s.op("dve", lambda: nc.vector.memset(onesD.bitcast(F32), 1.0 / D), wr=[cr])
    s.op("dve", lambda: nc.vector.memset(onesH.bitcast(F32), 1.0 / 128), wr=[cr])
    s.op("dve", lambda: nc.vector.memset(epst, EPS), wr=[cr])
    for k in range(2):
        s.op("dve", lambda k=k: nc.vector.scalar_tensor_tensor(out=gst[:, k, :], in0=sclt[:, k, :], scalar=1.0,
                                                               in1=g1t, op0=ALU.add, op1=ALU.mult), rd=[cr], wr=[cr])
    groups = []
    cur, tot = [], 0
    for tl in tiles:
        if cur and tot + tl[0] > 1088:
            groups.append(cur); cur, tot = [], 0
        cur.append(tl); tot += tl[0]
    if cur:
        groups.append(cur)
    GMAX = max(sum(n for n, _ in g) for g in groups)
    xr = Ring(nc, "x", 1, [128, NCH, 512])
    hg = sb(nc, "hg", [128, NCH, GMAX], F32R); hgr = R()
    sqr = Ring(nc, "sq", 2, [128, 512], F32R)
    wr_ = Ring(nc, "w", 4, [128, NCH, 128], F32R)
    psA = Ring(nc, "pA", 3, [128, 512], F32, psum=True)
    psB = Ring(nc, "pB", 2, [128, 512], F32, psum=True)
    psC = Ring(nc, "pC", 2, [128, 512], F32, psum=True)
    rsr = Ring(nc, "rs", 2, [128, 512])
    qnr = Ring(nc, "qn", 2, [128, 512])
    t1r = Ring(nc, "t1", 2, [128, 512])
    str_ = Ring(nc, "st", 4, [128, 512])
    t0 = 0
    tl0 = 0
    for grp in groups:
        info = []
        off = 0
        for (n, kind) in grp:
            xt, xtr = xr.nxt()
            s.dma("sp", xt[:, :, :n], xT[:, :, t0 + off:t0 + off + n].rearrange("c p t -> p c t"), wr=[xtr])
            pb, pbr = psB.nxt()
            for c in range(NCH):
                sq, sqr_ = sqr.nxt()
                s.op("act", lambda c=c, sq=sq: nc.scalar.activation(out=sq[:, :n], in_=xt[:, c, :n], func=AF.Square),
                     rd=[xtr], wr=[sqr_])
                mm(s, pb[:, :n], pbr, onesD, sq[:, :n], c == 0, c == NCH - 1, [sqr_, cr])
            rs, rsr_ = rsr.nxt()
            rstd(s, rs[:, :n], rsr_, pb[:, :n], pbr, epst, cr)
            for c in range(NCH):
                s.op("dve", lambda c=c: nc.vector.scalar_tensor_tensor(
                    out=xt[:, c, :n], in0=xt[:, c, :n], scalar=gst[:, kind, c:c + 1], in1=rs[:, :n],
                    op0=ALU.mult, op1=ALU.mult), rd=[xtr, rsr_, cr], wr=[xtr])
                s.op("act", lambda c=c: nc.scalar.activation(out=hg[:, c, off:off + n], in_=xt[:, c, :n],
                                                             func=AF.Identity, bias=sftt[:, kind, c:c + 1], scale=1.0),
                     rd=[xtr, cr], wr=[hgr])
            info.append((off, n, kind, tl0))
            off += n
            if kind == 0:
                tl0 += n
        for j in range(NIC):
            wt, wtr = wr_.nxt()
            s.dma("sp", wt, w[j], wr=[wtr])
            for (o_, n, kind, tl) in info:
                pa, par = psA.nxt()
                for c in range(NCH):
                    mm(s, pa[:, :n], par, wt[:, c, :], hg[:, c, o_:o_ + n], c == 0, c == NCH - 1, [wtr, hgr])
                st, sr = str_.nxt()
                if j < 6:
                    gi = 0 if j < 4 else 1
                    sq, sqr_ = sqr.nxt()
                    s.op("act", lambda sq=sq: nc.scalar.activation(out=sq[:, :n], in_=pa[:, :n], func=AF.Square),
                         rd=[par], wr=[sqr_])
                    pb, pbr = psB.nxt()
                    mm(s, pb[:, :n], pbr, onesH, sq[:, :n], True, True, [sqr_, cr])
                    rs2, rs2r = rsr.nxt()
                    rstd(s, rs2[:, :n], rs2r, pb[:, :n], pbr, epst, cr)
                    if kind == 0:
                        qn, qnr_ = qnr.nxt()
                        s.op("dve", lambda qn=qn, rs2=rs2, pa=pa: nc.vector.scalar_tensor_tensor(
                            out=qn[:, :n], in0=pa[:, :n], scalar=gqt[:, gi:gi + 1], in1=rs2[:, :n],
                            op0=ALU.mult, op1=ALU.mult), rd=[par, rs2r, cr], wr=[qnr_])
                        pc, pcr = psC.nxt()
                        mm(s, pc[:, :n], pcr, rott, qn[:, :n], True, True, [qnr_, cr])
                        t1, t1r_ = t1r.nxt()
                        s.op("pool", lambda t1=t1, qn=qn: nc.gpsimd.tensor_tensor(
                            out=t1[:, :n], in0=qn[:, :n], in1=cst[:, 0, tl:tl + n], op=ALU.mult),
                            rd=[qnr_, cr], wr=[t1r_])
                        s.op("dve", lambda st=st, pc=pc: nc.vector.tensor_tensor(
                            out=st[:, :n], in0=pc[:, :n], in1=cst[:, 1, tl:tl + n], op=ALU.mult),
                            rd=[pcr, cr], wr=[sr])
                        s.op("dve", lambda st=st, t1=t1: nc.vector.tensor_tensor(
                            out=st[:, :n], in0=st[:, :n], in1=t1[:, :n], op=ALU.add), rd=[sr, t1r_], wr=[sr])
                    else:
                        s.op("dve", lambda st=st, rs2=rs2, pa=pa: nc.vector.scalar_tensor_tensor(
                            out=st[:, :n], in0=pa[:, :n], scalar=gqt[:, gi:gi + 1], in1=rs2[:, :n],
                            op0=ALU.mult, op1=ALU.mult), rd=[par, rs2r, cr], wr=[sr])
                else:
                    if j % 2 == 0:
                        s.op("act", lambda st=st, pa=pa: nc.scalar.copy(out=st[:, :n], in_=pa[:, :n]),
                             rd=[par], wr=[sr])
                    else:
                        s.op("dve", lambda st=st, pa=pa: nc.vector.tensor_copy(out=st[:, :n], in_=pa[:, :n]),
                             rd=[par], wr=[sr])
                s.dma("pool", PT[j, :, t0 + o_:t0 + o_ + n], st[:, :n], rd=[sr], wr=[outr])
        t0 += off
    s.finish([outr])
    return nc


def fm(v):
    v = np.asarray(v)
    lead = v.shape[:-1]
    a = v.reshape(*lead, NCH, 128)
    return np.ascontiguousarray(np.moveaxis(a, -1, 0))


def tok_fm(xtok):
    return np.ascontiguousarray(xtok.T.reshape(NCH, 128, xtok.shape[0]))


def fm_tok(xT):
    return np.ascontiguousarray(xT.reshape(D, xT.shape[2]).T)


def rope_consts(pos):
    pos = np.asarray(pos, dtype=np.int32)
    rows = (pos // 64).astype(np.float32)
    cols = (pos % 64).astype(np.float32)
    nf = 32
    inv = (np.float32(1.0) / (np.float32(10000.0) ** (np.arange(nf, dtype=np.float32) / np.float32(nf)))).astype(np.float32)
    ang = np.stack([rows[:, None] * inv, cols[:, None] * inv], axis=1).astype(np.float32)
    cos = np.cos(ang).astype(np.float32)
    sin = np.sin(ang).astype(np.float32)
    cs = np.zeros((128, 2, len(pos)), np.float32)
    for a in range(2):
        for h in range(2):
            cs[a * 64 + h * 32:a * 64 + h * 32 + 32, 0, :] = cos[:, a, :].T
            cs[a * 64 + h * 32:a * 64 + h * 32 + 32, 1, :] = sin[:, a, :].T
    return cs


def rot_const():
    rm = np.zeros((128, 128), np.float32)
    for a in range(2):
        for f in range(32):
            rm[a * 64 + 32 + f, a * 64 + f] = -1.0
            rm[a * 64 + f, a * 64 + 32 + f] = 1.0
    return rm


def w_in_layout(w_in_l):
    return np.ascontiguousarray(w_in_l.reshape(NCH, 128, NIC, 128).transpose(2, 1, 0, 3))


def run(nc, ins, n=NCORES):
    res = run_bass_kernel_spmd(nc, ins, core_ids=list(range(n)))
    return res.results


NKT = (S + C) // 128
SCALE = 128.0 ** -0.5


def build_p2(ctx_out, nqg=16, lchunks=16):
    nc = new_nc()
    s = Sch(nc)
    NQ = nqg * 512
    NTOK = NQ + (C if ctx_out else 0)
    LL = lchunks * 512
    qa = din(nc, "qa", [128, NQ + C], F32R)
    ka = din(nc, "ka", [128, S + C], F32R)
    va = din(nc, "va", [128, NKT, 128], F32R)
    qn = din(nc, "qn", [128, NQ + C], F32R)
    kn = din(nc, "kn", [128, S + C], F32R)
    vn = din(nc, "vn", [128, NKT, 128], F32R)
    bias = din(nc, "bias", [3, 8, 128, 512])
    ub = din(nc, "ub", [2, 128, LL + C], F32R)
    gb = din(nc, "gb", [2, 128, LL + C], F32R)
    cw = din(nc, "cw", [128, 2, 5])
    wri = din(nc, "wri", [2, 2, 2, 128, 128], F32R)
    bri = din(nc, "bri", [128, 2, 2, 2])
    lam = din(nc, "lam", [128, 2, 2])
    oT = dout(nc, "oT", [4, 128, max(NTOK, LL + (C if ctx_out else 0))])
    outr = R()
    cr = R()
    G = [sb(nc, "G%d" % i, [128, S + C], F32R) for i in range(3)]
    Gf = [g.bitcast(F32) for g in G]
    Gr = [R() for _ in range(3)]
    ones = sb(nc, "ones", [128, 128], F32R)
    s.op("dve", lambda: nc.vector.memset(ones.bitcast(F32), 1.0), wr=[cr])
    onec = sb(nc, "onec", [128, 1])
    s.op("dve", lambda: nc.vector.memset(onec, 1.0), wr=[cr])
    psS = Ring(nc, "pS", 3, [128, 512], F32, psum=True)
    psO = Ring(nc, "pO", 2, [128, 512], F32, psum=True)
    psD = Ring(nc, "pD", 2, [128, 512], F32, psum=True)
    ptr_ = Ring(nc, "pt", 3, [128, 512], F32R)
    tmr = Ring(nc, "tm", 3, [128, 512])
    bsr = Ring(nc, "bs", 4, [128, 512])
    rcr = Ring(nc, "rc", 2, [128, 512])
    str_ = Ring(nc, "st", 3, [128, 512])

    def attend(q_ap, nq, ktiles, out_ap):
        po, por = psO.nxt()
        pd, pdr = psD.nxt()
        nk = len(ktiles)

        def emit_s(i):
            ps, psr = psS.nxt()
            mm(s, ps[:, :nq], psr, ktiles[i][0], q_ap, True, True, [Gr[0], Gr[1]])
            return ps, psr
        cur = emit_s(0)
        for i in range(nk):
            nxt_ = emit_s(i + 1) if i + 1 < nk else None
            ps, psr = cur
            pt, ptr__ = ptr_.nxt()
            bd = ktiles[i][2]
            if bd is not None:
                bt, btr = bsr.nxt()
                s.dma("sp", bt[:, :nq], bd[:, :nq], wr=[btr])
                tm, tmr_ = tmr.nxt()
                s.op("dve", lambda tm=tm, ps=ps, bt=bt: nc.vector.scalar_tensor_tensor(
                    out=tm[:, :nq], in0=ps[:, :nq], scalar=SCALE, in1=bt[:, :nq], op0=ALU.mult, op1=ALU.add),
                    rd=[psr, btr], wr=[tmr_])
                s.op("act", lambda pt=pt, tm=tm: nc.scalar.activation(out=pt[:, :nq], in_=tm[:, :nq], func=AF.Exp),
                     rd=[tmr_], wr=[ptr__])
            else:
                s.op("act", lambda pt=pt, ps=ps: nc.scalar.activation(out=pt[:, :nq], in_=ps[:, :nq], func=AF.Exp,
                                                                      scale=SCALE), rd=[psr], wr=[ptr__])
            mm(s, po[:, :nq], por, ktiles[i][1], pt[:, :nq], i == 0, i == nk - 1, [ptr__, Gr[2]])
            mm(s, pd[:, :nq], pdr, ones, pt[:, :nq], i == 0, i == nk - 1, [ptr__, cr])
            cur = nxt_
        rc, rcr_ = rcr.nxt()
        s.op("dve", lambda: nc.vector.reciprocal(out=rc[:, :nq], in_=pd[:, :nq]), rd=[pdr], wr=[rcr_])
        st, sr = str_.nxt()
        s.op("dve", lambda: nc.vector.tensor_tensor(out=st[:, :nq], in0=po[:, :nq], in1=rc[:, :nq], op=ALU.mult),
             rd=[por, rcr_], wr=[sr])
        s.dma("pool", out_ap, st[:, :nq], rd=[sr], wr=[outr])

    Gq = G[0]; Gk = G[1]; Gv = G[2].rearrange("p (t d) -> p t d", d=128)
    for (qd, kd, vd, och, is_na) in ((qa, ka, va, 0, False), (qn, kn, vn, 3, True)):
        s.dma("sp", Gq[:, :NQ + C], qd, wr=[Gr[0]])
        for h in range(2):
            s.dma("sp", Gk[:, h * 4224:(h + 1) * 4224], kd[:, h * 4224:(h + 1) * 4224], wr=[Gr[1]])
            s.dma("sp", Gv[:, h * 33:(h + 1) * 33, :], vd[:, h * 33:(h + 1) * 33, :], wr=[Gr[2]])
        for g in range(nqg):
            if not is_na:
                kts = [(Gk[:, t * 128:(t + 1) * 128], Gv[:, t, :], None) for t in range(NKT)]
            else:
                kr0 = min(max(8 * g - 4, 0), 112)
                tab = 0 if g == 0 else (2 if g == 15 else 1)
                kts = []
                for t8 in range(8):
                    t = kr0 // 2 + t8
                    kts.append((Gk[:, t * 128:(t + 1) * 128], Gv[:, t, :], bias[tab, t8]))
                for t in (64, 65):
                    kts.append((Gk[:, t * 128:(t + 1) * 128], Gv[:, t, :], None))
            attend(Gq[:, g * 512:(g + 1) * 512], 512, kts, oT[och, :, g * 512:(g + 1) * 512])
        if ctx_out:
            kts = [(Gk[:, t * 128:(t + 1) * 128], Gv[:, t, :], None) for t in (64, 65)]
            attend(Gq[:, NQ:NQ + C], C, kts, oT[och, :, NQ:NQ + C])

    cwt = sb(nc, "cwt", [128, 2, 5]); brit = sb(nc, "brit", [128, 2, 2, 2]); lamt = sb(nc, "lamt", [128, 2, 2])
    spt = sb(nc, "spt", [128, 2, 2])
    wt = sb(nc, "wt", [128, 8, 128], F32R)
    cr2 = R()
    s.dma("sp", cwt, cw, wr=[cr2]); s.dma("sp", brit, bri, wr=[cr2]); s.dma("sp", lamt, lam, wr=[cr2])
    s.dma("sp", wt, wri.rearrange("a d b k m -> k (a d b) m"), wr=[cr2])
    s.op("act", lambda: nc.scalar.activation(out=spt, in_=lamt, func=AF.Exp, scale=-1.0), rd=[cr2], wr=[cr2])
    s.op("act", lambda: nc.scalar.activation(out=spt, in_=spt, func=AF.Ln, bias=onec[:, 0:1], scale=1.0),
         rd=[cr2, cr], wr=[cr2])
    s.op("dve", lambda: nc.vector.tensor_scalar(out=spt, in0=spt, scalar1=-8.0, scalar2=None, op0=ALU.mult),
         rd=[cr2], wr=[cr2])
    U, UC, Y = Gf
    UW, UCW, YW = G
    Ur, UCr, Yr = Gr
    ar = Ring(nc, "la", 2, [128, 512]); br_ = Ring(nc, "lb", 2, [128, 512]); ir = Ring(nc, "li", 2, [128, 512])
    hr = Ring(nc, "lh", 2, [128, 512]); gr = Ring(nc, "lg", 2, [128, 512]); g2r = Ring(nc, "lg2", 2, [128, 512])
    ucr = Ring(nc, "luc", 2, [128, 512], F32R)
    hst = sb(nc, "hst", [128, 4]); hstr = R()
    segs = [("ctx", LL, C), ("lat", 0, LL)]
    for blk in range(2):
        s.dma("sp", UW[:, :LL + C], ub[blk], wr=[Ur])
        for (_, o, L) in segs:
            s.op("dve", lambda o=o, L=L: nc.vector.tensor_scalar(
                out=UCW[:, o:o + L], in0=U[:, o:o + L], scalar1=cwt[:, blk, 1:2], scalar2=cwt[:, blk, 4:5],
                op0=ALU.mult, op1=ALU.add), rd=[Ur, cr2], wr=[UCr])
            for (tap, do, so, n) in ((0, 1, 0, L - 1), (2, 0, 1, L - 1), (3, 0, 2, L - 2)):
                s.op("dve", lambda o=o, tap=tap, do=do, so=so, n=n: nc.vector.scalar_tensor_tensor(
                    out=UCW[:, o + do:o + do + n], in0=U[:, o + so:o + so + n], scalar=cwt[:, blk, tap:tap + 1],
                    in1=UC[:, o + do:o + do + n], op0=ALU.mult, op1=ALU.add), rd=[Ur, cr2, UCr], wr=[UCr])
        s.op("dve", lambda: nc.vector.memset(hst, 0.0), wr=[hstr])
        for d in range(2):
            chunks = [(LL, C)] + [(c * 512, 512) for c in (range(lchunks) if d == 0 else range(lchunks - 1, -1, -1))]
            prev = None
            for ci, (o, n) in enumerate(chunks):
                ucc, uccr = ucr.nxt()
                s.op("act", lambda ucc=ucc, o=o, n=n: nc.scalar.copy(out=ucc[:, :n], in_=UC[:, o:o + n]),
                     rd=[UCr], wr=[uccr])
                pr_, prr = psS.nxt()
                pi_, pir = psS.nxt()
                mm(s, pr_[:, :n], prr, wt[:, (0 * 2 + d) * 2 + blk, :], ucc[:, :n], True, True, [uccr, cr2])
                mm(s, pi_[:, :n], pir, wt[:, (1 * 2 + d) * 2 + blk, :], ucc[:, :n], True, True, [uccr, cr2])
                a, ar_ = ar.nxt(); b_, bbr = br_.nxt(); it, itr = ir.nxt(); h, hrr = hr.nxt()
                s.op("act", lambda a=a, pr_=pr_, n=n: nc.scalar.activation(
                    out=a[:, :n], in_=pr_[:, :n], func=AF.Sigmoid, bias=brit[:, 0, d, blk:blk + 1], scale=1.0),
                    rd=[prr, cr2], wr=[ar_])
                s.op("act", lambda it=it, pi_=pi_, n=n: nc.scalar.activation(
                    out=it[:, :n], in_=pi_[:, :n], func=AF.Sigmoid, bias=brit[:, 1, d, blk:blk + 1], scale=1.0),
                    rd=[pir, cr2], wr=[itr])
                s.op("act", lambda a=a, n=n: nc.scalar.activation(
                    out=a[:, :n], in_=a[:, :n], func=AF.Exp, scale=spt[:, d, blk:blk + 1]), rd=[ar_, cr2], wr=[ar_])
                s.op("act", lambda a=a, b_=b_, n=n: nc.scalar.activation(out=b_[:, :n], in_=a[:, :n], func=AF.Square),
                     rd=[ar_], wr=[bbr])
                s.op("act", lambda b_=b_, n=n: nc.scalar.activation(
                    out=b_[:, :n], in_=b_[:, :n], func=AF.Sqrt, bias=onec[:, 0:1], scale=-1.0), rd=[bbr, cr], wr=[bbr])
                s.op("dve", lambda it=it, o=o, n=n: nc.vector.tensor_tensor(
                    out=it[:, :n], in0=it[:, :n], in1=UC[:, o:o + n], op=ALU.mult), rd=[itr, UCr], wr=[itr])
                s.op("dve", lambda it=it, b_=b_, n=n: nc.vector.tensor_tensor(
                    out=b_[:, :n], in0=b_[:, :n], in1=it[:, :n], op=ALU.mult), rd=[itr, bbr], wr=[bbr])
                if d == 0:
                    hv, av, bv = h[:, :n], a[:, :n], b_[:, :n]
                    last = h[:, n - 1:n]
                else:
                    hv, av, bv = h[:, n - 1::-1] if n < 512 else h[:, ::-1], None, None
                    hv = h[:, 0:n][:, ::-1]; av = a[:, 0:n][:, ::-1]; bv = b_[:, 0:n][:, ::-1]
                    last = h[:, 0:1]
                init = 0.0 if ci == 0 else hst[:, d:d + 1]
                s.op("dve", lambda hv=hv, av=av, bv=bv, init=init: nc.vector.tensor_tensor_scan(
                    out=hv, data0=av, data1=bv, initial=init, op0=ALU.mult, op1=ALU.add),
                    rd=[ar_, bbr, hstr], wr=[hrr])
                s.op("dve", lambda last=last: nc.vector.tensor_copy(out=hst[:, d:d + 1], in_=last),
                     rd=[hrr], wr=[hstr])
                if d == 0:
                    s.op("pool", lambda h=h, o=o, n=n: nc.gpsimd.tensor_copy(out=YW[:, o:o + n], in_=h[:, :n]),
                         rd=[hrr], wr=[Yr])
                else:
                    s.op("pool", lambda h=h, o=o, n=n: nc.gpsimd.tensor_tensor(
                        out=YW[:, o:o + n], in0=Y[:, o:o + n], in1=h[:, :n], op=ALU.add), rd=[hrr, Yr], wr=[Yr])
        s.dma("sp", UW[:, :LL + C], gb[blk], wr=[Ur])
        ochunks = [(c * 512, 512, c * 512) for c in range(lchunks)]
        if ctx_out:
            ochunks.append((LL, C, NQ if NQ >= LL else LL))
        for (o, n, oo) in ochunks:
            g2, g2r_ = g2r.nxt(); gg, ggr = gr.nxt()
            gv = U[:, o:o + n]
            s.op("dve", lambda g2=g2, gv=gv, n=n: nc.vector.tensor_tensor(out=g2[:, :n], in0=gv, in1=gv, op=ALU.mult),
                 rd=[Ur], wr=[g2r_])
            s.op("dve", lambda g2=g2, n=n: nc.vector.tensor_scalar(
                out=g2[:, :n], in0=g2[:, :n], scalar1=0.044715, scalar2=1.0, op0=ALU.mult, op1=ALU.add),
                rd=[g2r_], wr=[g2r_])
            s.op("dve", lambda g2=g2, gv=gv, n=n: nc.vector.tensor_tensor(
                out=g2[:, :n], in0=g2[:, :n], in1=gv, op=ALU.mult), rd=[g2r_, Ur], wr=[g2r_])
            s.op("act", lambda g2=g2, gg=gg, n=n: nc.scalar.activation(
                out=gg[:, :n], in_=g2[:, :n], func=AF.Sigmoid, scale=1.5957691216057308), rd=[g2r_], wr=[ggr])
            s.op("dve", lambda gg=gg, gv=gv, n=n: nc.vector.tensor_tensor(
                out=gg[:, :n], in0=gg[:, :n], in1=gv, op=ALU.mult), rd=[ggr, Ur], wr=[ggr])
            st, sr = str_.nxt()
            s.op("dve", lambda st=st, gg=gg, o=o, n=n: nc.vector.tensor_tensor(
                out=st[:, :n], in0=gg[:, :n], in1=Y[:, o:o + n], op=ALU.mult), rd=[ggr, Yr], wr=[sr])
            s.dma("pool", oT[1 + blk, :, oo:oo + n], st[:, :n], rd=[sr], wr=[outr])
    s.finish([outr])
    return nc


def na_bias_tables(rpb_h):
    out = np.full((3, 8, 128, 512), -30000.0, np.float32)
    p = np.arange(128)
    qi = np.arange(512)
    qro = qi // 64
    j = qi % 64
    for ti, G in enumerate((0, 1, 15)):
        r = 8 * G + qro
        rs = np.clip(r - 4, 0, 120)
        cs = np.clip(j - 8, 0, 48)
        kr0 = min(max(8 * G - 4, 0), 112)
        for t8 in range(8):
            krow = (kr0 + 2 * t8 + p // 64)[:, None]
            kcol = (p % 64)[:, None]
            ok = (krow >= rs[None]) & (krow < rs[None] + 8) & (kcol >= cs[None]) & (kcol < cs[None] + 16)
            dr = np.clip(krow - r[None] + 7, 0, 14)
            dc = np.clip(kcol - j[None] + 15, 0, 30)
            vals = rpb_h[dr, dc]
            out[ti, t8] = np.where(ok, vals, np.float32(-30000.0))
    return out


def build_p3(tiles):
    nc = new_nc()
    s = Sch(nc)
    T = sum(n for n, _ in tiles)
    xT = din(nc, "xT", [NCH, 128, T])
    oT = din(nc, "oT", [NCH, 128, T], F32R)
    wo = din(nc, "wo", [NCH, 128, NCH, 128], F32R)
    gate = din(nc, "gate", [128, 2, NCH])
    g2 = din(nc, "g2", [128, NCH])
    scl = din(nc, "scl", [128, 2, NCH])
    sft = din(nc, "sft", [128, 2, NCH])
    wr = din(nc, "wr", [128, NCH, 36])
    rb = din(nc, "rb", [128, 36])
    iot = din(nc, "iot", [128, 32])
    x1T = dout(nc, "x1T", [NCH, 128, T])
    h2T = dout(nc, "h2T", [NCH, 128, T])
    rt = dout(nc, "rt", [T, 4])
    outr = R(); cr = R()
    gatet = sb(nc, "gatet", [128, 2, NCH]); g2t = sb(nc, "g2t", [128, NCH]); sclt = sb(nc, "sclt", [128, 2, NCH])
    sftt = sb(nc, "sftt", [128, 2, NCH]); wrt = sb(nc, "wrt", [128, NCH, 36]); rbt = sb(nc, "rbt", [128, 36])
    iott = sb(nc, "iott", [128, 32]); gst = sb(nc, "gst", [128, 2, NCH])
    onesD = sb(nc, "onesD", [128, 128], F32R); epst = sb(nc, "epst", [128, 1])
    for a, b_ in ((gatet, gate), (g2t, g2), (sclt, scl), (sftt, sft), (wrt, wr), (rbt, rb), (iott, iot)):
        s.dma("sp", a, b_, wr=[cr])
    s.op("dve", lambda: nc.vector.memset(onesD.bitcast(F32), 1.0 / D), wr=[cr])
    s.op("dve", lambda: nc.vector.memset(epst, EPS), wr=[cr])
    for k in range(2):
        s.op("dve", lambda k=k: nc.vector.scalar_tensor_tensor(out=gst[:, k, :], in0=sclt[:, k, :], scalar=1.0,
                                                               in1=g2t, op0=ALU.add, op1=ALU.mult), rd=[cr], wr=[cr])
    xr = Ring(nc, "x", 2, [128, NCH, 512])
    orr = Ring(nc, "o", 1, [128, NCH, 512], F32R)
    hr = Ring(nc, "h", 1, [128, NCH, 512])
    sqr = Ring(nc, "sq", 2, [128, 512], F32R)
    wr_ = Ring(nc, "w", 4, [128, NCH, 128], F32R)
    psA = Ring(nc, "pA", 3, [128, 512], F32, psum=True)
    psB = Ring(nc, "pB", 2, [128, 512], F32, psum=True)
    psR = Ring(nc, "pR", 2, [128, 64], F32, psum=True)
    rsr = Ring(nc, "rs", 2, [128, 512])
    smr = Ring(nc, "sm", 2, [128, 160])
    t0 = 0
    for (n, kind) in tiles:
        xt, xtr = xr.nxt()
        ot, otr = orr.nxt()
        s.dma("sp", xt[:, :, :n], xT[:, :, t0:t0 + n].rearrange("c p t -> p c t"), wr=[xtr])
        s.dma("sp", ot[:, :, :n], oT[:, :, t0:t0 + n].rearrange("c p t -> p c t"), wr=[otr])
        for fc in range(NCH):
            wt, wtr = wr_.nxt()
            s.dma("sp", wt, wo[fc], wr=[wtr])
            pa, par = psA.nxt()
            for c in range(NCH):
                mm(s, pa[:, :n], par, wt[:, c, :], ot[:, c, :n], c == 0, c == NCH - 1, [wtr, otr])
            s.op("dve", lambda fc=fc, pa=pa: nc.vector.scalar_tensor_tensor(
                out=xt[:, fc, :n], in0=pa[:, :n], scalar=gatet[:, kind, fc:fc + 1], in1=xt[:, fc, :n],
                op0=ALU.mult, op1=ALU.add), rd=[par, cr, xtr], wr=[xtr])
        s.dma("pool", x1T[:, :, t0:t0 + n].rearrange("c p t -> p c t"), xt[:, :, :n], rd=[xtr], wr=[outr])
        pb, pbr = psB.nxt()
        for c in range(NCH):
            sq, sqr_ = sqr.nxt()
            s.op("act", lambda c=c, sq=sq: nc.scalar.activation(out=sq[:, :n], in_=xt[:, c, :n], func=AF.Square),
                 rd=[xtr], wr=[sqr_])
            mm(s, pb[:, :n], pbr, onesD, sq[:, :n], c == 0, c == NCH - 1, [sqr_, cr])
        rs, rsr_ = rsr.nxt()
        rstd(s, rs[:, :n], rsr_, pb[:, :n], pbr, epst, cr)
        ht, htr = hr.nxt()
        for c in range(NCH):
            s.op("dve", lambda c=c: nc.vector.scalar_tensor_tensor(
                out=ht[:, c, :n], in0=xt[:, c, :n], scalar=gst[:, kind, c:c + 1], in1=rs[:, :n],
                op0=ALU.mult, op1=ALU.mult), rd=[xtr, rsr_, cr], wr=[htr])
            s.op("act", lambda c=c: nc.scalar.activation(out=ht[:, c, :n], in_=ht[:, c, :n], func=AF.Identity,
                                                         bias=sftt[:, kind, c:c + 1], scale=1.0),
                 rd=[htr, cr], wr=[htr])
        s.dma("pool", h2T[:, :, t0:t0 + n].rearrange("c p t -> p c t"), ht[:, :, :n], rd=[htr], wr=[outr])
        for sub in range((n + 127) // 128):
            nt = min(128, n - sub * 128)
            pr_, prr = psR.nxt()
            for c in range(NCH):
                mm(s, pr_[:nt, :36], prr, ht[:, c, sub * 128:sub * 128 + nt], wrt[:, c, :], c == 0, c == NCH - 1,
                   [htr, cr])
            sm, smr_ = smr.nxt()
            lg = sm[:nt, 0:36]; lem = sm[:nt, 36:68]; mk = sm[:nt, 68:100]; tmp = sm[:nt, 100:132]
            sc = sm[:nt, 132:160]

            def V(fn, rd=(), wr=()):
                s.op("dve", fn, rd=list(rd) + [smr_], wr=[smr_] + list(wr))
            V(lambda: nc.vector.tensor_tensor(out=lg, in0=pr_[:nt, :36], in1=rbt[:nt, :], op=ALU.add), rd=[prr, cr])
            V(lambda: nc.vector.tensor_reduce(out=sc[:, 0:1], in_=lg[:, 0:4], axis=AX.X, op=ALU.max))
            V(lambda: nc.vector.tensor_scalar(out=sc[:, 1:2], in0=sc[:, 0:1], scalar1=-1.0, scalar2=None, op0=ALU.mult))
            s.op("act", lambda: nc.scalar.activation(out=sc[:, 12:16], in_=lg[:, 0:4], func=AF.Exp, bias=sc[:, 1:2],
                                                     scale=1.0), rd=[smr_], wr=[smr_])
            V(lambda: nc.vector.tensor_reduce(out=sc[:, 2:3], in_=sc[:, 12:16], axis=AX.X, op=ALU.add))
            V(lambda: nc.vector.reciprocal(out=sc[:, 3:4], in_=sc[:, 2:3]))
            V(lambda: nc.vector.tensor_scalar(out=sc[:, 16:20], in0=lg[:, 0:4], scalar1=sc[:, 0:1], scalar2=None,
                                              op0=ALU.is_ge))
            V(lambda: nc.vector.tensor_scalar(out=sc[:, 16:20], in0=sc[:, 16:20], scalar1=-1.0, scalar2=1e30,
                                              op0=ALU.add, op1=ALU.mult))
            for g in range(4):
                V(lambda g=g: nc.vector.tensor_scalar(out=lem[:, g * 8:(g + 1) * 8], in0=lg[:, 4 + g * 8:12 + g * 8],
                                                      scalar1=sc[:, 16 + g:17 + g], scalar2=None, op0=ALU.add))
            for k in range(2):
                V(lambda k=k: nc.vector.tensor_reduce(out=sc[:, 4 + k:5 + k], in_=lem, axis=AX.X, op=ALU.max))
                V(lambda k=k: nc.vector.tensor_scalar(out=mk, in0=lem, scalar1=sc[:, 4 + k:5 + k], scalar2=None,
                                                      op0=ALU.is_ge))
                V(lambda: nc.vector.tensor_tensor(out=tmp, in0=mk, in1=iott[:nt, :], op=ALU.mult), rd=[cr])
                V(lambda k=k: nc.vector.tensor_reduce(out=sc[:, 6 + k:7 + k], in_=tmp, axis=AX.X, op=ALU.add))
                if k == 0:
                    V(lambda: nc.vector.scalar_tensor_tensor(out=lem, in0=mk, scalar=-1e30, in1=lem,
                                                             op0=ALU.mult, op1=ALU.add))
            V(lambda: nc.vector.tensor_tensor(out=sc[:, 10:11], in0=sc[:, 5:6], in1=sc[:, 4:5], op=ALU.subtract))
            s.op("act", lambda: nc.scalar.activation(out=sc[:, 10:11], in_=sc[:, 10:11], func=AF.Exp),
                 rd=[smr_], wr=[smr_])
            V(lambda: nc.vector.tensor_scalar(out=sc[:, 10:11], in0=sc[:, 10:11], scalar1=1.0, scalar2=None,
                                              op0=ALU.add))
            V(lambda: nc.vector.reciprocal(out=sc[:, 11:12], in_=sc[:, 10:11]))
            V(lambda: nc.vector.tensor_tensor(out=sc[:, 8:9], in0=sc[:, 11:12], in1=sc[:, 3:4], op=ALU.mult))
            V(lambda: nc.vector.tensor_tensor(out=sc[:, 9:10], in0=sc[:, 3:4], in1=sc[:, 8:9], op=ALU.subtract))
            s.dma("pool", rt[t0 + sub * 128:t0 + sub * 128 + nt, :], sc[:, 6:10], rd=[smr_], wr=[outr])
        t0 += n
    s.finish([outr])
    return nc


def build_p4(cap, nel=4):
    nc = new_nc()
    s = Sch(nc)
    XT = din(nc, "XT", [nel, NCH, 128, cap], F32R)
    wg = din(nc, "wg", [nel, NHC, 128, NCH, 128], F32R)
    wu = din(nc, "wu", [nel, NHC, 128, NCH, 128], F32R)
    wd = din(nc, "wd", [nel, NCH, 128, NHC, 128], F32R)
    YT = dout(nc, "YT", [nel, NCH, 128, cap])
    outr = R()
    GS = 1024
    xr = Ring(nc, "x", 1, [128, NCH, GS], F32R)
    wgr = Ring(nc, "wg", 3, [128, NCH, 128], F32R)
    wur = Ring(nc, "wu", 3, [128, NCH, 128], F32R)
    wdr = Ring(nc, "wd", 4, [128, NHC, 128], F32R)
    ar = Ring(nc, "a", 1, [128, NHC, GS], F32R)
    sgr = Ring(nc, "sg", 2, [128, 512])
    str_ = Ring(nc, "st", 3, [128, 512])
    psG = Ring(nc, "pG", 2, [128, 512], F32, psum=True)
    psU = Ring(nc, "pU", 2, [128, 512], F32, psum=True)
    psO = Ring(nc, "pO", 3, [128, 512], F32, psum=True)
    groups = [(g0, min(GS, cap - g0)) for g0 in range(0, cap, GS)]
    for e in range(nel):
        for (g0, gn) in groups:
            tl_ = [(q0, min(512, gn - q0)) for q0 in range(0, gn, 512)]
            xt, xtr = xr.nxt()
            s.dma("sp", xt[:, :, :gn], XT[e, :, :, g0:g0 + gn].rearrange("c p t -> p c t"), wr=[xtr])
            at, atr = ar.nxt()
            for hc in range(NHC):
                wgt, wgtr = wgr.nxt(); wut, wutr = wur.nxt()
                s.dma("sp", wgt, wg[e, hc], wr=[wgtr])
                s.dma("sp", wut, wu[e, hc], wr=[wutr])
                for (q0, qn_) in tl_:
                    ts_ = slice(q0, q0 + qn_)
                    pg, pgr = psG.nxt(); pu, pur = psU.nxt()
                    for c in range(NCH):
                        mm(s, pg[:, :qn_], pgr, wgt[:, c, :], xt[:, c, ts_], c == 0, c == NCH - 1, [wgtr, xtr])
                    for c in range(NCH):
                        mm(s, pu[:, :qn_], pur, wut[:, c, :], xt[:, c, ts_], c == 0, c == NCH - 1, [wutr, xtr])
                    sg, sgr_ = sgr.nxt()
                    s.op("act", lambda sg=sg, pg=pg: nc.scalar.activation(out=sg[:, :qn_], in_=pg[:, :qn_], func=AF.Silu),
                         rd=[pgr], wr=[sgr_])
                    s.op("dve", lambda sg=sg, pu=pu, hc=hc, at=at, ts_=ts_: nc.vector.tensor_tensor(
                        out=at[:, hc, ts_], in0=sg[:, :qn_], in1=pu[:, :qn_], op=ALU.mult), rd=[sgr_, pur], wr=[atr])
            for fc in range(NCH):
                wdt, wdtr = wdr.nxt()
                s.dma("sp", wdt, wd[e, fc], wr=[wdtr])
                for ti, (q0, qn_) in enumerate(tl_):
                    ts_ = slice(q0, q0 + qn_)
                    po, por = psO.nxt()
                    for hc in range(NHC):
                        mm(s, po[:, :qn_], por, wdt[:, hc, :], at[:, hc, ts_], hc == 0, hc == NHC - 1, [wdtr, atr])
                    st, sr = str_.nxt()
                    if (fc + ti) % 2 == 0:
                        s.op("act", lambda st=st, po=po: nc.scalar.copy(out=st[:, :qn_], in_=po[:, :qn_]),
                             rd=[por], wr=[sr])
                    else:
                        s.op("dve", lambda st=st, po=po: nc.vector.tensor_copy(out=st[:, :qn_], in_=po[:, :qn_]),
                             rd=[por], wr=[sr])
                    s.dma("pool", YT[e, fc, :, g0 + q0:g0 + q0 + qn_], st[:, :qn_], rd=[sr], wr=[outr])
    s.finish([outr])
    return nc


def build_p5(tiles, final):
    nc = new_nc()
    s = Sch(nc)
    T = sum(n for n, _ in tiles)
    x1T = din(nc, "x1T", [NCH, 128, T])
    y1T = din(nc, "y1T", [NCH, 128, T])
    y2T = din(nc, "y2T", [NCH, 128, T])
    wbc = din(nc, "wbc", [128, 2, T])
    gate = din(nc, "gate", [128, 2, NCH])
    fg = din(nc, "fg", [128, NCH])
    xoT = dout(nc, "xoT", [NCH, 128, T])
    outr = R(); cr = R()
    gatet = sb(nc, "gatet", [128, 2, NCH]); fgt = sb(nc, "fgt", [128, NCH])
    onesD = sb(nc, "onesD", [128, 128], F32R); epst = sb(nc, "epst", [128, 1])
    s.dma("sp", gatet, gate, wr=[cr]); s.dma("sp", fgt, fg, wr=[cr])
    s.op("dve", lambda: nc.vector.memset(onesD.bitcast(F32), 1.0 / D), wr=[cr])
    s.op("dve", lambda: nc.vector.memset(epst, EPS), wr=[cr])
    xr = Ring(nc, "x", 2, [128, NCH, 512])
    y1r = Ring(nc, "y1", 1, [128, NCH, 512])
    y2r = Ring(nc, "y2", 1, [128, NCH, 512])
    wr_ = Ring(nc, "w", 2, [128, 2, 512])
    sqr = Ring(nc, "sq", 2, [128, 512], F32R)
    rsr = Ring(nc, "rs", 2, [128, 512])
    psB = Ring(nc, "pB", 2, [128, 512], F32, psum=True)
    t0 = 0
    for (n, kind) in tiles:
        xt, xtr = xr.nxt(); y1, y1r_ = y1r.nxt(); y2, y2r_ = y2r.nxt(); wt, wtr = wr_.nxt()
        s.dma("sp", xt[:, :, :n], x1T[:, :, t0:t0 + n].rearrange("c p t -> p c t"), wr=[xtr])
        s.dma("sp", y1[:, :, :n], y1T[:, :, t0:t0 + n].rearrange("c p t -> p c t"), wr=[y1r_])
        s.dma("sp", y2[:, :, :n], y2T[:, :, t0:t0 + n].rearrange("c p t -> p c t"), wr=[y2r_])
        s.dma("sp", wt[:, :, :n], wbc[:, :, t0:t0 + n], wr=[wtr])
        for c in range(NCH):
            s.op("dve", lambda c=c: nc.vector.tensor_tensor(out=y1[:, c, :n], in0=y1[:, c, :n], in1=wt[:, 0, :n],
                                                            op=ALU.mult), rd=[y1r_, wtr], wr=[y1r_])
            s.op("pool", lambda c=c: nc.gpsimd.tensor_tensor(out=y2[:, c, :n], in0=y2[:, c, :n], in1=wt[:, 1, :n],
                                                             op=ALU.mult), rd=[y2r_, wtr], wr=[y2r_])
            s.op("dve", lambda c=c: nc.vector.tensor_tensor(out=y1[:, c, :n], in0=y1[:, c, :n], in1=y2[:, c, :n],
                                                            op=ALU.add), rd=[y1r_, y2r_], wr=[y1r_])
            s.op("dve", lambda c=c: nc.vector.scalar_tensor_tensor(
                out=xt[:, c, :n], in0=y1[:, c, :n], scalar=gatet[:, kind, c:c + 1], in1=xt[:, c, :n],
                op0=ALU.mult, op1=ALU.add), rd=[y1r_, cr, xtr], wr=[xtr])
        if final:
            pb, pbr = psB.nxt()
            for c in range(NCH):
                sq, sqr_ = sqr.nxt()
                s.op("act", lambda c=c, sq=sq: nc.scalar.activation(out=sq[:, :n], in_=xt[:, c, :n], func=AF.Square),
                     rd=[xtr], wr=[sqr_])
                mm(s, pb[:, :n], pbr, onesD, sq[:, :n], c == 0, c == NCH - 1, [sqr_, cr])
            rs, rsr_ = rsr.nxt()
            rstd(s, rs[:, :n], rsr_, pb[:, :n], pbr, epst, cr)
            for c in range(NCH):
                s.op("dve", lambda c=c: nc.vector.scalar_tensor_tensor(
                    out=xt[:, c, :n], in0=xt[:, c, :n], scalar=fgt[:, c:c + 1], in1=rs[:, :n],
                    op0=ALU.mult, op1=ALU.mult), rd=[xtr, rsr_, cr], wr=[xtr])
        s.dma("pool", xoT[:, :, t0:t0 + n].rearrange("c p t -> p c t"), xt[:, :, :n], rd=[xtr], wr=[outr])
        t0 += n
    s.finish([outr])
    return nc


def _vt(vT):
    nt = vT.shape[1] // 128
    return np.ascontiguousarray(vT.T.reshape(nt, 128, 128).transpose(1, 0, 2))


def _wo_layout(w):
    return np.ascontiguousarray(w.reshape(NCH, 128, NCH, 128).transpose(2, 1, 0, 3))


def _wg_layout(w):
    return w.reshape(NCH, 128, NHC, 128).transpose(2, 1, 0, 3)


def _wd_layout(w):
    return w.reshape(NHC, 128, NCH, 128).transpose(2, 1, 0, 3)


def kernel(x, c, ctx, c_ctx, ada_w, ada_b, norm1_g, norm2_g, w_in, w_out, att_q_norm, att_k_norm,
           conv_w, conv_b, lru_wr, lru_br, lru_wi, lru_bi, lru_lambda, na_rpb,
           router_wg, router_bg, router_we, router_be, moe_w_gate, moe_w_up, moe_w_down, final_g):
    f32 = np.float32
    A = lambda a: np.asarray(a, dtype=f32)
    x = A(x); c = A(c); ctx = A(ctx); c_ctx = A(c_ctx); ada_w = A(ada_w); ada_b = A(ada_b)
    norm1_g = A(norm1_g); norm2_g = A(norm2_g); w_in = A(w_in); w_out = A(w_out)
    att_q_norm = A(att_q_norm); att_k_norm = A(att_k_norm); conv_w = A(conv_w); conv_b = A(conv_b)
    lru_wr = A(lru_wr); lru_br = A(lru_br); lru_wi = A(lru_wi); lru_bi = A(lru_bi); lru_lambda = A(lru_lambda)
    na_rpb = A(na_rpb); router_wg = A(router_wg); router_bg = A(router_bg); router_we = A(router_we)
    router_be = A(router_be); moe_w_gate = A(moe_w_gate); moe_w_up = A(moe_w_up); moe_w_down = A(moe_w_down)
    final_g = A(final_g)
    TLc = S // 4
    TCc = C // 4
    tiles_lc = [(512, 0)] * 4 + [(TCc, 1)]
    tiles_l = [(512, 0)] * 4

    NCOL = 12288 // NCORES
    cc = np.stack([c[0], c[1], c_ctx, c_ctx], 0)
    cT = np.ascontiguousarray(cc.T.reshape(NCH, 128, 4).transpose(1, 0, 2))
    ins = []
    for k in range(NCORES):
        ins.append({"cT": cT,
                    "w": np.ascontiguousarray(ada_w[:, :, k * NCOL:(k + 1) * NCOL].reshape(DEPTH, NCH, 128, NCOL)),
                    "bias": np.ascontiguousarray(np.broadcast_to(ada_b[None, :, k * NCOL:(k + 1) * NCOL],
                                                                 (4, DEPTH, NCOL)))})
    res = run(build_ada(), ins)
    mod = np.concatenate([r["mod"] for r in res], axis=2)

    xs = []
    for k in range(NCORES):
        b, r = k // 4, k % 4
        xs.append(tok_fm(np.concatenate([x[b, r * TLc:(r + 1) * TLc], ctx[b, r * TCc:(r + 1) * TCc]], 0)))
    rot = rot_const()
    iot = np.ascontiguousarray(np.broadcast_to(np.arange(32, dtype=f32)[None], (128, 32)))
    out = np.zeros((B, S, D), f32)

    for l in range(DEPTH):
        last = l == DEPTH - 1
        m = mod[:, l].reshape(4, 6, D)
        wl = w_in_layout(w_in[l])
        g1 = fm(norm1_g[l])
        gqk = np.ascontiguousarray(np.stack([att_q_norm[l], att_k_norm[l]], 1))
        ins = []
        for k in range(NCORES):
            b, r = k // 4, k % 4
            ins.append({"xT": xs[k], "w": wl, "g1": g1,
                        "scl": fm(np.stack([m[b, 1], m[2, 1]])), "sft": fm(np.stack([m[b, 0], m[2, 0]])),
                        "gqk": gqk, "cs": rope_consts(np.arange(r * TLc, (r + 1) * TLc)), "rot": rot})
        res = run(build_p1(tiles_lc), ins)
        full = []
        for b in range(B):
            lat = np.concatenate([res[4 * b + r]["PT"][:, :, :TLc] for r in range(4)], axis=2)
            cx = np.concatenate([res[4 * b + r]["PT"][:, :, TLc:] for r in range(4)], axis=2)
            full.append(np.concatenate([lat, cx], axis=2))
        del res
        ins = []
        for k in range(NCORES):
            b, i = k // 4, k % 4
            F = full[b]
            cw = np.zeros((128, 2, 5), f32)
            bri = np.zeros((128, 2, 2, 2), f32)
            lam = np.zeros((128, 2, 2), f32)
            wri = np.zeros((2, 2, 2, 128, 128), f32)
            for kk in range(2):
                j = 2 * i + kk
                ch = slice(j * 128, (j + 1) * 128)
                cw[:, kk, :4] = conv_w[l][:, ch].T
                cw[:, kk, 4] = conv_b[l][ch]
                for d_ in range(2):
                    bri[:, 0, d_, kk] = lru_br[l][d_, ch]
                    bri[:, 1, d_, kk] = lru_bi[l][d_, ch]
                    lam[:, d_, kk] = lru_lambda[l][d_, ch]
                    wri[0, d_, kk] = lru_wr[l][d_, j]
                    wri[1, d_, kk] = lru_wi[l][d_, j]
            ins.append({"qa": np.ascontiguousarray(F[i]), "ka": np.ascontiguousarray(F[4 + i // 2]),
                        "va": _vt(F[6 + i // 2]),
                        "qn": np.ascontiguousarray(F[24 + i]), "kn": np.ascontiguousarray(F[28 + i]),
                        "vn": _vt(F[32 + i]), "bias": na_bias_tables(na_rpb[l, i]),
                        "ub": np.ascontiguousarray(F[8 + 2 * i:10 + 2 * i]),
                        "gb": np.ascontiguousarray(F[16 + 2 * i:18 + 2 * i]),
                        "cw": cw, "wri": wri, "bri": bri, "lam": lam})
        del full
        res = run(build_p2(not last), ins)
        ntok = S + (0 if last else C)
        OT = []
        for b in range(B):
            o = np.zeros((NCH, 128, ntok), f32)
            for i in range(4):
                oc = res[4 * b + i]["oT"]
                o[i] = oc[0][:, :ntok]
                o[4 + 2 * i] = oc[1][:, :ntok]
                o[5 + 2 * i] = oc[2][:, :ntok]
                o[12 + i] = oc[3][:, :ntok]
            OT.append(o)
        del res
        tiles = tiles_l if last else tiles_lc
        Tc = sum(n for n, _ in tiles)
        wol = _wo_layout(w_out[l])
        wcat = np.concatenate([router_wg[l], router_we[l]], 1)
        bcat = np.concatenate([router_bg[l], router_be[l]])
        wrl = np.ascontiguousarray(wcat.reshape(NCH, 128, 36).transpose(1, 0, 2))
        rbl = np.ascontiguousarray(np.broadcast_to(bcat[None], (128, 36)))
        g2 = fm(norm2_g[l])
        ins = []
        for k in range(NCORES):
            b, r = k // 4, k % 4
            if last:
                oTk = np.ascontiguousarray(OT[b][:, :, r * TLc:(r + 1) * TLc])
                xTk = np.ascontiguousarray(xs[k][:, :, :TLc])
            else:
                oTk = np.concatenate([OT[b][:, :, r * TLc:(r + 1) * TLc],
                                      OT[b][:, :, S + r * TCc:S + (r + 1) * TCc]], axis=2)
                xTk = xs[k]
            ins.append({"xT": xTk, "oT": np.ascontiguousarray(oTk), "wo": wol,
                        "gate": fm(np.stack([m[b, 2], m[2, 2]])), "g2": g2,
                        "scl": fm(np.stack([m[b, 4], m[2, 4]])), "sft": fm(np.stack([m[b, 3], m[2, 3]])),
                        "wr": wrl, "rb": rbl, "iot": iot})
        del OT
        res = run(build_p3(tiles), ins)
        x1 = [r_["x1T"] for r_ in res]
        rts = [r_["rt"] for r_ in res]
        H = np.concatenate([fm_tok(r_["h2T"]) for r_ in res], axis=0)
        del res
        rt_all = np.concatenate(rts, axis=0)
        E = np.rint(rt_all[:, :2]).astype(np.int64)
        lists = []
        for e in range(NE):
            tok, kk = np.nonzero(E == e)
            lists.append((tok, kk))
        cap = 128 * max(2, -(-max(len(t) for t, _ in lists) // 128))
        ins = []
        for k in range(NCORES):
            XT = np.zeros((4, NCH, 128, cap), f32)
            wg = np.empty((4, NHC, 128, NCH, 128), f32)
            wu = np.empty((4, NHC, 128, NCH, 128), f32)
            wd = np.empty((4, NCH, 128, NHC, 128), f32)
            for j in range(4):
                e = 4 * k + j
                tok = lists[e][0]
                XT[j][:, :, :len(tok)] = tok_fm(H[tok])
                wg[j] = _wg_layout(moe_w_gate[l, e])
                wu[j] = _wg_layout(moe_w_up[l, e])
                wd[j] = _wd_layout(moe_w_down[l, e])
            ins.append({"XT": XT, "wg": wg, "wu": wu, "wd": wd})
        del H
        res = run(build_p4(cap, 4), ins)
        del ins
        Y = np.zeros((2, NCORES * Tc, D), f32)
        for k in range(NCORES):
            for j in range(4):
                e = 4 * k + j
                tok, kk = lists[e]
                ye = fm_tok(res[k]["YT"][j])
                Y[kk, tok] = ye[:len(tok)]
        del res
        fg = fm(final_g)
        ins = []
        for k in range(NCORES):
            b = k // 4
            sl = slice(k * Tc, (k + 1) * Tc)
            ins.append({"x1T": x1[k], "y1T": tok_fm(Y[0, sl]), "y2T": tok_fm(Y[1, sl]),
                        "wbc": np.ascontiguousarray(np.broadcast_to(rts[k][:, 2:4].T[None], (128, 2, Tc))),
                        "gate": fm(np.stack([m[b, 5], m[2, 5]])), "fg": fg})
        del Y
        res = run(build_p5(tiles, last), ins)
        if last:
            for k in range(NCORES):
                b, r = k // 4, k % 4
                out[b, r * TLc:(r + 1) * TLc] = fm_tok(res[k]["xoT"])
        else:
            xs = [r_["xoT"] for r_ in res]
        del res
    return out
```
